# Optimizing a Trainium2 kernel written in Bass

```python
import jax, jax.numpy as jnp
from jax import lax
import numpy as np

D_MODEL = 1024
BATCH = 4
SEQ = 4096
DEPTH = 2

D_MIX = D_MODEL
CONV_W = D_MIX // 4
LRU_W = (D_MIX - CONV_W) // 2
GLA_V = D_MIX - CONV_W - LRU_W
CONV_K = 31
LRU_CONV_K = 4
LRU_BLOCKS = 6
LRU_BW = LRU_W // LRU_BLOCKS
LRU_C = 8.0
GLA_HEADS = 4
GLA_DV = GLA_V // GLA_HEADS
GLA_DK = GLA_DV // 2
GLA_RANK = 16
GLA_TAU = 16.0
GLA_CHUNK = 64
IN_WIDTH = 2 * CONV_W + 2 * LRU_W + 2 * GLA_HEADS * GLA_DK + GLA_V + GLA_RANK + GLA_V
N_GROUPS = 4
EXPERTS_PER_GROUP = 4
TOP_K = 2
D_EXPERT = D_MODEL // 2
EPS = 1e-6

kernel_name = "hybrid_conv_rglru_gla_hmoe_adaln"


def rms_norm(x, g):
    xf = x.astype(jnp.float32)
    y = xf * lax.rsqrt(jnp.mean(xf * xf, axis=-1, keepdims=True) + EPS)
    return (y * g.astype(jnp.float32)).astype(x.dtype)


def layer_norm(x, g, b):
    xf = x.astype(jnp.float32)
    mu = jnp.mean(xf, axis=-1, keepdims=True)
    var = jnp.mean(jnp.square(xf - mu), axis=-1, keepdims=True)
    y = (xf - mu) * lax.rsqrt(var + EPS)
    return (y * g.astype(jnp.float32) + b.astype(jnp.float32)).astype(x.dtype)


def causal_dw_conv(x, w, b):
    k = w.shape[0]
    y = lax.conv_general_dilated(
        x, w[:, None, :].astype(x.dtype), window_strides=(1,), padding=((k - 1, 0),),
        dimension_numbers=("NWC", "WIO", "NWC"), feature_group_count=x.shape[-1])
    return y + b


def rg_lru(xb, w_a, b_a, w_i, b_i, lam):
    bsz, s, w = xb.shape
    xh = xb.reshape(bsz, s, LRU_BLOCKS, LRU_BW)
    r = jax.nn.sigmoid(jnp.einsum("bshi,hij->bshj", xh, w_a).reshape(bsz, s, w) + b_a)
    i = jax.nn.sigmoid(jnp.einsum("bshi,hij->bshj", xh, w_i).reshape(bsz, s, w) + b_i)
    log_a = -LRU_C * r.astype(jnp.float32) * jax.nn.softplus(-lam.astype(jnp.float32))
    a = jnp.exp(log_a)
    mult = jnp.sqrt(-jnp.expm1(2.0 * log_a))
    u = mult * (i * xb).astype(jnp.float32)

    def combine(left, right):
        a1, b1 = left
        a2, b2 = right
        return a1 * a2, a2 * b1 + b2

    _, h = lax.associative_scan(combine, (a, u), axis=1)
    return h.astype(xb.dtype)


def gla_chunked(q, k, v, lg):
    bsz, s, h, dk = q.shape
    n = s // GLA_CHUNK

    def chunk(t):
        return t.reshape(bsz, n, GLA_CHUNK, h, t.shape[-1]).transpose(0, 3, 1, 2, 4)

    q, k, v, lg = chunk(q) * (dk ** -0.5), chunk(k), chunk(v), chunk(lg)
    b = lax.cumsum(lg, axis=3)
    b_last = b[:, :, :, -1:, :]
    q_in = q * jnp.exp(b)
    k_in = k * jnp.exp(-b)
    mask = jnp.tril(jnp.ones((GLA_CHUNK, GLA_CHUNK), dtype=bool))
    scores = jnp.where(mask, jnp.einsum("bhncd,bhnjd->bhncj", q_in, k_in), 0.0)
    o_intra = jnp.einsum("bhncj,bhnje->bhnce", scores, v)
    kv = jnp.einsum("bhncd,bhnce->bhnde", k * jnp.exp(b_last - b), v)
    decay = jnp.exp(b_last[:, :, :, 0, :])

    def step(state, inp):
        d, kvn = inp
        return d[..., None] * state + kvn, state

    s0 = jnp.zeros((bsz, h, dk, v.shape[-1]), jnp.float32)
    _, s_prev = lax.scan(step, s0, (decay.transpose(2, 0, 1, 3), kv.transpose(2, 0, 1, 3, 4)))
    s_prev = s_prev.transpose(1, 2, 0, 3, 4)
    o = o_intra + jnp.einsum("bhncd,bhnde->bhnce", q_in, s_prev)
    return o.transpose(0, 2, 3, 1, 4).reshape(bsz, s, h, v.shape[-1])


def token_mix(h, w_in, conv_dw_w, conv_dw_b, conv_ln_g, conv_ln_b, lru_conv_w, lru_conv_b,
              lru_w_a, lru_b_a, lru_w_i, lru_b_i, lru_lam, gla_w_gate, gla_b_gate, gla_norm_g, w_out):
    bsz, s, _ = h.shape
    sizes = [CONV_W, CONV_W, LRU_W, LRU_W, GLA_HEADS * GLA_DK, GLA_HEADS * GLA_DK, GLA_V, GLA_RANK, GLA_V]
    z = h @ w_in
    cv_v, cv_g, lr_x, lr_y, q, k, v, g_lr, og = jnp.split(z, np.cumsum(sizes)[:-1].tolist(), axis=-1)
    u = cv_v * jax.nn.sigmoid(cv_g)
    u = causal_dw_conv(u, conv_dw_w, conv_dw_b)
    u = jax.nn.silu(layer_norm(u, conv_ln_g, conv_ln_b))
    r = rg_lru(causal_dw_conv(lr_x, lru_conv_w, lru_conv_b), lru_w_a, lru_b_a, lru_w_i, lru_b_i, lru_lam)
    r = r * jax.nn.gelu(lr_y)
    lg = jax.nn.log_sigmoid((g_lr @ gla_w_gate + gla_b_gate).astype(jnp.float32)) / GLA_TAU
    o = gla_chunked(q.reshape(bsz, s, GLA_HEADS, GLA_DK).astype(jnp.float32),
                    k.reshape(bsz, s, GLA_HEADS, GLA_DK).astype(jnp.float32),
                    v.reshape(bsz, s, GLA_HEADS, GLA_DV).astype(jnp.float32),
                    lg.reshape(bsz, s, GLA_HEADS, GLA_DK))
    o = rms_norm(o, gla_norm_g.reshape(GLA_HEADS, GLA_DV)).reshape(bsz, s, GLA_V).astype(h.dtype)
    o = o * jax.nn.silu(og)
    mixed = jnp.concatenate([u, r, o], axis=-1)
    return mixed @ w_out


def hier_moe(h, w_rg, b_rg, w_re, b_re, w_gate, w_up, w_down):
    hf = h.astype(jnp.float32)
    g_logits = hf @ w_rg.astype(jnp.float32) + b_rg.astype(jnp.float32)
    p_g = jax.nn.softmax(g_logits, axis=-1)
    g_star = jnp.argmax(g_logits, axis=-1)
    p_sel = jnp.take_along_axis(p_g, g_star[..., None], axis=-1)
    e_all = jnp.einsum("bsd,gde->bsge", hf, w_re.astype(jnp.float32)) + b_re.astype(jnp.float32)
    e_logits = jnp.take_along_axis(e_all, g_star[..., None, None], axis=2)[:, :, 0]
    top_v, top_i = lax.top_k(e_logits, TOP_K)
    top_w = jax.nn.softmax(top_v, axis=-1) * p_sel
    w_in_group = jnp.sum(jax.nn.one_hot(top_i, EXPERTS_PER_GROUP) * top_w[..., None], axis=-2)
    comb = (jax.nn.one_hot(g_star, N_GROUPS)[..., None] * w_in_group[..., None, :]).astype(h.dtype)
    y = jnp.zeros_like(h)
    for g in range(N_GROUPS):
        a = jnp.einsum("bsd,edf->bsef", h, w_gate[g])
        up = jnp.einsum("bsd,edf->bsef", h, w_up[g])
        hid = jax.nn.silu(a) * up * comb[:, :, g, :, None]
        y = y + jnp.einsum("bsef,efd->bsd", hid, w_down[g])
    return y


def setup_inputs(seed: int = 0) -> dict:
    key = jax.random.key(seed)
    ks = jax.random.split(key, 32)
    f32 = jnp.float32

    def nrm(k, shape, scale):
        return jax.random.normal(k, shape, f32) * scale

    L = DEPTH
    a0 = jax.random.uniform(ks[14], (L, LRU_W), f32, 0.9, 0.999)
    return {
        "x": nrm(ks[0], (BATCH, SEQ, D_MODEL), 1.0),
        "c": nrm(ks[1], (BATCH, D_MODEL), 1.0),
        "w_ada": nrm(ks[2], (L, D_MODEL, 6 * D_MODEL), 0.5 * D_MODEL ** -0.5),
        "b_ada": nrm(ks[3], (L, 6 * D_MODEL), 0.02),
        "g_mix": 1.0 + nrm(ks[4], (L, D_MODEL), 0.02),
        "w_in": nrm(ks[5], (L, D_MODEL, IN_WIDTH), D_MODEL ** -0.5),
        "conv_dw_w": nrm(ks[6], (L, CONV_K, CONV_W), CONV_K ** -0.5),
        "conv_dw_b": nrm(ks[7], (L, CONV_W), 0.02),
        "conv_ln_g": 1.0 + nrm(ks[8], (L, CONV_W), 0.02),
        "conv_ln_b": nrm(ks[9], (L, CONV_W), 0.02),
        "lru_conv_w": nrm(ks[10], (L, LRU_CONV_K, LRU_W), LRU_CONV_K ** -0.5),
        "lru_conv_b": nrm(ks[11], (L, LRU_W), 0.02),
        "lru_w_a": nrm(ks[12], (L, LRU_BLOCKS, LRU_BW, LRU_BW), LRU_BW ** -0.5),
        "lru_b_a": nrm(ks[13], (L, LRU_W), 0.02),
        "lru_w_i": nrm(ks[15], (L, LRU_BLOCKS, LRU_BW, LRU_BW), LRU_BW ** -0.5),
        "lru_b_i": nrm(ks[16], (L, LRU_W), 0.02),
        "lru_lam": jnp.log(a0) - jnp.log1p(-a0),
        "gla_w_gate": nrm(ks[17], (L, GLA_RANK, GLA_HEADS * GLA_DK), GLA_RANK ** -0.5),
        "gla_b_gate": nrm(ks[18], (L, GLA_HEADS * GLA_DK), 0.02),
        "gla_norm_g": 1.0 + nrm(ks[19], (L, GLA_V), 0.02),
        "w_out": nrm(ks[20], (L, D_MIX, D_MODEL), D_MIX ** -0.5),
        "g_ffn": 1.0 + nrm(ks[21], (L, D_MODEL), 0.02),
        "w_route_group": nrm(ks[22], (L, D_MODEL, N_GROUPS), D_MODEL ** -0.5),
        "b_route_group": nrm(ks[23], (L, N_GROUPS), 0.01),
        "w_route_expert": nrm(ks[24], (L, N_GROUPS, D_MODEL, EXPERTS_PER_GROUP), D_MODEL ** -0.5),
        "b_route_expert": nrm(ks[25], (L, N_GROUPS, EXPERTS_PER_GROUP), 0.01),
        "w_gate": nrm(ks[26], (L, N_GROUPS, EXPERTS_PER_GROUP, D_MODEL, D_EXPERT), D_MODEL ** -0.5),
        "w_up": nrm(ks[27], (L, N_GROUPS, EXPERTS_PER_GROUP, D_MODEL, D_EXPERT), D_MODEL ** -0.5),
        "w_down": nrm(ks[28], (L, N_GROUPS, EXPERTS_PER_GROUP, D_EXPERT, D_MODEL), D_EXPERT ** -0.5),
        "g_final": 1.0 + nrm(ks[29], (D_MODEL,), 0.02),
    }


def reference(x, c, w_ada, b_ada, g_mix, w_in, conv_dw_w, conv_dw_b, conv_ln_g, conv_ln_b,
              lru_conv_w, lru_conv_b, lru_w_a, lru_b_a, lru_w_i, lru_b_i, lru_lam,
              gla_w_gate, gla_b_gate, gla_norm_g, w_out, g_ffn, w_route_group, b_route_group,
              w_route_expert, b_route_expert, w_gate, w_up, w_down, g_final):
    c_act = jax.nn.silu(c)
    for l in range(DEPTH):
        mod = (c_act @ w_ada[l] + b_ada[l])[:, None, :]
        sh1, sc1, gt1, sh2, sc2, gt2 = jnp.split(mod, 6, axis=-1)
        h = rms_norm(x, g_mix[l]) * (1.0 + sc1) + sh1
        x = x + gt1 * token_mix(h, w_in[l], conv_dw_w[l], conv_dw_b[l], conv_ln_g[l], conv_ln_b[l],
                                lru_conv_w[l], lru_conv_b[l], lru_w_a[l], lru_b_a[l], lru_w_i[l],
                                lru_b_i[l], lru_lam[l], gla_w_gate[l], gla_b_gate[l], gla_norm_g[l],
                                w_out[l])
        h = rms_norm(x, g_ffn[l]) * (1.0 + sc2) + sh2
        x = x + gt2 * hier_moe(h, w_route_group[l], b_route_group[l], w_route_expert[l],
                               b_route_expert[l], w_gate[l], w_up[l], w_down[l])
    return rms_norm(x, g_final)
```

```python
import numpy as np
from contextlib import ExitStack
import concourse.bass as bass
import concourse.mybir as mybir
from concourse.bass_utils import run_bass_kernel_spmd

F32 = mybir.dt.float32
BF16 = mybir.dt.bfloat16
AF = mybir.ActivationFunctionType
ALU = mybir.AluOpType
AX = mybir.AxisListType

ENGS = ("pe", "act", "dve", "pool", "sp")
INTERLEAVE = True

D = 1024
NTOK = 2048
T = 256
NT = NTOK // T
TM = 512
NTM = NTOK // TM
INW = 2512
EPS = 1e-6
NV = 170
C_CVV, C_CVG, C_LRX, C_LRY = 0, 256, 512, 896
C_Q, C_K, C_V, C_GLR, C_OG = 1280, 1504, 1728, 2112, 2128
V_GMIX, V_GFFN, V_CDB, V_CLG, V_CLB, V_LCB, V_LBA, V_LBI, V_LAM = 0, 8, 16, 18, 20, 22, 25, 28, 31
V_GNG, V_GBG, V_BADA, V_CDW, V_LCW, V_GFIN = 34, 38, 40, 88, 150, 162
K_ID, K_ONE, K_MASK, K_SEL = 0, 128, 256, 384
NKB = 384 + 2048


def _region(ap):
    t = ap.tensor
    pstride = 1
    for s in list(t.shape)[1:]:
        pstride *= int(s)
    off = int(ap.offset)
    p0 = off // pstride
    f0 = off % pstride
    pe = 0
    fe = 0
    for step, cnt in ap.ap:
        step = int(step)
        cnt = int(cnt)
        if cnt <= 1:
            continue
        if step >= pstride and step % pstride == 0:
            pe += (cnt - 1) * (step // pstride)
        else:
            fe += (cnt - 1) * abs(step)
    return (t.name, p0, p0 + pe + 1, f0, f0 + fe + 1)


class _Op:
    __slots__ = ("eng", "fn", "deps", "idx", "need", "dma", "val")

    def __init__(self, eng, fn):
        self.eng = eng
        self.fn = fn
        self.deps = {}
        self.idx = -1
        self.need = False
        self.dma = None
        self.val = 0


class Prog:
    def __init__(self, nc):
        self.nc = nc
        self.ops = {e: [] for e in ENGS}
        self.track = {}
        self.dma_counts = {}
        self.gen_end = {}
        self.sems = {}

    def _tok(self, op):
        if op.dma is not None:
            return ("d", op.dma[0], op.dma[1])
        return ("e", op.eng, op.idx)

    def _add_dep(self, op, tok, kind):
        if tok is None:
            return
        if tok[0] == "e":
            if tok[1] == op.eng and op.dma is None:
                if tok[2] == op.idx or op.eng == "pe":
                    return
            key = ("e", tok[1])
        else:
            key = ("d", tok[1])
        if op.deps.get(key, -1) < tok[2]:
            op.deps[key] = tok[2]

    @staticmethod
    def _compress(toks):
        best = {}
        for t in toks:
            k = (t[0], t[1])
            if k not in best or best[k][2] < t[2]:
                best[k] = t
        return list(best.values())

    def _read(self, op, ap):
        name, p0, p1, f0, f1 = _region(ap)
        if name.startswith("ps"):
            return self._write(op, ap)
        ents = self.track.setdefault(name, [])
        tok = self._tok(op)
        for e in ents:
            if e[0] < p1 and p0 < e[1] and e[2] < f1 and f0 < e[3]:
                self._add_dep(op, e[4], "raw")
                e[5].append(tok)
                if len(e[5]) > 12:
                    e[5] = self._compress(e[5])

    def _write(self, op, ap):
        name, p0, p1, f0, f1 = _region(ap)
        if name.startswith("ps"):
            p0, p1, f0, f1 = 0, 128, 0, 1 << 20
        ents = self.track.setdefault(name, [])
        tok = self._tok(op)
        keep = []
        for e in ents:
            if e[0] < p1 and p0 < e[1] and e[2] < f1 and f0 < e[3]:
                self._add_dep(op, e[4], "waw")
                for r in e[5]:
                    self._add_dep(op, r, "war")
                if p0 <= e[0] and e[1] <= p1:
                    if e[2] < f0:
                        keep.append([e[0], e[1], e[2], f0, e[4], list(e[5])])
                    if f1 < e[3]:
                        keep.append([e[0], e[1], f1, e[3], e[4], list(e[5])])
                else:
                    keep.append(e)
            else:
                keep.append(e)
        keep.append([p0, p1, f0, f1, tok, []])
        self.track[name] = keep

    def op(self, eng, fn, writes=(), reads=()):
        o = _Op(eng, fn)
        o.idx = len(self.ops[eng])
        for ap in reads:
            self._read(o, ap)
        for ap in writes:
            self._write(o, ap)
        self.ops[eng].append(o)
        return o

    def dma(self, eng, out, in_, sem, out_sb=True, in_sb=False):
        o = _Op(eng, None)
        o.idx = len(self.ops[eng])
        self.dma_counts[sem] = self.dma_counts.get(sem, 0) + 16
        ends = self.gen_end.setdefault(sem, [])
        o.dma = (sem, len(ends))
        if len(ends) > 0:
            o.deps[("d", sem)] = len(ends) - 1
        o.fn = lambda e, sems: e.dma_start(out=out, in_=in_).then_inc(sems[sem], 16)
        if in_sb:
            self._read(o, in_)
        if out_sb:
            self._write(o, out)
        self.ops[eng].append(o)
        return o

    def xdma(self, eng, fn, sem, writes=(), reads=()):
        o = _Op(eng, None)
        o.idx = len(self.ops[eng])
        self.dma_counts[sem] = self.dma_counts.get(sem, 0) + 16
        ends = self.gen_end.setdefault(sem, [])
        o.dma = (sem, len(ends))
        if len(ends) > 0:
            o.deps[("d", sem)] = len(ends) - 1
        o.fn = lambda e, sems: fn(e).then_inc(sems[sem], 16)
        for ap in reads:
            self._read(o, ap)
        for ap in writes:
            self._write(o, ap)
        self.ops[eng].append(o)
        return o

    def close(self, sem):
        ends = self.gen_end.setdefault(sem, [])
        c = self.dma_counts.get(sem, 0)
        if not ends or ends[-1] != c:
            ends.append(c)

    def emit(self, stack, final_waits=()):
        nc = self.nc
        for sname in list(self.dma_counts):
            self.close(sname)
        for e in ENGS:
            for o in self.ops[e]:
                for k, v in o.deps.items():
                    if k[0] == "e":
                        self.ops[k[1]][v].need = True
        for e in ENGS:
            c = 0
            for o in self.ops[e]:
                if o.need and o.dma is None:
                    c += 1
                o.val = c
        sems = self.sems
        for e in ENGS:
            sems["e:" + e] = stack.enter_context(nc.semaphore("s_" + e))
        for s in self.dma_counts:
            sems[s] = stack.enter_context(nc.semaphore("d_" + s))
        block = stack.enter_context(nc.Block())
        prog = self

        def run(engname, eng):
            waited = {}
            for o in prog.ops[engname]:
                for k, v in o.deps.items():
                    if k[0] == "e":
                        val = prog.ops[k[1]][v].val
                        sk = "e:" + k[1]
                    else:
                        val = prog.gen_end[k[1]][v]
                        sk = k[1]
                    if waited.get(sk, 0) < val:
                        eng.wait_ge(sems[sk], val)
                        waited[sk] = val
                if o.dma is not None:
                    o.fn(eng, sems)
                else:
                    ins = o.fn(eng)
                    if o.need:
                        ins.then_inc(sems["e:" + engname], 1)
            if engname == "sp":
                for s in final_waits:
                    eng.wait_ge(sems[s], prog.dma_counts[s])

        @block.tensor
        def _(eng):
            run("pe", eng)

        @block.scalar
        def _(eng):
            run("act", eng)

        @block.vector
        def _(eng):
            run("dve", eng)

        @block.gpsimd
        def _(eng):
            run("pool", eng)

        @block.sync
        def _(eng):
            run("sp", eng)

    def mm(self, out, lhsT, rhs, start=True, stop=True, sync_prev=False):
        o = self.op("pe", lambda e: e.matmul(out, lhsT, rhs, start=start, stop=stop),
                    [out], [lhsT, rhs])
        if sync_prev and o.idx > 0:
            o.deps[("e", "pe")] = max(o.deps.get(("e", "pe"), -1), o.idx - 1)
        return o

    def transpose(self, out, in_, ident):
        return self.op("pe", lambda e: e.transpose(out, in_, ident), [out], [in_, ident])

    def act(self, out, in_, func, bias=None, scale=None):
        kw = {}
        rd = [in_]
        if bias is not None:
            kw["bias"] = bias
            if not isinstance(bias, (int, float)):
                rd.append(bias)
        if scale is not None:
            kw["scale"] = scale
            if not isinstance(scale, (int, float)):
                rd.append(scale)
        return self.op("act", lambda e: e.activation(out=out, in_=in_, func=func, **kw), [out], rd)

    def tt(self, eng, out, in0, in1, op):
        return self.op(eng, lambda e: e.tensor_tensor(out=out, in0=in0, in1=in1, op=op),
                       [out], [in0, in1])

    def ts(self, eng, out, in0, s1, op0, s2=None, op1=None):
        rd = [in0]
        if not isinstance(s1, (int, float)):
            rd.append(s1)
        if s2 is not None and not isinstance(s2, (int, float)):
            rd.append(s2)
        if op1 is None:
            return self.op(eng, lambda e: e.tensor_single_scalar(out=out, in_=in0, scalar=s1, op=op0),
                           [out], rd)
        return self.op(eng, lambda e: e.tensor_scalar(out=out, in0=in0, scalar1=s1, scalar2=s2,
                                                      op0=op0, op1=op1), [out], rd)

    def stt(self, eng, out, in0, scalar, in1, op0, op1):
        rd = [in0, in1]
        if not isinstance(scalar, (int, float)):
            rd.append(scalar)
        return self.op(eng, lambda e: e.scalar_tensor_tensor(out=out, in0=in0, scalar=scalar, in1=in1,
                                                             op0=op0, op1=op1), [out], rd)

    def copy(self, eng, out, in_):
        if eng == "act":
            return self.op(eng, lambda e: e.copy(out=out, in_=in_), [out], [in_])
        return self.op(eng, lambda e: e.tensor_copy(out=out, in_=in_), [out], [in_])

    def memset(self, eng, ap, val):
        return self.op(eng, lambda e: e.memset(ap, val), [ap], [])

    def scan(self, eng, out, d0, d1, initial, op0, op1):
        rd = [d0, d1]
        if not isinstance(initial, (int, float)):
            rd.append(initial)
        return self.op(eng, lambda e: e.tensor_tensor_scan(out=out, data0=d0, data1=d1, initial=initial,
                                                           op0=op0, op1=op1), [out], rd)

    def recip(self, eng, out, in_):
        return self.op(eng, lambda e: e.reciprocal(out=out, in_=in_), [out], [in_])

    def reduce(self, eng, out, in_, op):
        return self.op(eng, lambda e: e.tensor_reduce(out=out, in_=in_, axis=AX.X, op=op), [out], [in_])


def build_program():
    nc = bass.Bass("TRN2", target_bir_lowering=False)
    dram = {}

    def din(name, shape):
        dram[name] = nc.dram_tensor(name, shape, F32, kind="ExternalInput").ap()
        return dram[name]

    def dout(name, shape):
        dram[name] = nc.dram_tensor(name, shape, F32, kind="ExternalOutput").ap()
        return dram[name]

    n_layers = 2
    xT_ds = [din("xT1", [D, NTOK]), din("xT2", [D, NTOK])]
    cT_d = din("cT", [128, 8])
    role_d = din("role", [128, 1])
    kf_d = din("kf", [128, 512])
    kb_d = din("kb", [128, NKB])
    L = []
    for l in range(n_layers):
        L.append(dict(
            w_ada=din(f"w_ada{l}", [D, 6 * D]),
            vecs=din(f"vecs{l}", [128, NV]),
            w_in=din(f"w_in{l}", [D, INW]),
            w_out=din(f"w_out{l}", [D, D]),
            lru_w=din(f"lru_w{l}", [128, 6 * 128]),
            gla_wg=din(f"gla_wg{l}", [16, 224]),
            w_r=din(f"w_r{l}", [D, 20]),
            b_r=din(f"b_r{l}", [128, 20]),
            w_gate=din(f"w_gate{l}", [16, D, 512]),
            w_up=din(f"w_up{l}", [16, D, 512]),
            w_down=din(f"w_down{l}", [16, 512, D]),
        ))
    yN_d = dout("yN", [D, NTOK])

    with ExitStack() as st:
        def sb(name, shape, dt=F32):
            return st.enter_context(nc.sbuf_tensor("sb_" + name, shape, dt))

        P = Prog(nc)

        x = sb("x", [128, 8, NTOK])
        AB = sb("arenaB", [128, 45056], BF16)
        AFt = sb("arenaF", [128, 8320])
        cur = {"l": 0}

        class PerLayer:
            def __init__(self, name, shape, dt=F32):
                self.t = [sb(f"{name}{i}", shape, dt) for i in range(n_layers)]

            def __getitem__(self, idx):
                return self.t[cur["l"]][idx]

        vecs = PerLayer("vecs", [128, NV])
        modv = PerLayer("modv", [128, 64])
        drv = PerLayer("drv", [128, 48])
        saved = PerLayer("saved", [128, 264])
        role = sb("role", [128, 1])
        kf = sb("kf", [128, 512])
        kb = sb("kb", [128, NKB], BF16)
        cact = sb("cact", [128, 8])
        state = sb("state", [128, 264])
        b_r = PerLayer("b_r", [128, 20])

        ident = kb[:, K_ID:K_ID + 128]
        ones = kb[:, K_ONE:K_ONE + 128]
        maskb = kb[:, K_MASK:K_MASK + 128]

        def carveB(off, shape):
            n = 1
            for s in shape[1:]:
                n *= s
            v = AB[0:shape[0], off:off + n]
            if len(shape) == 3:
                v = v.rearrange("p (a b) -> p a b", b=shape[2])
            elif len(shape) == 4:
                v = v.rearrange("p (a b c) -> p a b c", b=shape[2], c=shape[3])
            return v

        def carveF(off, shape):
            n = 1
            for s in shape[1:]:
                n *= s
            v = AFt[0:shape[0], off:off + n]
            if len(shape) == 3:
                v = v.rearrange("p (a b) -> p a b", b=shape[2])
            elif len(shape) == 4:
                v = v.rearrange("p (a b c) -> p a b c", b=shape[2], c=shape[3])
            return v

        o = 0
        w_in = carveB(o, [128, 8, INW]); o += 8 * INW
        w_out = carveB(o, [128, 9, D]); o += 9 * D
        lru_w = carveB(o, [128, 6, 128]); o += 768
        gla_wg = carveB(o, [16, 224]); o += 224
        hT = carveB(o, [128, 8, T]); o += 8 * T
        xsq = carveB(o, [128, 2, T]); o += 2 * T
        xb_bf = carveB(o, [128, 3, T]); o += 3 * T
        ybf = carveB(o, [128, 2, T]); o += 2 * T
        ysq = carveB(o, [128, 2, T]); o += 2 * T
        glr_bf = carveB(o, [16, T]); o += T
        qin = carveB(o, [128, 2, T]); o += 2 * T
        kin = carveB(o, [128, 2, T]); o += 2 * T
        kout = carveB(o, [128, 2, T]); o += 2 * T
        kT = carveB(o, [128, T // 128, 224]); o += (T // 128) * 224
        v_bf = carveB(o, [128, T // 128, 384]); o += (T // 128) * 384
        scT = carveB(o, [128, T // 128, 512]); o += (T // 128) * 512
        sprev = carveB(o, [128, T // 64, 192]); o += (T // 64) * 192
        osq = carveB(o, [128, 4, T]); o += 4 * T
        mixed = carveB(o, [128, 9, T]); o += 9 * T
        dg = carveB(o, [128, 8, 128]); o += 1024
        ubuf = carveB(o, [128, 2, 30 + T]); o += 2 * (30 + T)
        assert o <= 45056, o
        o = 0
        h2 = carveB(o, [128, 8, NTOK]); o += 8 * NTOK
        wg_s = [carveB(o + i * 12288, [128, 8, 512]) for i in range(2)]
        wu_s = [carveB(o + i * 12288 + 4096, [128, 8, 512]) for i in range(2)]
        wd_s = [carveB(o + i * 12288 + 8192, [128, 4, D]) for i in range(2)]
        o += 2 * 12288
        hid = [carveB(o, [128, 4, TM]) for i in range(2)]
        xsq2 = carveB(o, [128, 2, TM])
        comb2 = carveB(o + 1024, [128, 16, 32])
        o += 2048
        combT = carveB(o, [32, NTOK]); o += NTOK
        assert o <= 45056, o
        w_r = PerLayer("w_r", [128, 8, 20], BF16)

        o = 0
        rstd = carveF(o, [128, T]); o += T
        xs = carveF(o, [128, 2, T]); o += 2 * T
        sg = carveF(o, [128, 2, T]); o += 2 * T
        yc = carveF(o, [128, 2, T]); o += 2 * T
        cmean = carveF(o, [128, T]); o += T
        crstd = carveF(o, [128, T]); o += T
        lrx = carveF(o, [128, 3, 3 + T]); o += 3 * (3 + T)
        LBs = []
        for c in range(2):
            LBs.append([carveF(o + i * T, [128, T]) for i in range(4)])
            o += 4 * T
        LB = [LBs[0], LBs[1], LBs[0]]
        T1 = carveF(o, [128, 2, T]); o += 2 * T
        T2 = carveF(o, [128, 2, T]); o += 2 * T
        HB = []
        for c in range(4):
            HB.append([carveF(o + i * T, [128, T]) for i in range(2)])
            o += 2 * T
        assert o <= 8320, o
        wada_f = carveF(0, [128, 8, 512])
        modrow = carveF(4096, [1, 512])
        wada_b = [carveB(i * 4096, [128, 8, 512]) for i in range(2)]
        o = 0
        rstd2 = carveF(o, [128, TM]); o += TM
        xs2 = carveF(o, [128, 2, TM]); o += 2 * TM
        sa = carveF(o, [128, 2, TM]); o += 2 * TM
        tu = carveF(o, [128, 2, TM]); o += 2 * TM
        cb = carveF(o, [128, 2, TM]); o += 2 * TM
        ost = carveF(o, [128, 2, TM]); o += 2 * TM
        small = carveF(o, [128, 2048]); o += 2048
        assert o <= 8320, o

        psb = [st.enter_context(nc.psum_tensor(f"ps{i}", [128, 512], F32)) for i in range(7)]
        pst = st.enter_context(nc.psum_tensor("pst", [128, 1024], BF16))
        ps_state = {"h": 0, "b": 0}

        def ps_bank():
            i = ps_state["b"]
            ps_state["b"] = (i + 1) % 7
            return psb[i][:, :]

        def ps_half():
            return ps_bank()[:, 0:256]

        P.dma("sp", kf[:], kf_d[:], "ld_c")
        P.dma("sp", role[:], role_d[:], "ld_c")
        P.dma("sp", cact[:], cT_d[:], "ld_c")
        P.dma("pool", kb[:], kb_d[:], "ld_kb")
        P.close("ld_c")
        P.close("ld_kb")
        P.memset("dve", state[:], 0.0)
        P.memset("dve", AFt[:, :], 0.0)
        P.memset("dve", AB[:, :], 0.0)
        P.act(cact[:], cact[:], AF.Silu)

        def load_mixer_weights(Ld):
            for kc in range(8):
                P.dma("pool", w_in[:, kc, :], Ld["w_in"][kc * 128:(kc + 1) * 128, :], "ld_wm")
            for j in range(5):
                P.dma("pool", w_out[:, j, :], Ld["w_out"][j * 128:(j + 1) * 128, :], "ld_wm")
            for h in range(4):
                P.dma("pool", w_out[0:96, 5 + h, :], Ld["w_out"][640 + h * 96:640 + (h + 1) * 96, :], "ld_wm")
            P.dma("pool", lru_w.rearrange("p a b -> p (a b)"), Ld["lru_w"][:, :], "ld_wm")
            P.dma("pool", gla_wg, Ld["gla_wg"][:, :], "ld_wm")
            P.close("ld_wm")

        def layer_prologue(l, Ld):
            P.dma("sp", vecs[:], Ld["vecs"][:, :], "ld_v")
            P.dma("sp", b_r[:], Ld["b_r"][:, :], "ld_v")
            for kc in range(8):
                P.dma("pool", w_r[:, kc, :], Ld["w_r"][kc * 128:(kc + 1) * 128, :], "ld_wr")
            P.close("ld_wr")
            P.close("ld_v")
            mod_ps = psb[6][:, :]
            for piece in range(12):
                c0 = piece * 512
                for kc in range(8):
                    P.dma("sp" if kc % 2 == 0 else "act", wada_f[:, kc, :],
                          Ld["w_ada"][kc * 128:(kc + 1) * 128, c0:c0 + 512], "ld_wa")
                P.close("ld_wa")
                wb = wada_b[piece % 2]
                P.copy("dve", wb, wada_f)
                prow = psb[piece % 2][:, :]
                for kc in range(8):
                    P.mm(prow[0:1, :], cact_bf[:, kc:kc + 1], wb[:, kc, :], start=(kc == 0), stop=(kc == 7))
                P.copy("act", modrow, prow[0:1, :])
                for jj in range(4):
                    j = piece * 4 + jj
                    P.mm(mod_ps[:, j:j + 1], modrow[0:1, jj * 128:(jj + 1) * 128], onef[0:1, 0:1])
            P.tt("dve", modv[:, 0:48], mod_ps[:, 0:48], vecs[:, V_BADA:V_BADA + 48], ALU.add)
            P.stt("dve", drv[:, 0:8], modv[:, 8:16], 1.0, vecs[:, V_GMIX:V_GMIX + 8], ALU.add, ALU.mult)
            P.stt("dve", drv[:, 8:16], modv[:, 32:40], 1.0, vecs[:, V_GFFN:V_GFFN + 8], ALU.add, ALU.mult)
            P.act(drv[:, 22:25], vecs[:, V_LAM:V_LAM + 3], AF.Exp, scale=-1.0)
            P.act(drv[:, 22:25], drv[:, 22:25], AF.Ln, bias=1.0)
            P.ts("dve", drv[:, 16:19], drv[:, 22:25], -8.0, ALU.mult)
            P.ts("dve", drv[:, 19:22], drv[:, 22:25], -16.0, ALU.mult)

        A1 = lambda c: drv[:, c:c + 1]
        A2 = lambda c: drv[:, 8 + c:9 + c]
        SH1 = lambda c: modv[:, c:c + 1]
        GT1 = lambda c: modv[:, 16 + c:17 + c]
        SH2 = lambda c: modv[:, 24 + c:25 + c]
        GT2 = lambda c: modv[:, 40 + c:41 + c]
        vcol = lambda j: vecs[:, j:j + 1]

        def rms_mod(t0, tw, sq_buf, rstd_buf, xs_buf, dst, Afn, Bfn):
            ms = ps_bank() if tw == 512 else ps_half()
            for c in range(8):
                P.act(sq_buf[:, c % 2, :], x[:, c, t0:t0 + tw], AF.Square)
                P.mm(ms[:, 0:tw], ones, sq_buf[:, c % 2, :], start=(c == 0), stop=(c == 7))
            P.act(rstd_buf, ms[:, 0:tw], AF.Ln, bias=vcol_eps, scale=1.0 / D)
            P.act(rstd_buf, rstd_buf, AF.Exp, scale=-0.5)
            for c in range(8):
                P.tt("dve", xs_buf[:, c % 2, :], x[:, c, t0:t0 + tw], rstd_buf, ALU.mult)
                if Bfn is None:
                    P.act(dst(c), xs_buf[:, c % 2, :], AF.Identity, scale=Afn(c))
                else:
                    P.act(dst(c), xs_buf[:, c % 2, :], AF.Identity, scale=Afn(c), bias=Bfn(c))

        epsT = sb("epsT", [128, 1])
        P.memset("dve", epsT[:], EPS)
        onef = sb("onef", [128, 1])
        P.memset("dve", onef[:], 1.0)
        cact_bf = sb("cact_bf", [128, 8], BF16)
        P.copy("dve", cact_bf[:], cact[:])
        vcol_eps = epsT[:, 0:1]

        def rsqrt_act(dst, src, bias, scale):
            P.act(dst, src, AF.Ln, bias=bias, scale=scale)
            P.act(dst, dst, AF.Exp, scale=-0.5)

        def mixer_tile(j, light=False):
            t0 = j * T
            rms_mod(t0, T, xsq, rstd, xs, lambda c: hT[:, c, :], A1, SH1)

            def zproj(c0, m, dst):
                for kc in range(8):
                    P.mm(dst, w_in[:, kc, c0:c0 + m], hT[:, kc, :], start=(kc == 0), stop=(kc == 7))

            A, B, C = [], [[], [], []], []
            do_conv = (not light) or j == NT - 1
            cst = {}

            def a1(c):
                pg = ps_half()
                zproj(C_CVG + c * 128, 128, pg)
                P.act(sg[:, c, :], pg, AF.Sigmoid)
                pv = ps_half()
                zproj(C_CVV + c * 128, 128, pv)
                P.tt("dve", ubuf[:, c, 30:30 + T], pv, sg[:, c, :], ALU.mult)

            def a2(c):
                if not light:
                    wc = V_CDW + c * 31
                    pc = ps_half()
                    for k in range(31):
                        dk = dg[:, k % 8, :]
                        P.ts("dve", dk, ident, vcol(wc + k), ALU.mult)
                        P.mm(pc, dk, ubuf[:, c, k:k + T], start=(k == 0), stop=(k == 30))
                    P.act(yc[:, c, :], pc, AF.Identity, bias=vcol(V_CDB + c))
                    P.act(ysq[:, c, :], pc, AF.Square, bias=vcol(V_CDB + c))
                    P.act(ybf[:, c, :], pc, AF.Identity, bias=vcol(V_CDB + c))
                P.copy("dve", ubuf[:, c, 0:30], ubuf[:, c, T:T + 30])

            def a3():
                pm = ps_half()
                pq = ps_half()
                for c in range(2):
                    P.mm(pm, ones, ybf[:, c, :], start=(c == 0), stop=(c == 1))
                for c in range(2):
                    P.mm(pq, ones, ysq[:, c, :], start=(c == 0), stop=(c == 1))
                P.act(cmean, pm, AF.Identity, scale=1.0 / 256)
                P.tt("dve", crstd, cmean, cmean, ALU.mult)
                P.stt("dve", crstd, pq, 1.0 / 256, crstd, ALU.mult, ALU.subtract)
                rsqrt_act(crstd, crstd, vcol_eps, 1.0)
                for c in range(2):
                    P.tt("dve", yc[:, c, :], yc[:, c, :], cmean, ALU.subtract)
                    P.tt("dve", yc[:, c, :], yc[:, c, :], crstd, ALU.mult)
                    P.act(mixed[:, c, :], yc[:, c, :], AF.Silu, scale=vcol(V_CLG + c), bias=vcol(V_CLB + c))

            if do_conv:
                A += [lambda: a1(0), lambda: a1(1), lambda: a2(0), lambda: a2(1)]
                if not light:
                    A.append(a3)

            def b1(c):
                Bx, Ba, Bi, Bm = LB[c]
                px = ps_half()
                zproj(C_LRX + c * 128, 128, px)
                P.copy("act", lrx[:, c, 3:3 + T], px)
                wl = V_LCW + c * 4
                P.ts("dve", Bx, lrx[:, c, 0:T], vcol(wl), ALU.mult, vcol(V_LCB + c), ALU.add)
                for k in range(1, 4):
                    P.stt("dve", Bx, lrx[:, c, k:k + T], vcol(wl + k), Bx, ALU.mult, ALU.add)
                P.copy("dve", lrx[:, c, 0:3], lrx[:, c, T:T + 3])
                P.copy("act", xb_bf[:, c, :], Bx)

            def b2(c):
                Bx, Ba, Bi, Bm = LB[c]
                pa = ps_half()
                P.mm(pa, lru_w[:, c, :], xb_bf[:, c, :])
                pi = ps_half()
                P.mm(pi, lru_w[:, 3 + c, :], xb_bf[:, c, :])
                P.act(Ba, pa, AF.Sigmoid, bias=vcol(V_LBA + c))
                P.act(Bi, pi, AF.Sigmoid, bias=vcol(V_LBI + c))
                P.act(Bm, Ba, AF.Exp, scale=drv[:, 19 + c:20 + c])
                P.act(Ba, Ba, AF.Exp, scale=drv[:, 16 + c:17 + c])
                P.ts("dve", Bm, Bm, -1.0, ALU.mult, 1.0, ALU.add)
                P.act(Bm, Bm, AF.Ln)
                P.act(Bm, Bm, AF.Exp, scale=0.5)
                P.tt("dve", Bi, Bi, Bx, ALU.mult)
                P.tt("dve", Bi, Bi, Bm, ALU.mult)

            def b3(c):
                Bx, Ba, Bi, Bm = LB[c]
                Bh, Bg = Bm, Bx
                P.scan("dve", Bh, Ba, Bi, state[:, c:c + 1], ALU.mult, ALU.add)
                P.copy("dve", state[:, c:c + 1], Bh[:, T - 1:T])
                if not light:
                    py = ps_half()
                    zproj(C_LRY + c * 128, 128, py)
                    P.act(Bg, py, AF.Gelu_apprx_tanh)
                    P.tt("dve", mixed[:, 2 + c, :], Bh, Bg, ALU.mult)

            for c in range(3):
                B[c] += [lambda c=c: b1(c), lambda c=c: b2(c), lambda c=c: b3(c)]

            e1v = T1[0:112, :, :].rearrange("p a (n c) -> p a n c", c=64)

            def c1():
                pgl = ps_half()
                zproj(C_GLR, 16, pgl[0:16, :])
                P.copy("act", glr_bf, pgl[0:16, :])
                for p in range(2):
                    pl = ps_half()
                    P.mm(pl[0:112, :], gla_wg[:, p * 112:(p + 1) * 112], glr_bf)
                    P.act(T1[0:112, p, :], pl[0:112, :], AF.Identity, bias=vecs[0:112, V_GBG + p:V_GBG + p + 1])
                t1 = T1[0:112, :, :]
                t2 = T2[0:112, :, :]
                P.stt("dve", t2, t1, -1.0, t1, ALU.mult, ALU.min)
                P.act(t2, t2, AF.Exp)
                P.act(t2, t2, AF.Ln, bias=1.0)
                P.ts("dve", t1, t1, 0.0, ALU.min)
                P.tt("dve", t1, t1, t2, ALU.subtract)
                t1f = T1[0:112, :, :].rearrange("p a t -> p (a t)")
                t2f = T2[0:112, :, :].rearrange("p a t -> p (a t)")
                P.scan("dve", t2f, kf[0:112, 0:2 * T], t1f, 0.0, ALU.mult, ALU.add)
                P.act(t1, t2, AF.Exp, scale=1.0 / 16)
                P.act(t2, t2, AF.Exp, scale=-1.0 / 16)

            def c2():
                t2 = T2[0:112, :, :]
                for p in range(2):
                    if not light:
                        pqq = ps_half()
                        zproj(C_Q + p * 112, 112, pqq[0:112, :])
                        P.stt("dve", qin[0:112, p, :], pqq[0:112, :], 48.0 ** -0.5, T1[0:112, p, :], ALU.mult, ALU.mult)
                    pk = ps_half()
                    zproj(C_K + p * 112, 112, pk[0:112, :])
                    P.tt("dve", T2[0:112, p, :], pk[0:112, :], T2[0:112, p, :], ALU.mult)
                if not light:
                    P.copy("act", kin[0:112, :, :], t2)
                e1last = e1v[:, :, :, 63:64].to_broadcast([112, 2, T // 64, 64])
                P.tt("dve", kout[0:112, :, :].rearrange("p a (n c) -> p a n c", c=64),
                     T2[0:112, :, :].rearrange("p a (n c) -> p a n c", c=64), e1last, ALU.mult)

            def c0():
                for g in range(T // 128):
                    pvv = ps_bank()
                    for kc in range(8):
                        P.mm(pvv[:, 0:384], hT[:, kc, g * 128:(g + 1) * 128], w_in[:, kc, C_V:C_V + 384],
                             start=(kc == 0), stop=(kc == 7))
                    P.copy("act", v_bf[:, g, :], pvv[:, 0:384])

            def c3():
                for g in range(T // 128):
                    for p in range(2):
                        P.transpose(pst[:, (g * 2 + p) * 112:(g * 2 + p + 1) * 112],
                                    kout[0:112, p, g * 128:(g + 1) * 128], ident[0:112, 0:112])
                    P.copy("act", kT[:, g, :], pst[:, g * 224:(g + 1) * 224])
                if not light:
                    for g in range(T // 128):
                        pscs = [ps_bank(), ps_bank()]
                        for h in (0, 2, 1, 3):
                            p, hp = h // 2, (h % 2) * 64
                            P.mm(pscs[h % 2][:, p * 128:(p + 1) * 128], kin[hp:hp + 48, p, g * 128:(g + 1) * 128],
                                 qin[hp:hp + 48, p, g * 128:(g + 1) * 128])
                        scv = scT[:, g, :].rearrange("p (a b c) -> p a b c", b=2, c=128)
                        for par in range(2):
                            P.tt("dve", scv[:, :, par, :], pscs[par][:, 0:256].rearrange("p (a c) -> p a c", c=128),
                                 maskb.unsqueeze(1).to_broadcast([128, 2, 128]), ALU.mult)

            def c4():
                Sv = state[0:112, 3:195].rearrange("p (a e) -> p a e", e=96)
                for n in range(T // 64):
                    g, r0 = n // 2, (n % 2) * 64
                    pkv = ps_half()
                    for h in (0, 2, 1, 3):
                        p, hp = h // 2, (h % 2) * 64
                        P.mm(pkv[hp:hp + 48, p * 96:(p + 1) * 96],
                             kT[r0:r0 + 64, g, p * 112 + hp:p * 112 + hp + 48],
                             v_bf[r0:r0 + 64, g, h * 96:(h + 1) * 96], sync_prev=(h == 1))
                    if not light:
                        P.copy("act", sprev[0:112, n, :].rearrange("p (a e) -> p a e", e=96), Sv)
                    for p in range(2):
                        P.stt("dve", Sv[:, p, :], Sv[:, p, :], e1v[:, p, n, 63:64], pkv[0:112, p * 96:(p + 1) * 96],
                              ALU.mult, ALU.add)

            def c5_mm():
                b0 = ps_state["b"]
                idxs = [(b0 + k) % 7 for k in range(4)]
                ps_state["b"] = (b0 + 4) % 7
                pos = [psb[i][:, 0:256] for i in idxs]
                cst["po"] = pos
                cst["free"] = [i for i in range(7) if i not in idxs]
                cst["fi"] = 0
                for n in range(T // 64):
                    g = n // 2
                    for h in range(4):
                        P.mm(pos[h][0:96, n * 64:(n + 1) * 64], v_bf[:, g, h * 96:(h + 1) * 96],
                             scT[:, g, h * 128 + (n % 2) * 64:h * 128 + (n % 2) * 64 + 64], start=True, stop=False)
                    for h in (0, 2, 1, 3):
                        p, hp = h // 2, (h % 2) * 64
                        P.mm(pos[h][0:96, n * 64:(n + 1) * 64], sprev[hp:hp + 48, n, p * 96:(p + 1) * 96],
                             qin[hp:hp + 48, p, n * 64:(n + 1) * 64], start=False, stop=True)

            def c5_tail():
                pos = cst["po"]

                def fbank():
                    i = cst["free"][cst["fi"] % 3]
                    cst["fi"] += 1
                    return psb[i][:, 0:256]
                for h in range(4):
                    P.act(osq[0:96, h, :], pos[h][0:96, :], AF.Square)
                for h in range(4):
                    pms = fbank()
                    P.mm(pms[0:96, :], ones[0:96, 0:96], osq[0:96, h, :])
                    P.act(HB[h][0][0:96, :], pms[0:96, :], AF.Ln, bias=epsT[0:96, 0:1], scale=1.0 / 96)
                for h in range(4):
                    P.act(HB[h][0][0:96, :], HB[h][0][0:96, :], AF.Exp, scale=-0.5)
                for h in range(4):
                    pog = fbank()
                    zproj(C_OG + h * 96, 96, pog[0:96, :])
                    P.act(HB[h][1][0:96, :], pog[0:96, :], AF.Silu)
                for h in range(4):
                    rs_h, sog_h = HB[h]
                    P.stt("dve", rs_h[0:96, :], pos[h][0:96, :], vecs[0:96, V_GNG + h:V_GNG + h + 1], rs_h[0:96, :],
                          ALU.mult, ALU.mult)
                    P.tt("dve", mixed[0:96, 5 + h, :], rs_h[0:96, :], sog_h[0:96, :], ALU.mult)

            C += [c0, c1, c2, c3, c4]
            if not light:
                def c5_all():
                    c5_mm()
                    c5_tail()
                C += [c5_all]

            Bflat = B[0] + B[1] + B[2]
            if INTERLEAVE:
                lists = [A, Bflat, C]
                pos = [0, 0, 0]
                while any(pos[i] < len(lists[i]) for i in range(3)):
                    for i in range(3):
                        if pos[i] < len(lists[i]):
                            lists[i][pos[i]]()
                            pos[i] += 1
            else:
                for f in A + Bflat + C:
                    f()
            if light:
                return
            for dc in range(8):
                pw = ps_half()
                for kc in range(9):
                    kk = 128 if kc < 5 else 96
                    P.mm(pw, w_out[0:kk, kc, dc * 128:(dc + 1) * 128], mixed[0:kk, kc, :],
                         start=(kc == 0), stop=(kc == 8))
                P.stt("dve", x[:, dc, t0:t0 + T], pw, GT1(dc), x[:, dc, t0:t0 + T], ALU.mult, ALU.add)

        def moe_layer(Ld):
            def load_expert(e):
                s = e % 2
                for kc in range(8):
                    P.dma("pool", wg_s[s][:, kc, :], Ld["w_gate"][e, kc * 128:(kc + 1) * 128, :], f"ld_e{s}")
                    P.dma("pool", wu_s[s][:, kc, :], Ld["w_up"][e, kc * 128:(kc + 1) * 128, :], f"ld_e{s}")
                for kc in range(4):
                    P.dma("pool", wd_s[s][:, kc, :], Ld["w_down"][e, kc * 128:(kc + 1) * 128, :], f"ld_e{s}")
                P.close(f"ld_e{s}")

            for tm in range(NTM):
                rms_mod(tm * TM, TM, xsq2, rstd2, xs2, lambda c, tm=tm: h2[:, c, tm * TM:(tm + 1) * TM], A2, SH2)
            load_expert(0)
            load_expert(1)
            plg = ps_bank()
            for s in range(16):
                for kc in range(8):
                    P.mm(plg[:, s * 20:(s + 1) * 20], h2[:, kc, s * 128:(s + 1) * 128], w_r[:, kc, :],
                         start=(kc == 0), stop=(kc == 7))
            o = 0

            def sm(n):
                nonlocal o
                v = small[:, o:o + n]
                o += n
                return v
            Lg = sm(320).rearrange("p (s k) -> p s k", k=20)
            P.tt("dve", Lg, plg[:, 0:320].rearrange("p (s k) -> p s k", k=20),
                 b_r[:].unsqueeze(1).to_broadcast([128, 16, 20]), ALU.add)
            gl = Lg[:, :, 0:4]
            gmax = sm(16)
            P.reduce("dve", gmax, gl, ALU.max)
            ohg = sm(64).rearrange("p (s k) -> p s k", k=4)
            P.tt("dve", ohg, gl, gmax.unsqueeze(2).to_broadcast([128, 16, 4]), ALU.is_ge)
            ex = sm(64).rearrange("p (s k) -> p s k", k=4)
            P.tt("dve", ex, gl, gmax.unsqueeze(2).to_broadcast([128, 16, 4]), ALU.subtract)
            P.act(ex, ex, AF.Exp)
            psel = sm(16)
            P.reduce("dve", psel, ex, ALU.add)
            P.recip("dve", psel, psel)
            le = Lg[:, :, 4:20].rearrange("p s (g j) -> p s g j", j=4)
            tmp4 = sm(256).rearrange("p (s g j) -> p s g j", g=4, j=4)
            P.tt("dve", tmp4, le, ohg.unsqueeze(3).to_broadcast([128, 16, 4, 4]), ALU.mult)
            el = sm(64).rearrange("p (s j) -> p s j", j=4)
            P.reduce("dve", el, tmp4.rearrange("p s g j -> p s j g"), ALU.add)
            m1 = sm(16)
            P.reduce("dve", m1, el, ALU.max)
            oh1 = sm(64).rearrange("p (s j) -> p s j", j=4)
            P.tt("dve", oh1, el, m1.unsqueeze(2).to_broadcast([128, 16, 4]), ALU.is_ge)
            el2 = sm(64).rearrange("p (s j) -> p s j", j=4)
            P.stt("dve", el2, oh1, -1e30, el, ALU.mult, ALU.add)
            m2 = sm(16)
            P.reduce("dve", m2, el2, ALU.max)
            oh2 = sm(64).rearrange("p (s j) -> p s j", j=4)
            P.tt("dve", oh2, el2, m2.unsqueeze(2).to_broadcast([128, 16, 4]), ALU.is_ge)
            dd = sm(16)
            P.tt("dve", dd, m2, m1, ALU.subtract)
            P.act(dd, dd, AF.Exp)
            w1 = sm(16)
            P.ts("dve", w1, dd, 1.0, ALU.add)
            P.recip("dve", w1, w1)
            w2 = sm(16)
            P.tt("dve", w2, dd, w1, ALU.mult)
            P.tt("dve", w1, w1, psel, ALU.mult)
            P.tt("dve", w2, w2, psel, ALU.mult)
            wj = sm(64).rearrange("p (s j) -> p s j", j=4)
            P.tt("dve", wj, oh1, w1.unsqueeze(2).to_broadcast([128, 16, 4]), ALU.mult)
            P.tt("dve", oh2, oh2, w2.unsqueeze(2).to_broadcast([128, 16, 4]), ALU.mult)
            P.tt("dve", wj, wj, oh2, ALU.add)
            comb = sm(256).rearrange("p (s g j) -> p s g j", g=4, j=4)
            P.tt("dve", comb, ohg.unsqueeze(3).to_broadcast([128, 16, 4, 4]),
                 wj.unsqueeze(2).to_broadcast([128, 16, 4, 4]), ALU.mult)
            combf = comb.rearrange("p s g j -> p s (g j)")
            P.copy("dve", comb2[:, :, 0:16], combf)
            chif = sm(256).rearrange("p (s k) -> p s k", k=16)
            P.copy("dve", chif, comb2[:, :, 0:16])
            P.tt("dve", comb2[:, :, 16:32], combf, chif, ALU.subtract)
            for half in range(2):
                for s8 in range(8):
                    s = half * 8 + s8
                    P.transpose(pst[0:32, s8 * 128:(s8 + 1) * 128], comb2[:, s, :], ident)
                P.copy("act", combT[:, half * 1024:(half + 1) * 1024], pst[0:32, 0:1024])
            for e in range(16):
                s = e % 2
                wg, wu, wd = wg_s[s], wu_s[s], wd_s[s]
                for tm in range(NTM):
                    tsl = slice(tm * TM, (tm + 1) * TM)
                    pcb = ps_bank()
                    P.mm(pcb, kb[0:32, K_SEL + e * 128:K_SEL + (e + 1) * 128], combT[:, tsl])
                    cbv = cb[:, tm % 2, :]
                    P.copy("act", cbv, pcb)
                    hd = hid[0]
                    for fc in range(4):
                        pa = ps_bank()
                        for kc in range(8):
                            P.mm(pa, wg[:, kc, fc * 128:(fc + 1) * 128], h2[:, kc, tsl], start=(kc == 0), stop=(kc == 7))
                        pu = ps_bank()
                        for kc in range(8):
                            P.mm(pu, wu[:, kc, fc * 128:(fc + 1) * 128], h2[:, kc, tsl], start=(kc == 0), stop=(kc == 7))
                        P.act(sa[:, fc % 2, :], pa, AF.Silu)
                        P.tt("dve", tu[:, fc % 2, :], pu, cbv, ALU.mult)
                        P.tt("dve", hd[:, fc, :], sa[:, fc % 2, :], tu[:, fc % 2, :], ALU.mult)
                    for dc in range(8):
                        py = ps_bank()
                        for fc in range(4):
                            P.mm(py, wd[:, fc, dc * 128:(dc + 1) * 128], hd[:, fc, :], start=(fc == 0), stop=(fc == 3))
                        P.stt("dve", x[:, dc, tsl], py, GT2(dc), x[:, dc, tsl], ALU.mult, ALU.add)
                if e + 2 < 16:
                    load_expert(e + 2)

        for phase in range(2):
            for c in range(8):
                P.dma("sp", x[:, c, :], xT_ds[phase][c * 128:(c + 1) * 128, :], "ld_x")
            P.close("ld_x")
            for l in range(n_layers):
                Ld = L[l]
                cur["l"] = l
                if phase == 0:
                    layer_prologue(l, Ld)
                load_mixer_weights(Ld)
                if phase == 0:
                    P.memset("dve", state[:], 0.0)
                else:
                    P.ts("dve", state[:], saved[:, :], role[:, 0:1], ALU.mult)
                for c in range(2):
                    P.copy("dve", ubuf[:, c, 0:30], state[:, 195 + c * 30:195 + (c + 1) * 30])
                for c in range(3):
                    P.copy("dve", lrx[:, c, 0:3], state[:, 255 + c * 3:255 + (c + 1) * 3])
                light = (phase == 0 and l == n_layers - 1)
                for j in range(NT):
                    mixer_tile(j, light=light)
                if phase == 0:
                    for c in range(2):
                        P.copy("dve", state[:, 195 + c * 30:195 + (c + 1) * 30], ubuf[:, c, 0:30])
                    for c in range(3):
                        P.copy("dve", state[:, 255 + c * 3:255 + (c + 1) * 3], lrx[:, c, 0:3])
                    P.copy("dve", saved[:, :], state[:])
                if not (phase == 0 and l == n_layers - 1):
                    moe_layer(Ld)

        yNv = yN_d.rearrange("(c p) t -> p c t", p=128)
        GF = lambda c: vecs[:, V_GFIN + c:V_GFIN + c + 1]
        for tm in range(NTM):
            tsl = slice(tm * TM, (tm + 1) * TM)
            cnt = [0]

            def dstf(c):
                return ost[:, c % 2, :]
            ms = ps_bank()
            for c in range(8):
                P.act(xsq2[:, c % 2, :], x[:, c, tsl], AF.Square)
                P.mm(ms, ones, xsq2[:, c % 2, :], start=(c == 0), stop=(c == 7))
            P.act(rstd2, ms, AF.Ln, bias=vcol_eps, scale=1.0 / D)
            P.act(rstd2, rstd2, AF.Exp, scale=-0.5)
            for c in range(8):
                P.tt("dve", xs2[:, c % 2, :], x[:, c, tsl], rstd2, ALU.mult)
                P.act(ost[:, c % 2, :], xs2[:, c % 2, :], AF.Identity, scale=GF(c))
                P.dma("sp", yNv[:, c, tsl], ost[:, c % 2, :], f"st_o{c % 2}", out_sb=False, in_sb=True)
                P.close(f"st_o{c % 2}")
        P.emit(st, final_waits=["st_o0", "st_o1"])
    return nc


def _consts():
    kf = np.ones((128, 512), np.float32)
    kf[:, 0::64] = 0.0
    kb = np.zeros((128, NKB), np.float32)
    kb[:, K_ID:K_ID + 128] = np.eye(128, dtype=np.float32)
    kb[:, K_ONE:K_ONE + 128] = 1.0
    jj = np.arange(128)[:, None]
    cc = np.arange(128)[None, :]
    kb[:, K_MASK:K_MASK + 128] = ((jj // 64 == cc // 64) & (jj <= cc)).astype(np.float32)
    for e in range(16):
        kb[e, K_SEL + e * 128:K_SEL + (e + 1) * 128] = 1.0
        kb[16 + e, K_SEL + e * 128:K_SEL + (e + 1) * 128] = 1.0
    return kf, kb


def _col(v):
    v = np.asarray(v, np.float32)
    return np.ascontiguousarray(v.reshape(-1, 128).T)


def _layer_inputs(inp, l):
    f = lambda k: np.asarray(inp[k][l], np.float32)
    vecs = np.zeros((128, NV), np.float32)
    vecs[:, V_GMIX:V_GMIX + 8] = _col(f("g_mix"))
    vecs[:, V_GFFN:V_GFFN + 8] = _col(f("g_ffn"))
    vecs[:, V_CDB:V_CDB + 2] = _col(f("conv_dw_b"))
    vecs[:, V_CLG:V_CLG + 2] = _col(f("conv_ln_g"))
    vecs[:, V_CLB:V_CLB + 2] = _col(f("conv_ln_b"))
    vecs[:, V_LCB:V_LCB + 3] = _col(f("lru_conv_b"))
    vecs[:, V_LBA:V_LBA + 3] = _col(f("lru_b_a"))
    vecs[:, V_LBI:V_LBI + 3] = _col(f("lru_b_i"))
    vecs[:, V_LAM:V_LAM + 3] = _col(f("lru_lam"))
    gng = f("gla_norm_g").reshape(4, 96)
    bg = f("gla_b_gate").reshape(4, 48)
    for h in range(4):
        vecs[0:96, V_GNG + h] = gng[h]
        vecs[(h % 2) * 64:(h % 2) * 64 + 48, V_GBG + h // 2] = bg[h]
    vecs[:, V_BADA:V_BADA + 48] = _col(f("b_ada"))
    cw = f("conv_dw_w")
    for c in range(2):
        vecs[:, V_CDW + c * 31:V_CDW + (c + 1) * 31] = cw[:, c * 128:(c + 1) * 128].T
    lw = f("lru_conv_w")
    for c in range(3):
        vecs[:, V_LCW + c * 4:V_LCW + (c + 1) * 4] = lw[:, c * 128:(c + 1) * 128].T
    vecs[:, V_GFIN:V_GFIN + 8] = _col(np.asarray(inp["g_final"], np.float32))
    w_in = f("w_in")
    wp = np.zeros((D, INW), np.float32)
    wp[:, 0:1280] = w_in[:, 0:1280]
    for h in range(4):
        p, hp = h // 2, (h % 2) * 64
        wp[:, C_Q + p * 112 + hp:C_Q + p * 112 + hp + 48] = w_in[:, 1280 + h * 48:1280 + (h + 1) * 48]
        wp[:, C_K + p * 112 + hp:C_K + p * 112 + hp + 48] = w_in[:, 1472 + h * 48:1472 + (h + 1) * 48]
    wp[:, C_V:C_V + 384] = w_in[:, 1664:2048]
    wp[:, C_GLR:C_GLR + 16] = w_in[:, 2048:2064]
    wp[:, C_OG:C_OG + 384] = w_in[:, 2064:2448]
    wgt = f("gla_w_gate")
    gwg = np.zeros((16, 224), np.float32)
    for h in range(4):
        p, hp = h // 2, (h % 2) * 64
        gwg[:, p * 112 + hp:p * 112 + hp + 48] = wgt[:, h * 48:(h + 1) * 48]
    lru_w = np.zeros((128, 6, 128), np.float32)
    wa, wi = f("lru_w_a"), f("lru_w_i")
    for c in range(3):
        for b in range(2):
            lru_w[b * 64:(b + 1) * 64, c, b * 64:(b + 1) * 64] = wa[2 * c + b]
            lru_w[b * 64:(b + 1) * 64, 3 + c, b * 64:(b + 1) * 64] = wi[2 * c + b]
    w_r = np.concatenate([f("w_route_group")] + [f("w_route_expert")[g] for g in range(4)], axis=1)
    b_r = np.concatenate([f("b_route_group"), f("b_route_expert").reshape(-1)])
    return {
        "w_ada": np.ascontiguousarray(f("w_ada")),
        "vecs": vecs,
        "w_in": wp,
        "w_out": np.ascontiguousarray(f("w_out")),
        "lru_w": np.ascontiguousarray(lru_w.reshape(128, 768)),
        "gla_wg": gwg,
        "w_r": np.ascontiguousarray(w_r),
        "b_r": np.ascontiguousarray(np.broadcast_to(b_r[None, :], (128, 20))),
        "w_gate": np.ascontiguousarray(f("w_gate").reshape(16, D, 512)),
        "w_up": np.ascontiguousarray(f("w_up").reshape(16, D, 512)),
        "w_down": np.ascontiguousarray(f("w_down").reshape(16, 512, D)),
    }


_NC_CACHE = {}


def _get_nc():
    if "nc" not in _NC_CACHE:
        _NC_CACHE["nc"] = build_program()
    return _NC_CACHE["nc"]


def make_in_maps(inputs, cores=range(8)):
    x = np.asarray(inputs["x"], np.float32)
    c = np.asarray(inputs["c"], np.float32)
    kf, kb = _consts()
    Lin = [_layer_inputs(inputs, l) for l in range(2)]
    halves = {}
    in_maps = []
    for core in cores:
        b, h = core // 2, core % 2
        for hh in (0, h):
            if (b, hh) not in halves:
                halves[(b, hh)] = np.ascontiguousarray(x[b, hh * NTOK:(hh + 1) * NTOK, :].T)
        m = {"xT1": halves[(b, 0)], "xT2": halves[(b, h)], "cT": _col(c[b]),
             "role": np.full((128, 1), float(h), np.float32), "kf": kf, "kb": kb}
        for l in range(2):
            for k, v in Lin[l].items():
                m[f"{k}{l}"] = v
        in_maps.append(m)
    return in_maps


def kernel(**inputs):
    x = np.asarray(inputs["x"], np.float32)
    out = np.empty_like(x)
    nc = _get_nc()
    in_maps = make_in_maps(inputs)
    res = run_bass_kernel_spmd(nc, in_maps, core_ids=list(range(8)))
    for core in range(8):
        b, h = core // 2, core % 2
        out[b, h * NTOK:(h + 1) * NTOK, :] = res.results[core]["yN"].T
    return out
```

```python
import numpy as np
from contextlib import ExitStack
import concourse.bass as bass
import concourse.mybir as mybir
from concourse.bass_utils import run_bass_kernel_spmd

F32 = mybir.dt.float32
BF16 = mybir.dt.bfloat16
AF = mybir.ActivationFunctionType
ALU = mybir.AluOpType
AX = mybir.AxisListType

ENGS = ("pe", "act", "dve", "pool", "sp")
INTERLEAVE = True

D = 1024
NTOK = 2048
T = 256
NT = NTOK // T
TM = 512
NTM = NTOK // TM
INW = 2512
EPS = 1e-6
NV = 170
C_CVV, C_CVG, C_LRX, C_LRY = 0, 256, 512, 896
C_Q, C_K, C_V, C_GLR, C_OG = 1280, 1504, 1728, 2112, 2128
V_GMIX, V_GFFN, V_CDB, V_CLG, V_CLB, V_LCB, V_LBA, V_LBI, V_LAM = 0, 8, 16, 18, 20, 22, 25, 28, 31
V_GNG, V_GBG, V_BADA, V_CDW, V_LCW, V_GFIN = 34, 38, 40, 88, 150, 162
K_ID, K_ONE, K_MASK, K_SEL = 0, 128, 256, 384
NKB = 384 + 2048


def _region(ap):
    t = ap.tensor
    pstride = 1
    for s in list(t.shape)[1:]:
        pstride *= int(s)
    off = int(ap.offset)
    p0 = off // pstride
    f0 = off % pstride
    pe = 0
    fe = 0
    for step, cnt in ap.ap:
        step = int(step)
        cnt = int(cnt)
        if cnt <= 1:
            continue
        if step >= pstride and step % pstride == 0:
            pe += (cnt - 1) * (step // pstride)
        else:
            fe += (cnt - 1) * abs(step)
    return (t.name, p0, p0 + pe + 1, f0, f0 + fe + 1)


class _Op:
    __slots__ = ("eng", "fn", "deps", "idx", "need", "dma", "val")

    def __init__(self, eng, fn):
        self.eng = eng
        self.fn = fn
        self.deps = {}
        self.idx = -1
        self.need = False
        self.dma = None
        self.val = 0


class Prog:
    def __init__(self, nc):
        self.nc = nc
        self.ops = {e: [] for e in ENGS}
        self.track = {}
        self.dma_counts = {}
        self.gen_end = {}
        self.sems = {}

    def _tok(self, op):
        if op.dma is not None:
            return ("d", op.dma[0], op.dma[1])
        return ("e", op.eng, op.idx)

    def _add_dep(self, op, tok, kind):
        if tok is None:
            return
        if tok[0] == "e":
            if tok[1] == op.eng and op.dma is None:
                if tok[2] == op.idx or op.eng == "pe":
                    return
            key = ("e", tok[1])
        else:
            key = ("d", tok[1])
        if op.deps.get(key, -1) < tok[2]:
            op.deps[key] = tok[2]

    @staticmethod
    def _compress(toks):
        best = {}
        for t in toks:
            k = (t[0], t[1])
            if k not in best or best[k][2] < t[2]:
                best[k] = t
        return list(best.values())

    def _read(self, op, ap):
        name, p0, p1, f0, f1 = _region(ap)
        if name.startswith("ps"):
            return self._write(op, ap)
        ents = self.track.setdefault(name, [])
        tok = self._tok(op)
        for e in ents:
            if e[0] < p1 and p0 < e[1] and e[2] < f1 and f0 < e[3]:
                self._add_dep(op, e[4], "raw")
                e[5].append(tok)
                if len(e[5]) > 12:
                    e[5] = self._compress(e[5])

    def _write(self, op, ap):
        name, p0, p1, f0, f1 = _region(ap)
        if name.startswith("ps"):
            p0, p1, f0, f1 = 0, 128, 0, 1 << 20
        ents = self.track.setdefault(name, [])
        tok = self._tok(op)
        keep = []
        for e in ents:
            if e[0] < p1 and p0 < e[1] and e[2] < f1 and f0 < e[3]:
                self._add_dep(op, e[4], "waw")
                for r in e[5]:
                    self._add_dep(op, r, "war")
                if p0 <= e[0] and e[1] <= p1:
                    if e[2] < f0:
                        keep.append([e[0], e[1], e[2], f0, e[4], list(e[5])])
                    if f1 < e[3]:
                        keep.append([e[0], e[1], f1, e[3], e[4], list(e[5])])
                else:
                    keep.append(e)
            else:
                keep.append(e)
        keep.append([p0, p1, f0, f1, tok, []])
        self.track[name] = keep

    def op(self, eng, fn, writes=(), reads=()):
        o = _Op(eng, fn)
        o.idx = len(self.ops[eng])
        for ap in reads:
            self._read(o, ap)
        for ap in writes:
            self._write(o, ap)
        self.ops[eng].append(o)
        return o

    def dma(self, eng, out, in_, sem, out_sb=True, in_sb=False):
        o = _Op(eng, None)
        o.idx = len(self.ops[eng])
        self.dma_counts[sem] = self.dma_counts.get(sem, 0) + 16
        ends = self.gen_end.setdefault(sem, [])
        o.dma = (sem, len(ends))
        if len(ends) > 0:
            o.deps[("d", sem)] = len(ends) - 1
        o.fn = lambda e, sems: e.dma_start(out=out, in_=in_).then_inc(sems[sem], 16)
        if in_sb:
            self._read(o, in_)
        if out_sb:
            self._write(o, out)
        self.ops[eng].append(o)
        return o

    def xdma(self, eng, fn, sem, writes=(), reads=()):
        o = _Op(eng, None)
        o.idx = len(self.ops[eng])
        self.dma_counts[sem] = self.dma_counts.get(sem, 0) + 16
        ends = self.gen_end.setdefault(sem, [])
        o.dma = (sem, len(ends))
        if len(ends) > 0:
            o.deps[("d", sem)] = len(ends) - 1
        o.fn = lambda e, sems: fn(e).then_inc(sems[sem], 16)
        for ap in reads:
            self._read(o, ap)
        for ap in writes:
            self._write(o, ap)
        self.ops[eng].append(o)
        return o

    def close(self, sem):
        ends = self.gen_end.setdefault(sem, [])
        c = self.dma_counts.get(sem, 0)
        if not ends or ends[-1] != c:
            ends.append(c)

    def emit(self, stack, final_waits=()):
        nc = self.nc
        for sname in list(self.dma_counts):
            self.close(sname)
        for e in ENGS:
            for o in self.ops[e]:
                for k, v in o.deps.items():
                    if k[0] == "e":
                        self.ops[k[1]][v].need = True
        for e in ENGS:
            c = 0
            for o in self.ops[e]:
                if o.need and o.dma is None:
                    c += 1
                o.val = c
        sems = self.sems
        for e in ENGS:
            sems["e:" + e] = stack.enter_context(nc.semaphore("s_" + e))
        for s in self.dma_counts:
            sems[s] = stack.enter_context(nc.semaphore("d_" + s))
        block = stack.enter_context(nc.Block())
        prog = self

        def run(engname, eng):
            waited = {}
            for o in prog.ops[engname]:
                for k, v in o.deps.items():
                    if k[0] == "e":
                        val = prog.ops[k[1]][v].val
                        sk = "e:" + k[1]
                    else:
                        val = prog.gen_end[k[1]][v]
                        sk = k[1]
                    if waited.get(sk, 0) < val:
                        eng.wait_ge(sems[sk], val)
                        waited[sk] = val
                if o.dma is not None:
                    o.fn(eng, sems)
                else:
                    ins = o.fn(eng)
                    if o.need:
                        ins.then_inc(sems["e:" + engname], 1)
            if engname == "sp":
                for s in final_waits:
                    eng.wait_ge(sems[s], prog.dma_counts[s])

        @block.tensor
        def _(eng):
            run("pe", eng)

        @block.scalar
        def _(eng):
            run("act", eng)

        @block.vector
        def _(eng):
            run("dve", eng)

        @block.gpsimd
        def _(eng):
            run("pool", eng)

        @block.sync
        def _(eng):
            run("sp", eng)

    def mm(self, out, lhsT, rhs, start=True, stop=True, sync_prev=False):
        o = self.op("pe", lambda e: e.matmul(out, lhsT, rhs, start=start, stop=stop),
                    [out], [lhsT, rhs])
        if sync_prev and o.idx > 0:
            o.deps[("e", "pe")] = max(o.deps.get(("e", "pe"), -1), o.idx - 1)
        return o

    def transpose(self, out, in_, ident):
        return self.op("pe", lambda e: e.transpose(out, in_, ident), [out], [in_, ident])

    def act(self, out, in_, func, bias=None, scale=None):
        kw = {}
        rd = [in_]
        if bias is not None:
            kw["bias"] = bias
            if not isinstance(bias, (int, float)):
                rd.append(bias)
        if scale is not None:
            kw["scale"] = scale
            if not isinstance(scale, (int, float)):
                rd.append(scale)
        return self.op("act", lambda e: e.activation(out=out, in_=in_, func=func, **kw), [out], rd)

    def tt(self, eng, out, in0, in1, op):
        return self.op(eng, lambda e: e.tensor_tensor(out=out, in0=in0, in1=in1, op=op),
                       [out], [in0, in1])

    def ts(self, eng, out, in0, s1, op0, s2=None, op1=None):
        rd = [in0]
        if not isinstance(s1, (int, float)):
            rd.append(s1)
        if s2 is not None and not isinstance(s2, (int, float)):
            rd.append(s2)
        if op1 is None:
            return self.op(eng, lambda e: e.tensor_single_scalar(out=out, in_=in0, scalar=s1, op=op0),
                           [out], rd)
        return self.op(eng, lambda e: e.tensor_scalar(out=out, in0=in0, scalar1=s1, scalar2=s2,
                                                      op0=op0, op1=op1), [out], rd)

    def stt(self, eng, out, in0, scalar, in1, op0, op1):
        rd = [in0, in1]
        if not isinstance(scalar, (int, float)):
            rd.append(scalar)
        return self.op(eng, lambda e: e.scalar_tensor_tensor(out=out, in0=in0, scalar=scalar, in1=in1,
                                                             op0=op0, op1=op1), [out], rd)

    def copy(self, eng, out, in_):
        if eng == "act":
            return self.op(eng, lambda e: e.copy(out=out, in_=in_), [out], [in_])
        return self.op(eng, lambda e: e.tensor_copy(out=out, in_=in_), [out], [in_])

    def memset(self, eng, ap, val):
        return self.op(eng, lambda e: e.memset(ap, val), [ap], [])

    def scan(self, eng, out, d0, d1, initial, op0, op1):
        rd = [d0, d1]
        if not isinstance(initial, (int, float)):
            rd.append(initial)
        return self.op(eng, lambda e: e.tensor_tensor_scan(out=out, data0=d0, data1=d1, initial=initial,
                                                           op0=op0, op1=op1), [out], rd)

    def recip(self, eng, out, in_):
        return self.op(eng, lambda e: e.reciprocal(out=out, in_=in_), [out], [in_])

    def reduce(self, eng, out, in_, op):
        return self.op(eng, lambda e: e.tensor_reduce(out=out, in_=in_, axis=AX.X, op=op), [out], [in_])


def build_program():
    nc = bass.Bass("TRN2", target_bir_lowering=False)
    dram = {}

    def din(name, shape):
        dram[name] = nc.dram_tensor(name, shape, F32, kind="ExternalInput").ap()
        return dram[name]

    def dout(name, shape):
        dram[name] = nc.dram_tensor(name, shape, F32, kind="ExternalOutput").ap()
        return dram[name]

    n_layers = 2
    xT_ds = [din("xT1", [D, NTOK]), din("xT2", [D, NTOK])]
    cT_d = din("cT", [128, 8])
    role_d = din("role", [128, 1])
    kf_d = din("kf", [128, 512])
    kb_d = din("kb", [128, NKB])
    L = []
    for l in range(n_layers):
        L.append(dict(
            w_ada=din(f"w_ada{l}", [D, 6 * D]),
            vecs=din(f"vecs{l}", [128, NV]),
            w_in=din(f"w_in{l}", [D, INW]),
            w_out=din(f"w_out{l}", [D, D]),
            lru_w=din(f"lru_w{l}", [128, 6 * 128]),
            gla_wg=din(f"gla_wg{l}", [16, 224]),
            w_r=din(f"w_r{l}", [D, 20]),
            b_r=din(f"b_r{l}", [128, 20]),
            w_gate=din(f"w_gate{l}", [16, D, 512]),
            w_up=din(f"w_up{l}", [16, D, 512]),
            w_down=din(f"w_down{l}", [16, 512, D]),
        ))
    yN_d = dout("yN", [D, NTOK])

    with ExitStack() as st:
        def sb(name, shape, dt=F32):
            return st.enter_context(nc.sbuf_tensor("sb_" + name, shape, dt))

        P = Prog(nc)

        x = sb("x", [128, 8, NTOK])
        AB = sb("arenaB", [128, 45056], BF16)
        AFt = sb("arenaF", [128, 8320])
        cur = {"l": 0}

        class PerLayer:
            def __init__(self, name, shape, dt=F32):
                self.t = [sb(f"{name}{i}", shape, dt) for i in range(n_layers)]

            def __getitem__(self, idx):
                return self.t[cur["l"]][idx]

        vecs = PerLayer("vecs", [128, NV])
        modv = PerLayer("modv", [128, 64])
        drv = PerLayer("drv", [128, 48])
        saved = PerLayer("saved", [128, 264])
        role = sb("role", [128, 1])
        kf = sb("kf", [128, 512])
        kb = sb("kb", [128, NKB], BF16)
        cact = sb("cact", [128, 8])
        state = sb("state", [128, 264])
        b_r = PerLayer("b_r", [128, 20])

        ident = kb[:, K_ID:K_ID + 128]
        ones = kb[:, K_ONE:K_ONE + 128]
        maskb = kb[:, K_MASK:K_MASK + 128]

        def carveB(off, shape):
            n = 1
            for s in shape[1:]:
                n *= s
            v = AB[0:shape[0], off:off + n]
            if len(shape) == 3:
                v = v.rearrange("p (a b) -> p a b", b=shape[2])
            elif len(shape) == 4:
                v = v.rearrange("p (a b c) -> p a b c", b=shape[2], c=shape[3])
            return v

        def carveF(off, shape):
            n = 1
            for s in shape[1:]:
                n *= s
            v = AFt[0:shape[0], off:off + n]
            if len(shape) == 3:
                v = v.rearrange("p (a b) -> p a b", b=shape[2])
            elif len(shape) == 4:
                v = v.rearrange("p (a b c) -> p a b c", b=shape[2], c=shape[3])
            return v

        o = 0
        w_in = carveB(o, [128, 8, INW]); o += 8 * INW
        w_out = carveB(o, [128, 9, D]); o += 9 * D
        lru_w = carveB(o, [128, 6, 128]); o += 768
        gla_wg = carveB(o, [16, 224]); o += 224
        hT = carveB(o, [128, 8, T]); o += 8 * T
        xsq = carveB(o, [128, 2, T]); o += 2 * T
        xb_bf = carveB(o, [128, 3, T]); o += 3 * T
        ybf = carveB(o, [128, 2, T]); o += 2 * T
        ysq = carveB(o, [128, 2, T]); o += 2 * T
        glr_bf = carveB(o, [16, T]); o += T
        qin = carveB(o, [128, 2, T]); o += 2 * T
        kin = carveB(o, [128, 2, T]); o += 2 * T
        kout = carveB(o, [128, 2, T]); o += 2 * T
        kT = carveB(o, [128, T // 128, 224]); o += (T // 128) * 224
        v_bf = carveB(o, [128, T // 128, 384]); o += (T // 128) * 384
        scT = carveB(o, [128, T // 128, 512]); o += (T // 128) * 512
        sprev = carveB(o, [128, T // 64, 192]); o += (T // 64) * 192
        osq = carveB(o, [128, 4, T]); o += 4 * T
        mixed = carveB(o, [128, 9, T]); o += 9 * T
        dg = carveB(o, [128, 8, 128]); o += 1024
        ubuf = carveB(o, [128, 2, 30 + T]); o += 2 * (30 + T)
        assert o <= 45056, o
        o = 0
        h2 = carveB(o, [128, 8, NTOK]); o += 8 * NTOK
        wg_s = [carveB(o + i * 12288, [128, 8, 512]) for i in range(2)]
        wu_s = [carveB(o + i * 12288 + 4096, [128, 8, 512]) for i in range(2)]
        wd_s = [carveB(o + i * 12288 + 8192, [128, 4, D]) for i in range(2)]
        o += 2 * 12288
        hid = [carveB(o, [128, 4, TM]) for i in range(2)]
        xsq2 = carveB(o, [128, 2, TM])
        comb2 = carveB(o + 1024, [128, 16, 32])
        o += 2048
        combT = carveB(o, [32, NTOK]); o += NTOK
        assert o <= 45056, o
        w_r = PerLayer("w_r", [128, 8, 20], BF16)

        o = 0
        rstd = carveF(o, [128, T]); o += T
        xs = carveF(o, [128, 2, T]); o += 2 * T
        sg = carveF(o, [128, 2, T]); o += 2 * T
        yc = carveF(o, [128, 2, T]); o += 2 * T
        cmean = carveF(o, [128, T]); o += T
        crstd = carveF(o, [128, T]); o += T
        lrx = carveF(o, [128, 3, 3 + T]); o += 3 * (3 + T)
        LBs = []
        for c in range(2):
            LBs.append([carveF(o + i * T, [128, T]) for i in range(4)])
            o += 4 * T
        LB = [LBs[0], LBs[1], LBs[0]]
        T1 = carveF(o, [128, 2, T]); o += 2 * T
        T2 = carveF(o, [128, 2, T]); o += 2 * T
        HB = []
        for c in range(4):
            HB.append([carveF(o + i * T, [128, T]) for i in range(2)])
            o += 2 * T
        assert o <= 8320, o
        wada_f = carveF(0, [128, 8, 512])
        modrow = carveF(4096, [1, 512])
        wada_b = [carveB(i * 4096, [128, 8, 512]) for i in range(2)]
        o = 0
        rstd2 = carveF(o, [128, TM]); o += TM
        xs2 = carveF(o, [128, 2, TM]); o += 2 * TM
        sa = carveF(o, [128, 2, TM]); o += 2 * TM
        tu = carveF(o, [128, 2, TM]); o += 2 * TM
        cb = carveF(o, [128, 2, TM]); o += 2 * TM
        ost = carveF(o, [128, 2, TM]); o += 2 * TM
        small = carveF(o, [128, 2048]); o += 2048
        assert o <= 8320, o

        psb = [st.enter_context(nc.psum_tensor(f"ps{i}", [128, 512], F32)) for i in range(7)]
        pst = st.enter_context(nc.psum_tensor("pst", [128, 1024], BF16))
        ps_state = {"h": 0, "b": 0}

        def ps_bank():
            i = ps_state["b"]
            ps_state["b"] = (i + 1) % 7
            return psb[i][:, :]

        def ps_half():
            return ps_bank()[:, 0:256]

        P.dma("sp", kf[:], kf_d[:], "ld_c")
        P.dma("sp", role[:], role_d[:], "ld_c")
        P.dma("sp", cact[:], cT_d[:], "ld_c")
        P.dma("pool", kb[:], kb_d[:], "ld_kb")
        P.close("ld_c")
        P.close("ld_kb")
        P.memset("dve", state[:], 0.0)
        P.memset("dve", AFt[:, :], 0.0)
        P.memset("dve", AB[:, :], 0.0)
        P.act(cact[:], cact[:], AF.Silu)

        def load_mixer_weights(Ld):
            for kc in range(8):
                P.dma("pool", w_in[:, kc, :], Ld["w_in"][kc * 128:(kc + 1) * 128, :], "ld_wm")
            for j in range(5):
                P.dma("pool", w_out[:, j, :], Ld["w_out"][j * 128:(j + 1) * 128, :], "ld_wm")
            for h in range(4):
                P.dma("pool", w_out[0:96, 5 + h, :], Ld["w_out"][640 + h * 96:640 + (h + 1) * 96, :], "ld_wm")
            P.dma("pool", lru_w.rearrange("p a b -> p (a b)"), Ld["lru_w"][:, :], "ld_wm")
            P.dma("pool", gla_wg, Ld["gla_wg"][:, :], "ld_wm")
            P.close("ld_wm")

        def layer_prologue(l, Ld):
            P.dma("sp", vecs[:], Ld["vecs"][:, :], "ld_v")
            P.dma("sp", b_r[:], Ld["b_r"][:, :], "ld_v")
            for kc in range(8):
                P.dma("pool", w_r[:, kc, :], Ld["w_r"][kc * 128:(kc + 1) * 128, :], "ld_wr")
            P.close("ld_wr")
            P.close("ld_v")
            mod_ps = psb[6][:, :]
            for piece in range(12):
                c0 = piece * 512
                for kc in range(8):
                    P.dma("sp" if kc % 2 == 0 else "act", wada_f[:, kc, :],
                          Ld["w_ada"][kc * 128:(kc + 1) * 128, c0:c0 + 512], "ld_wa")
                P.close("ld_wa")
                wb = wada_b[piece % 2]
                P.copy("dve", wb, wada_f)
                prow = psb[piece % 2][:, :]
                for kc in range(8):
                    P.mm(prow[0:1, :], cact_bf[:, kc:kc + 1], wb[:, kc, :], start=(kc == 0), stop=(kc == 7))
                P.copy("act", modrow, prow[0:1, :])
                for jj in range(4):
                    j = piece * 4 + jj
                    P.mm(mod_ps[:, j:j + 1], modrow[0:1, jj * 128:(jj + 1) * 128], onef[0:1, 0:1])
            P.tt("dve", modv[:, 0:48], mod_ps[:, 0:48], vecs[:, V_BADA:V_BADA + 48], ALU.add)
            P.stt("dve", drv[:, 0:8], modv[:, 8:16], 1.0, vecs[:, V_GMIX:V_GMIX + 8], ALU.add, ALU.mult)
            P.stt("dve", drv[:, 8:16], modv[:, 32:40], 1.0, vecs[:, V_GFFN:V_GFFN + 8], ALU.add, ALU.mult)
            P.act(drv[:, 22:25], vecs[:, V_LAM:V_LAM + 3], AF.Exp, scale=-1.0)
            P.act(drv[:, 22:25], drv[:, 22:25], AF.Ln, bias=1.0)
            P.ts("dve", drv[:, 16:19], drv[:, 22:25], -8.0, ALU.mult)
            P.ts("dve", drv[:, 19:22], drv[:, 22:25], -16.0, ALU.mult)

        A1 = lambda c: drv[:, c:c + 1]
        A2 = lambda c: drv[:, 8 + c:9 + c]
        SH1 = lambda c: modv[:, c:c + 1]
        GT1 = lambda c: modv[:, 16 + c:17 + c]
        SH2 = lambda c: modv[:, 24 + c:25 + c]
        GT2 = lambda c: modv[:, 40 + c:41 + c]
        vcol = lambda j: vecs[:, j:j + 1]

        def rms_mod(t0, tw, sq_buf, rstd_buf, xs_buf, dst, Afn, Bfn):
            ms = ps_bank() if tw == 512 else ps_half()
            for c in range(8):
                P.act(sq_buf[:, c % 2, :], x[:, c, t0:t0 + tw], AF.Square)
                P.mm(ms[:, 0:tw], ones, sq_buf[:, c % 2, :], start=(c == 0), stop=(c == 7))
            P.act(rstd_buf, ms[:, 0:tw], AF.Ln, bias=vcol_eps, scale=1.0 / D)
            P.act(rstd_buf, rstd_buf, AF.Exp, scale=-0.5)
            for c in range(8):
                P.tt("dve", xs_buf[:, c % 2, :], x[:, c, t0:t0 + tw], rstd_buf, ALU.mult)
                if Bfn is None:
                    P.act(dst(c), xs_buf[:, c % 2, :], AF.Identity, scale=Afn(c))
                else:
                    P.act(dst(c), xs_buf[:, c % 2, :], AF.Identity, scale=Afn(c), bias=Bfn(c))

        epsT = sb("epsT", [128, 1])
        P.memset("dve", epsT[:], EPS)
        onef = sb("onef", [128, 1])
        P.memset("dve", onef[:], 1.0)
        cact_bf = sb("cact_bf", [128, 8], BF16)
        P.copy("dve", cact_bf[:], cact[:])
        vcol_eps = epsT[:, 0:1]

        def rsqrt_act(dst, src, bias, scale):
            P.act(dst, src, AF.Ln, bias=bias, scale=scale)
            P.act(dst, dst, AF.Exp, scale=-0.5)

        def mixer_tile(j, light=False):
            t0 = j * T
            rms_mod(t0, T, xsq, rstd, xs, lambda c: hT[:, c, :], A1, SH1)

            def zproj(c0, m, dst):
                for kc in range(8):
                    P.mm(dst, w_in[:, kc, c0:c0 + m], hT[:, kc, :], start=(kc == 0), stop=(kc == 7))

            A, B, C = [], [[], [], []], []
            do_conv = (not light) or j == NT - 1
            cst = {}

            def a1(c):
                pg = ps_half()
                zproj(C_CVG + c * 128, 128, pg)
                P.act(sg[:, c, :], pg, AF.Sigmoid)
                pv = ps_half()
                zproj(C_CVV + c * 128, 128, pv)
                P.tt("dve", ubuf[:, c, 30:30 + T], pv, sg[:, c, :], ALU.mult)

            def a2(c):
                if not light:
                    wc = V_CDW + c * 31
                    pc = ps_half()
                    for k in range(31):
                        dk = dg[:, k % 8, :]
                        P.ts("dve", dk, ident, vcol(wc + k), ALU.mult)
                        P.mm(pc, dk, ubuf[:, c, k:k + T], start=(k == 0), stop=(k == 30))
                    P.act(yc[:, c, :], pc, AF.Identity, bias=vcol(V_CDB + c))
                    P.act(ysq[:, c, :], pc, AF.Square, bias=vcol(V_CDB + c))
                    P.act(ybf[:, c, :], pc, AF.Identity, bias=vcol(V_CDB + c))
                P.copy("dve", ubuf[:, c, 0:30], ubuf[:, c, T:T + 30])

            def a3():
                pm = ps_half()
                pq = ps_half()
                for c in range(2):
                    P.mm(pm, ones, ybf[:, c, :], start=(c == 0), stop=(c == 1))
                for c in range(2):
                    P.mm(pq, ones, ysq[:, c, :], start=(c == 0), stop=(c == 1))
                P.act(cmean, pm, AF.Identity, scale=1.0 / 256)
                P.tt("dve", crstd, cmean, cmean, ALU.mult)
                P.stt("dve", crstd, pq, 1.0 / 256, crstd, ALU.mult, ALU.subtract)
                rsqrt_act(crstd, crstd, vcol_eps, 1.0)
                for c in range(2):
                    P.tt("dve", yc[:, c, :], yc[:, c, :], cmean, ALU.subtract)
                    P.tt("dve", yc[:, c, :], yc[:, c, :], crstd, ALU.mult)
                    P.act(mixed[:, c, :], yc[:, c, :], AF.Silu, scale=vcol(V_CLG + c), bias=vcol(V_CLB + c))

            if do_conv:
                A += [lambda: a1(0), lambda: a1(1), lambda: a2(0), lambda: a2(1)]
                if not light:
                    A.append(a3)

            def b1(c):
                Bx, Ba, Bi, Bm = LB[c]
                px = ps_half()
                zproj(C_LRX + c * 128, 128, px)
                P.copy("act", lrx[:, c, 3:3 + T], px)
                wl = V_LCW + c * 4
                P.ts("dve", Bx, lrx[:, c, 0:T], vcol(wl), ALU.mult, vcol(V_LCB + c), ALU.add)
                for k in range(1, 4):
                    P.stt("dve", Bx, lrx[:, c, k:k + T], vcol(wl + k), Bx, ALU.mult, ALU.add)
                P.copy("dve", lrx[:, c, 0:3], lrx[:, c, T:T + 3])
                P.copy("act", xb_bf[:, c, :], Bx)

            def b2(c):
                Bx, Ba, Bi, Bm = LB[c]
                pa = ps_half()
                P.mm(pa, lru_w[:, c, :], xb_bf[:, c, :])
                pi = ps_half()
                P.mm(pi, lru_w[:, 3 + c, :], xb_bf[:, c, :])
                P.act(Ba, pa, AF.Sigmoid, bias=vcol(V_LBA + c))
                P.act(Bi, pi, AF.Sigmoid, bias=vcol(V_LBI + c))
                P.act(Bm, Ba, AF.Exp, scale=drv[:, 19 + c:20 + c])
                P.act(Ba, Ba, AF.Exp, scale=drv[:, 16 + c:17 + c])
                P.ts("dve", Bm, Bm, -1.0, ALU.mult, 1.0, ALU.add)
                P.act(Bm, Bm, AF.Ln)
                P.act(Bm, Bm, AF.Exp, scale=0.5)
                P.tt("dve", Bi, Bi, Bx, ALU.mult)
                P.tt("dve", Bi, Bi, Bm, ALU.mult)

            def b3(c):
                Bx, Ba, Bi, Bm = LB[c]
                Bh, Bg = Bm, Bx
                P.scan("dve", Bh, Ba, Bi, state[:, c:c + 1], ALU.mult, ALU.add)
                P.copy("dve", state[:, c:c + 1], Bh[:, T - 1:T])
                if not light:
                    py = ps_half()
                    zproj(C_LRY + c * 128, 128, py)
                    P.act(Bg, py, AF.Gelu_apprx_tanh)
                    P.tt("dve", mixed[:, 2 + c, :], Bh, Bg, ALU.mult)

            for c in range(3):
                B[c] += [lambda c=c: b1(c), lambda c=c: b2(c), lambda c=c: b3(c)]

            e1v = T1[0:112, :, :].rearrange("p a (n c) -> p a n c", c=64)

            def c1():
                pgl = ps_half()
                zproj(C_GLR, 16, pgl[0:16, :])
                P.copy("act", glr_bf, pgl[0:16, :])
                for p in range(2):
                    pl = ps_half()
                    P.mm(pl[0:112, :], gla_wg[:, p * 112:(p + 1) * 112], glr_bf)
                    P.act(T1[0:112, p, :], pl[0:112, :], AF.Identity, bias=vecs[0:112, V_GBG + p:V_GBG + p + 1])
                t1 = T1[0:112, :, :]
                t2 = T2[0:112, :, :]
                P.stt("dve", t2, t1, -1.0, t1, ALU.mult, ALU.min)
                P.act(t2, t2, AF.Exp)
                P.act(t2, t2, AF.Ln, bias=1.0)
                P.ts("dve", t1, t1, 0.0, ALU.min)
                P.tt("dve", t1, t1, t2, ALU.subtract)
                t1f = T1[0:112, :, :].rearrange("p a t -> p (a t)")
                t2f = T2[0:112, :, :].rearrange("p a t -> p (a t)")
                P.scan("dve", t2f, kf[0:112, 0:2 * T], t1f, 0.0, ALU.mult, ALU.add)
                P.act(t1, t2, AF.Exp, scale=1.0 / 16)
                P.act(t2, t2, AF.Exp, scale=-1.0 / 16)

            def c2():
                t2 = T2[0:112, :, :]
                for p in range(2):
                    if not light:
                        pqq = ps_half()
                        zproj(C_Q + p * 112, 112, pqq[0:112, :])
                        P.stt("dve", qin[0:112, p, :], pqq[0:112, :], 48.0 ** -0.5, T1[0:112, p, :], ALU.mult, ALU.mult)
                    pk = ps_half()
                    zproj(C_K + p * 112, 112, pk[0:112, :])
                    P.tt("dve", T2[0:112, p, :], pk[0:112, :], T2[0:112, p, :], ALU.mult)
                if not light:
                    P.copy("act", kin[0:112, :, :], t2)
                e1last = e1v[:, :, :, 63:64].to_broadcast([112, 2, T // 64, 64])
                P.tt("dve", kout[0:112, :, :].rearrange("p a (n c) -> p a n c", c=64),
                     T2[0:112, :, :].rearrange("p a (n c) -> p a n c", c=64), e1last, ALU.mult)

            def c0():
                for g in range(T // 128):
                    pvv = ps_bank()
                    for kc in range(8):
                        P.mm(pvv[:, 0:384], hT[:, kc, g * 128:(g + 1) * 128], w_in[:, kc, C_V:C_V + 384],
                             start=(kc == 0), stop=(kc == 7))
                    P.copy("act", v_bf[:, g, :], pvv[:, 0:384])

            def c3():
                for g in range(T // 128):
                    for p in range(2):
                        P.transpose(pst[:, (g * 2 + p) * 112:(g * 2 + p + 1) * 112],
                                    kout[0:112, p, g * 128:(g + 1) * 128], ident[0:112, 0:112])
                    P.copy("act", kT[:, g, :], pst[:, g * 224:(g + 1) * 224])
                if not light:
                    for g in range(T // 128):
                        pscs = [ps_bank(), ps_bank()]
                        for h in (0, 2, 1, 3):
                            p, hp = h // 2, (h % 2) * 64
                            P.mm(pscs[h % 2][:, p * 128:(p + 1) * 128], kin[hp:hp + 48, p, g * 128:(g + 1) * 128],
                                 qin[hp:hp + 48, p, g * 128:(g + 1) * 128])
                        scv = scT[:, g, :].rearrange("p (a b c) -> p a b c", b=2, c=128)
                        for par in range(2):
                            P.tt("dve", scv[:, :, par, :], pscs[par][:, 0:256].rearrange("p (a c) -> p a c", c=128),
                                 maskb.unsqueeze(1).to_broadcast([128, 2, 128]), ALU.mult)

            def c4():
                Sv = state[0:112, 3:195].rearrange("p (a e) -> p a e", e=96)
                for n in range(T // 64):
                    g, r0 = n // 2, (n % 2) * 64
                    pkv = ps_half()
                    for h in (0, 2, 1, 3):
                        p, hp = h // 2, (h % 2) * 64
                        P.mm(pkv[hp:hp + 48, p * 96:(p + 1) * 96],
                             kT[r0:r0 + 64, g, p * 112 + hp:p * 112 + hp + 48],
                             v_bf[r0:r0 + 64, g, h * 96:(h + 1) * 96], sync_prev=(h == 1))
                    if not light:
                        P.copy("act", sprev[0:112, n, :].rearrange("p (a e) -> p a e", e=96), Sv)
                    for p in range(2):
                        P.stt("dve", Sv[:, p, :], Sv[:, p, :], e1v[:, p, n, 63:64], pkv[0:112, p * 96:(p + 1) * 96],
                              ALU.mult, ALU.add)

            def c5_mm():
                b0 = ps_state["b"]
                idxs = [(b0 + k) % 7 for k in range(4)]
                ps_state["b"] = (b0 + 4) % 7
                pos = [psb[i][:, 0:256] for i in idxs]
                cst["po"] = pos
                cst["free"] = [i for i in range(7) if i not in idxs]
                cst["fi"] = 0
                for n in range(T // 64):
                    g = n // 2
                    for h in range(4):
                        P.mm(pos[h][0:96, n * 64:(n + 1) * 64], v_bf[:, g, h * 96:(h + 1) * 96],
                             scT[:, g, h * 128 + (n % 2) * 64:h * 128 + (n % 2) * 64 + 64], start=True, stop=False)
                    for h in (0, 2, 1, 3):
                        p, hp = h // 2, (h % 2) * 64
                        P.mm(pos[h][0:96, n * 64:(n + 1) * 64], sprev[hp:hp + 48, n, p * 96:(p + 1) * 96],
                             qin[hp:hp + 48, p, n * 64:(n + 1) * 64], start=False, stop=True)

            def c5_tail():
                pos = cst["po"]

                def fbank():
                    i = cst["free"][cst["fi"] % 3]
                    cst["fi"] += 1
                    return psb[i][:, 0:256]
                for h in range(4):
                    P.act(osq[0:96, h, :], pos[h][0:96, :], AF.Square)
                for h in range(4):
                    pms = fbank()
                    P.mm(pms[0:96, :], ones[0:96, 0:96], osq[0:96, h, :])
                    P.act(HB[h][0][0:96, :], pms[0:96, :], AF.Ln, bias=epsT[0:96, 0:1], scale=1.0 / 96)
                for h in range(4):
                    P.act(HB[h][0][0:96, :], HB[h][0][0:96, :], AF.Exp, scale=-0.5)
                for h in range(4):
                    rs_h, sog_h = HB[h]
                    P.stt("dve", rs_h[0:96, :], pos[h][0:96, :], vecs[0:96, V_GNG + h:V_GNG + h + 1], rs_h[0:96, :],
                          ALU.mult, ALU.mult)
                    P.tt("dve", mixed[0:96, 5 + h, :], rs_h[0:96, :], sog_h[0:96, :], ALU.mult)

            def c_og():
                for h in range(4):
                    pog = ps_half()
                    zproj(C_OG + h * 96, 96, pog[0:96, :])
                    P.act(HB[h][1][0:96, :], pog[0:96, :], AF.Silu)

            C += [c0, c1, c2, c3, c4]
            if not light:
                C.insert(1, c_og)
            if not light:
                def c5_all():
                    c5_mm()
                    c5_tail()
                C += [c5_all]

            Bflat = B[0] + B[1] + B[2]
            if INTERLEAVE:
                lists = [A, Bflat, C]
                pos = [0, 0, 0]
                while any(pos[i] < len(lists[i]) for i in range(3)):
                    for i in range(3):
                        if pos[i] < len(lists[i]):
                            lists[i][pos[i]]()
                            pos[i] += 1
            else:
                for f in A + Bflat + C:
                    f()
            if light:
                return
            for dc in range(8):
                pw = ps_half()
                for kc in range(9):
                    kk = 128 if kc < 5 else 96
                    P.mm(pw, w_out[0:kk, kc, dc * 128:(dc + 1) * 128], mixed[0:kk, kc, :],
                         start=(kc == 0), stop=(kc == 8))
                P.stt("dve", x[:, dc, t0:t0 + T], pw, GT1(dc), x[:, dc, t0:t0 + T], ALU.mult, ALU.add)

        def moe_layer(Ld):
            def load_expert(e):
                s = e % 2
                for kc in range(8):
                    P.dma("pool", wg_s[s][:, kc, :], Ld["w_gate"][e, kc * 128:(kc + 1) * 128, :], f"ld_e{s}")
                    P.dma("pool", wu_s[s][:, kc, :], Ld["w_up"][e, kc * 128:(kc + 1) * 128, :], f"ld_e{s}")
                for kc in range(4):
                    P.dma("pool", wd_s[s][:, kc, :], Ld["w_down"][e, kc * 128:(kc + 1) * 128, :], f"ld_e{s}")
                P.close(f"ld_e{s}")

            for tm in range(NTM):
                rms_mod(tm * TM, TM, xsq2, rstd2, xs2, lambda c, tm=tm: h2[:, c, tm * TM:(tm + 1) * TM], A2, SH2)
            load_expert(0)
            load_expert(1)
            plg = ps_bank()
            for s in range(16):
                for kc in range(8):
                    P.mm(plg[:, s * 20:(s + 1) * 20], h2[:, kc, s * 128:(s + 1) * 128], w_r[:, kc, :],
                         start=(kc == 0), stop=(kc == 7))
            o = 0

            def sm(n):
                nonlocal o
                v = small[:, o:o + n]
                o += n
                return v
            Lg = sm(320).rearrange("p (s k) -> p s k", k=20)
            P.tt("dve", Lg, plg[:, 0:320].rearrange("p (s k) -> p s k", k=20),
                 b_r[:].unsqueeze(1).to_broadcast([128, 16, 20]), ALU.add)
            gl = Lg[:, :, 0:4]
            gmax = sm(16)
            P.reduce("dve", gmax, gl, ALU.max)
            ohg = sm(64).rearrange("p (s k) -> p s k", k=4)
            P.tt("dve", ohg, gl, gmax.unsqueeze(2).to_broadcast([128, 16, 4]), ALU.is_ge)
            ex = sm(64).rearrange("p (s k) -> p s k", k=4)
            P.tt("dve", ex, gl, gmax.unsqueeze(2).to_broadcast([128, 16, 4]), ALU.subtract)
            P.act(ex, ex, AF.Exp)
            psel = sm(16)
            P.reduce("dve", psel, ex, ALU.add)
            P.recip("dve", psel, psel)
            le = Lg[:, :, 4:20].rearrange("p s (g j) -> p s g j", j=4)
            tmp4 = sm(256).rearrange("p (s g j) -> p s g j", g=4, j=4)
            P.tt("dve", tmp4, le, ohg.unsqueeze(3).to_broadcast([128, 16, 4, 4]), ALU.mult)
            el = sm(64).rearrange("p (s j) -> p s j", j=4)
            P.reduce("dve", el, tmp4.rearrange("p s g j -> p s j g"), ALU.add)
            m1 = sm(16)
            P.reduce("dve", m1, el, ALU.max)
            oh1 = sm(64).rearrange("p (s j) -> p s j", j=4)
            P.tt("dve", oh1, el, m1.unsqueeze(2).to_broadcast([128, 16, 4]), ALU.is_ge)
            el2 = sm(64).rearrange("p (s j) -> p s j", j=4)
            P.stt("dve", el2, oh1, -1e30, el, ALU.mult, ALU.add)
            m2 = sm(16)
            P.reduce("dve", m2, el2, ALU.max)
            oh2 = sm(64).rearrange("p (s j) -> p s j", j=4)
            P.tt("dve", oh2, el2, m2.unsqueeze(2).to_broadcast([128, 16, 4]), ALU.is_ge)
            dd = sm(16)
            P.tt("dve", dd, m2, m1, ALU.subtract)
            P.act(dd, dd, AF.Exp)
            w1 = sm(16)
            P.ts("dve", w1, dd, 1.0, ALU.add)
            P.recip("dve", w1, w1)
            w2 = sm(16)
            P.tt("dve", w2, dd, w1, ALU.mult)
            P.tt("dve", w1, w1, psel, ALU.mult)
            P.tt("dve", w2, w2, psel, ALU.mult)
            wj = sm(64).rearrange("p (s j) -> p s j", j=4)
            P.tt("dve", wj, oh1, w1.unsqueeze(2).to_broadcast([128, 16, 4]), ALU.mult)
            P.tt("dve", oh2, oh2, w2.unsqueeze(2).to_broadcast([128, 16, 4]), ALU.mult)
            P.tt("dve", wj, wj, oh2, ALU.add)
            comb = sm(256).rearrange("p (s g j) -> p s g j", g=4, j=4)
            P.tt("dve", comb, ohg.unsqueeze(3).to_broadcast([128, 16, 4, 4]),
                 wj.unsqueeze(2).to_broadcast([128, 16, 4, 4]), ALU.mult)
            combf = comb.rearrange("p s g j -> p s (g j)")
            P.copy("dve", comb2[:, :, 0:16], combf)
            chif = sm(256).rearrange("p (s k) -> p s k", k=16)
            P.copy("dve", chif, comb2[:, :, 0:16])
            P.tt("dve", comb2[:, :, 16:32], combf, chif, ALU.subtract)
            for half in range(2):
                for s8 in range(8):
                    s = half * 8 + s8
                    P.transpose(pst[0:32, s8 * 128:(s8 + 1) * 128], comb2[:, s, :], ident)
                P.copy("act", combT[:, half * 1024:(half + 1) * 1024], pst[0:32, 0:1024])
            for e in range(16):
                s = e % 2
                wg, wu, wd = wg_s[s], wu_s[s], wd_s[s]
                for tm in range(NTM):
                    tsl = slice(tm * TM, (tm + 1) * TM)
                    pcb = ps_bank()
                    P.mm(pcb, kb[0:32, K_SEL + e * 128:K_SEL + (e + 1) * 128], combT[:, tsl])
                    cbv = cb[:, tm % 2, :]
                    P.copy("act", cbv, pcb)
                    hd = hid[0]
                    for fc in range(4):
                        pa = ps_bank()
                        for kc in range(8):
                            P.mm(pa, wg[:, kc, fc * 128:(fc + 1) * 128], h2[:, kc, tsl], start=(kc == 0), stop=(kc == 7))
                        pu = ps_bank()
                        for kc in range(8):
                            P.mm(pu, wu[:, kc, fc * 128:(fc + 1) * 128], h2[:, kc, tsl], start=(kc == 0), stop=(kc == 7))
                        P.act(sa[:, fc % 2, :], pa, AF.Silu)
                        P.tt("dve", tu[:, fc % 2, :], pu, cbv, ALU.mult)
                        P.tt("dve", hd[:, fc, :], sa[:, fc % 2, :], tu[:, fc % 2, :], ALU.mult)
                    for dc in range(8):
                        py = ps_bank()
                        for fc in range(4):
                            P.mm(py, wd[:, fc, dc * 128:(dc + 1) * 128], hd[:, fc, :], start=(fc == 0), stop=(fc == 3))
                        P.stt("dve", x[:, dc, tsl], py, GT2(dc), x[:, dc, tsl], ALU.mult, ALU.add)
                if e + 2 < 16:
                    load_expert(e + 2)

        for phase in range(2):
            for c in range(8):
                P.dma("sp", x[:, c, :], xT_ds[phase][c * 128:(c + 1) * 128, :], "ld_x")
            P.close("ld_x")
            for l in range(n_layers):
                Ld = L[l]
                cur["l"] = l
                if phase == 0:
                    layer_prologue(l, Ld)
                load_mixer_weights(Ld)
                if phase == 0:
                    P.memset("dve", state[:], 0.0)
                else:
                    P.ts("dve", state[:], saved[:, :], role[:, 0:1], ALU.mult)
                for c in range(2):
                    P.copy("dve", ubuf[:, c, 0:30], state[:, 195 + c * 30:195 + (c + 1) * 30])
                for c in range(3):
                    P.copy("dve", lrx[:, c, 0:3], state[:, 255 + c * 3:255 + (c + 1) * 3])
                light = (phase == 0 and l == n_layers - 1)
                for j in range(NT):
                    mixer_tile(j, light=light)
                if phase == 0:
                    for c in range(2):
                        P.copy("dve", state[:, 195 + c * 30:195 + (c + 1) * 30], ubuf[:, c, 0:30])
                    for c in range(3):
                        P.copy("dve", state[:, 255 + c * 3:255 + (c + 1) * 3], lrx[:, c, 0:3])
                    P.copy("dve", saved[:, :], state[:])
                if not (phase == 0 and l == n_layers - 1):
                    moe_layer(Ld)

        yNv = yN_d.rearrange("(c p) t -> p c t", p=128)
        GF = lambda c: vecs[:, V_GFIN + c:V_GFIN + c + 1]
        for tm in range(NTM):
            tsl = slice(tm * TM, (tm + 1) * TM)
            cnt = [0]

            def dstf(c):
                return ost[:, c % 2, :]
            ms = ps_bank()
            for c in range(8):
                P.act(xsq2[:, c % 2, :], x[:, c, tsl], AF.Square)
                P.mm(ms, ones, xsq2[:, c % 2, :], start=(c == 0), stop=(c == 7))
            P.act(rstd2, ms, AF.Ln, bias=vcol_eps, scale=1.0 / D)
            P.act(rstd2, rstd2, AF.Exp, scale=-0.5)
            for c in range(8):
                P.tt("dve", xs2[:, c % 2, :], x[:, c, tsl], rstd2, ALU.mult)
                P.act(ost[:, c % 2, :], xs2[:, c % 2, :], AF.Identity, scale=GF(c))
                P.dma("sp", yNv[:, c, tsl], ost[:, c % 2, :], f"st_o{c % 2}", out_sb=False, in_sb=True)
                P.close(f"st_o{c % 2}")
        P.emit(st, final_waits=["st_o0", "st_o1"])
    return nc


def _consts():
    kf = np.ones((128, 512), np.float32)
    kf[:, 0::64] = 0.0
    kb = np.zeros((128, NKB), np.float32)
    kb[:, K_ID:K_ID + 128] = np.eye(128, dtype=np.float32)
    kb[:, K_ONE:K_ONE + 128] = 1.0
    jj = np.arange(128)[:, None]
    cc = np.arange(128)[None, :]
    kb[:, K_MASK:K_MASK + 128] = ((jj // 64 == cc // 64) & (jj <= cc)).astype(np.float32)
    for e in range(16):
        kb[e, K_SEL + e * 128:K_SEL + (e + 1) * 128] = 1.0
        kb[16 + e, K_SEL + e * 128:K_SEL + (e + 1) * 128] = 1.0
    return kf, kb


def _col(v):
    v = np.asarray(v, np.float32)
    return np.ascontiguousarray(v.reshape(-1, 128).T)


def _layer_inputs(inp, l):
    f = lambda k: np.asarray(inp[k][l], np.float32)
    vecs = np.zeros((128, NV), np.float32)
    vecs[:, V_GMIX:V_GMIX + 8] = _col(f("g_mix"))
    vecs[:, V_GFFN:V_GFFN + 8] = _col(f("g_ffn"))
    vecs[:, V_CDB:V_CDB + 2] = _col(f("conv_dw_b"))
    vecs[:, V_CLG:V_CLG + 2] = _col(f("conv_ln_g"))
    vecs[:, V_CLB:V_CLB + 2] = _col(f("conv_ln_b"))
    vecs[:, V_LCB:V_LCB + 3] = _col(f("lru_conv_b"))
    vecs[:, V_LBA:V_LBA + 3] = _col(f("lru_b_a"))
    vecs[:, V_LBI:V_LBI + 3] = _col(f("lru_b_i"))
    vecs[:, V_LAM:V_LAM + 3] = _col(f("lru_lam"))
    gng = f("gla_norm_g").reshape(4, 96)
    bg = f("gla_b_gate").reshape(4, 48)
    for h in range(4):
        vecs[0:96, V_GNG + h] = gng[h]
        vecs[(h % 2) * 64:(h % 2) * 64 + 48, V_GBG + h // 2] = bg[h]
    vecs[:, V_BADA:V_BADA + 48] = _col(f("b_ada"))
    cw = f("conv_dw_w")
    for c in range(2):
        vecs[:, V_CDW + c * 31:V_CDW + (c + 1) * 31] = cw[:, c * 128:(c + 1) * 128].T
    lw = f("lru_conv_w")
    for c in range(3):
        vecs[:, V_LCW + c * 4:V_LCW + (c + 1) * 4] = lw[:, c * 128:(c + 1) * 128].T
    vecs[:, V_GFIN:V_GFIN + 8] = _col(np.asarray(inp["g_final"], np.float32))
    w_in = f("w_in")
    wp = np.zeros((D, INW), np.float32)
    wp[:, 0:1280] = w_in[:, 0:1280]
    for h in range(4):
        p, hp = h // 2, (h % 2) * 64
        wp[:, C_Q + p * 112 + hp:C_Q + p * 112 + hp + 48] = w_in[:, 1280 + h * 48:1280 + (h + 1) * 48]
        wp[:, C_K + p * 112 + hp:C_K + p * 112 + hp + 48] = w_in[:, 1472 + h * 48:1472 + (h + 1) * 48]
    wp[:, C_V:C_V + 384] = w_in[:, 1664:2048]
    wp[:, C_GLR:C_GLR + 16] = w_in[:, 2048:2064]
    wp[:, C_OG:C_OG + 384] = w_in[:, 2064:2448]
    wgt = f("gla_w_gate")
    gwg = np.zeros((16, 224), np.float32)
    for h in range(4):
        p, hp = h // 2, (h % 2) * 64
        gwg[:, p * 112 + hp:p * 112 + hp + 48] = wgt[:, h * 48:(h + 1) * 48]
    lru_w = np.zeros((128, 6, 128), np.float32)
    wa, wi = f("lru_w_a"), f("lru_w_i")
    for c in range(3):
        for b in range(2):
            lru_w[b * 64:(b + 1) * 64, c, b * 64:(b + 1) * 64] = wa[2 * c + b]
            lru_w[b * 64:(b + 1) * 64, 3 + c, b * 64:(b + 1) * 64] = wi[2 * c + b]
    w_r = np.concatenate([f("w_route_group")] + [f("w_route_expert")[g] for g in range(4)], axis=1)
    b_r = np.concatenate([f("b_route_group"), f("b_route_expert").reshape(-1)])
    return {
        "w_ada": np.ascontiguousarray(f("w_ada")),
        "vecs": vecs,
        "w_in": wp,
        "w_out": np.ascontiguousarray(f("w_out")),
        "lru_w": np.ascontiguousarray(lru_w.reshape(128, 768)),
        "gla_wg": gwg,
        "w_r": np.ascontiguousarray(w_r),
        "b_r": np.ascontiguousarray(np.broadcast_to(b_r[None, :], (128, 20))),
        "w_gate": np.ascontiguousarray(f("w_gate").reshape(16, D, 512)),
        "w_up": np.ascontiguousarray(f("w_up").reshape(16, D, 512)),
        "w_down": np.ascontiguousarray(f("w_down").reshape(16, 512, D)),
    }


_NC_CACHE = {}


def _get_nc():
    if "nc" not in _NC_CACHE:
        _NC_CACHE["nc"] = build_program()
    return _NC_CACHE["nc"]


def make_in_maps(inputs, cores=range(8)):
    x = np.asarray(inputs["x"], np.float32)
    c = np.asarray(inputs["c"], np.float32)
    kf, kb = _consts()
    Lin = [_layer_inputs(inputs, l) for l in range(2)]
    halves = {}
    in_maps = []
    for core in cores:
        b, h = core // 2, core % 2
        for hh in (0, h):
            if (b, hh) not in halves:
                halves[(b, hh)] = np.ascontiguousarray(x[b, hh * NTOK:(hh + 1) * NTOK, :].T)
        m = {"xT1": halves[(b, 0)], "xT2": halves[(b, h)], "cT": _col(c[b]),
             "role": np.full((128, 1), float(h), np.float32), "kf": kf, "kb": kb}
        for l in range(2):
            for k, v in Lin[l].items():
                m[f"{k}{l}"] = v
        in_maps.append(m)
    return in_maps


def kernel(**inputs):
    x = np.asarray(inputs["x"], np.float32)
    out = np.empty_like(x)
    nc = _get_nc()
    in_maps = make_in_maps(inputs)
    res = run_bass_kernel_spmd(nc, in_maps, core_ids=list(range(8)))
    for core in range(8):
        b, h = core // 2, core % 2
        out[b, h * NTOK:(h + 1) * NTOK, :] = res.results[core]["yN"].T
    return out
```

```python
import numpy as np
from contextlib import ExitStack
import concourse.bass as bass
import concourse.mybir as mybir
from concourse.bass_utils import run_bass_kernel_spmd

F32 = mybir.dt.float32
BF16 = mybir.dt.bfloat16
AF = mybir.ActivationFunctionType
ALU = mybir.AluOpType
AX = mybir.AxisListType

ENGS = ("pe", "act", "dve", "pool", "sp")
INTERLEAVE = True

D = 1024
NTOK = 2048
T = 256
NT = NTOK // T
TM = 512
NTM = NTOK // TM
INW = 2512
EPS = 1e-6
NV = 170
C_CVV, C_CVG, C_LRX, C_LRY = 0, 256, 512, 896
C_Q, C_K, C_V, C_GLR, C_OG = 1280, 1504, 1728, 2112, 2128
V_GMIX, V_GFFN, V_CDB, V_CLG, V_CLB, V_LCB, V_LBA, V_LBI, V_LAM = 0, 8, 16, 18, 20, 22, 25, 28, 31
V_GNG, V_GBG, V_BADA, V_CDW, V_LCW, V_GFIN = 34, 38, 40, 88, 150, 162
K_ID, K_ONE, K_MASK, K_SEL = 0, 128, 256, 384
NKB = 384 + 2048


def _region(ap):
    t = ap.tensor
    pstride = 1
    for s in list(t.shape)[1:]:
        pstride *= int(s)
    off = int(ap.offset)
    p0 = off // pstride
    f0 = off % pstride
    pe = 0
    fe = 0
    for step, cnt in ap.ap:
        step = int(step)
        cnt = int(cnt)
        if cnt <= 1:
            continue
        if step >= pstride and step % pstride == 0:
            pe += (cnt - 1) * (step // pstride)
        else:
            fe += (cnt - 1) * abs(step)
    return (t.name, p0, p0 + pe + 1, f0, f0 + fe + 1)


class _Op:
    __slots__ = ("eng", "fn", "deps", "idx", "need", "dma", "val")

    def __init__(self, eng, fn):
        self.eng = eng
        self.fn = fn
        self.deps = {}
        self.idx = -1
        self.need = False
        self.dma = None
        self.val = 0


class Prog:
    def __init__(self, nc):
        self.nc = nc
        self.ops = {e: [] for e in ENGS}
        self.track = {}
        self.dma_counts = {}
        self.gen_end = {}
        self.sems = {}

    def _tok(self, op):
        if op.dma is not None:
            return ("d", op.dma[0], op.dma[1])
        return ("e", op.eng, op.idx)

    def _add_dep(self, op, tok, kind):
        if tok is None:
            return
        if tok[0] == "e":
            if tok[1] == op.eng and op.dma is None:
                if tok[2] == op.idx or op.eng == "pe":
                    return
            key = ("e", tok[1])
        else:
            key = ("d", tok[1])
        if op.deps.get(key, -1) < tok[2]:
            op.deps[key] = tok[2]

    @staticmethod
    def _compress(toks):
        best = {}
        for t in toks:
            k = (t[0], t[1])
            if k not in best or best[k][2] < t[2]:
                best[k] = t
        return list(best.values())

    def _read(self, op, ap):
        name, p0, p1, f0, f1 = _region(ap)
        if name.startswith("ps"):
            return self._write(op, ap)
        ents = self.track.setdefault(name, [])
        tok = self._tok(op)
        for e in ents:
            if e[0] < p1 and p0 < e[1] and e[2] < f1 and f0 < e[3]:
                self._add_dep(op, e[4], "raw")
                e[5].append(tok)
                if len(e[5]) > 12:
                    e[5] = self._compress(e[5])

    def _write(self, op, ap):
        name, p0, p1, f0, f1 = _region(ap)
        if name.startswith("ps"):
            p0, p1, f0, f1 = 0, 128, 0, 1 << 20
        ents = self.track.setdefault(name, [])
        tok = self._tok(op)
        keep = []
        for e in ents:
            if e[0] < p1 and p0 < e[1] and e[2] < f1 and f0 < e[3]:
                self._add_dep(op, e[4], "waw")
                for r in e[5]:
                    self._add_dep(op, r, "war")
                if p0 <= e[0] and e[1] <= p1:
                    if e[2] < f0:
                        keep.append([e[0], e[1], e[2], f0, e[4], list(e[5])])
                    if f1 < e[3]:
                        keep.append([e[0], e[1], f1, e[3], e[4], list(e[5])])
                else:
                    keep.append(e)
            else:
                keep.append(e)
        keep.append([p0, p1, f0, f1, tok, []])
        self.track[name] = keep

    def op(self, eng, fn, writes=(), reads=()):
        o = _Op(eng, fn)
        o.idx = len(self.ops[eng])
        for ap in reads:
            self._read(o, ap)
        for ap in writes:
            self._write(o, ap)
        self.ops[eng].append(o)
        return o

    def dma(self, eng, out, in_, sem, out_sb=True, in_sb=False):
        o = _Op(eng, None)
        o.idx = len(self.ops[eng])
        self.dma_counts[sem] = self.dma_counts.get(sem, 0) + 16
        ends = self.gen_end.setdefault(sem, [])
        o.dma = (sem, len(ends))
        if len(ends) > 0:
            o.deps[("d", sem)] = len(ends) - 1
        o.fn = lambda e, sems: e.dma_start(out=out, in_=in_).then_inc(sems[sem], 16)
        if in_sb:
            self._read(o, in_)
        if out_sb:
            self._write(o, out)
        self.ops[eng].append(o)
        return o

    def xdma(self, eng, fn, sem, writes=(), reads=()):
        o = _Op(eng, None)
        o.idx = len(self.ops[eng])
        self.dma_counts[sem] = self.dma_counts.get(sem, 0) + 16
        ends = self.gen_end.setdefault(sem, [])
        o.dma = (sem, len(ends))
        if len(ends) > 0:
            o.deps[("d", sem)] = len(ends) - 1
        o.fn = lambda e, sems: fn(e).then_inc(sems[sem], 16)
        for ap in reads:
            self._read(o, ap)
        for ap in writes:
            self._write(o, ap)
        self.ops[eng].append(o)
        return o

    def close(self, sem):
        ends = self.gen_end.setdefault(sem, [])
        c = self.dma_counts.get(sem, 0)
        if not ends or ends[-1] != c:
            ends.append(c)

    def emit(self, stack, final_waits=()):
        nc = self.nc
        for sname in list(self.dma_counts):
            self.close(sname)
        for e in ENGS:
            for o in self.ops[e]:
                for k, v in o.deps.items():
                    if k[0] == "e":
                        self.ops[k[1]][v].need = True
        for e in ENGS:
            c = 0
            for o in self.ops[e]:
                if o.need and o.dma is None:
                    c += 1
                o.val = c
        sems = self.sems
        for e in ENGS:
            sems["e:" + e] = stack.enter_context(nc.semaphore("s_" + e))
        for s in self.dma_counts:
            sems[s] = stack.enter_context(nc.semaphore("d_" + s))
        block = stack.enter_context(nc.Block())
        prog = self

        def run(engname, eng):
            waited = {}
            for o in prog.ops[engname]:
                for k, v in o.deps.items():
                    if k[0] == "e":
                        val = prog.ops[k[1]][v].val
                        sk = "e:" + k[1]
                    else:
                        val = prog.gen_end[k[1]][v]
                        sk = k[1]
                    if waited.get(sk, 0) < val:
                        eng.wait_ge(sems[sk], val)
                        waited[sk] = val
                if o.dma is not None:
                    o.fn(eng, sems)
                else:
                    ins = o.fn(eng)
                    if o.need:
                        ins.then_inc(sems["e:" + engname], 1)
            if engname == "sp":
                for s in final_waits:
                    eng.wait_ge(sems[s], prog.dma_counts[s])

        @block.tensor
        def _(eng):
            run("pe", eng)

        @block.scalar
        def _(eng):
            run("act", eng)

        @block.vector
        def _(eng):
            run("dve", eng)

        @block.gpsimd
        def _(eng):
            run("pool", eng)

        @block.sync
        def _(eng):
            run("sp", eng)

    def mm(self, out, lhsT, rhs, start=True, stop=True, sync_prev=False):
        o = self.op("pe", lambda e: e.matmul(out, lhsT, rhs, start=start, stop=stop),
                    [out], [lhsT, rhs])
        if sync_prev and o.idx > 0:
            o.deps[("e", "pe")] = max(o.deps.get(("e", "pe"), -1), o.idx - 1)
        return o

    def transpose(self, out, in_, ident):
        return self.op("pe", lambda e: e.transpose(out, in_, ident), [out], [in_, ident])

    def act(self, out, in_, func, bias=None, scale=None):
        kw = {}
        rd = [in_]
        if bias is not None:
            kw["bias"] = bias
            if not isinstance(bias, (int, float)):
                rd.append(bias)
        if scale is not None:
            kw["scale"] = scale
            if not isinstance(scale, (int, float)):
                rd.append(scale)
        return self.op("act", lambda e: e.activation(out=out, in_=in_, func=func, **kw), [out], rd)

    def tt(self, eng, out, in0, in1, op):
        return self.op(eng, lambda e: e.tensor_tensor(out=out, in0=in0, in1=in1, op=op),
                       [out], [in0, in1])

    def ts(self, eng, out, in0, s1, op0, s2=None, op1=None):
        rd = [in0]
        if not isinstance(s1, (int, float)):
            rd.append(s1)
        if s2 is not None and not isinstance(s2, (int, float)):
            rd.append(s2)
        if op1 is None:
            return self.op(eng, lambda e: e.tensor_single_scalar(out=out, in_=in0, scalar=s1, op=op0),
                           [out], rd)
        return self.op(eng, lambda e: e.tensor_scalar(out=out, in0=in0, scalar1=s1, scalar2=s2,
                                                      op0=op0, op1=op1), [out], rd)

    def stt(self, eng, out, in0, scalar, in1, op0, op1):
        rd = [in0, in1]
        if not isinstance(scalar, (int, float)):
            rd.append(scalar)
        return self.op(eng, lambda e: e.scalar_tensor_tensor(out=out, in0=in0, scalar=scalar, in1=in1,
                                                             op0=op0, op1=op1), [out], rd)

    def copy(self, eng, out, in_):
        if eng == "act":
            return self.op(eng, lambda e: e.copy(out=out, in_=in_), [out], [in_])
        return self.op(eng, lambda e: e.tensor_copy(out=out, in_=in_), [out], [in_])

    def memset(self, eng, ap, val):
        return self.op(eng, lambda e: e.memset(ap, val), [ap], [])

    def scan(self, eng, out, d0, d1, initial, op0, op1):
        rd = [d0, d1]
        if not isinstance(initial, (int, float)):
            rd.append(initial)
        return self.op(eng, lambda e: e.tensor_tensor_scan(out=out, data0=d0, data1=d1, initial=initial,
                                                           op0=op0, op1=op1), [out], rd)

    def recip(self, eng, out, in_):
        return self.op(eng, lambda e: e.reciprocal(out=out, in_=in_), [out], [in_])

    def reduce(self, eng, out, in_, op):
        return self.op(eng, lambda e: e.tensor_reduce(out=out, in_=in_, axis=AX.X, op=op), [out], [in_])


def build_program():
    nc = bass.Bass("TRN2", target_bir_lowering=False)
    dram = {}

    def din(name, shape):
        dram[name] = nc.dram_tensor(name, shape, F32, kind="ExternalInput").ap()
        return dram[name]

    def dout(name, shape):
        dram[name] = nc.dram_tensor(name, shape, F32, kind="ExternalOutput").ap()
        return dram[name]

    n_layers = 2
    xT_ds = [din("xT1", [D, NTOK]), din("xT2", [D, NTOK])]
    cT_d = din("cT", [128, 8])
    role_d = din("role", [128, 1])
    kf_d = din("kf", [128, 512])
    kb_d = din("kb", [128, NKB])
    L = []
    for l in range(n_layers):
        L.append(dict(
            w_ada=din(f"w_ada{l}", [D, 6 * D]),
            vecs=din(f"vecs{l}", [128, NV]),
            w_in=din(f"w_in{l}", [D, INW]),
            w_out=din(f"w_out{l}", [D, D]),
            lru_w=din(f"lru_w{l}", [128, 6 * 128]),
            gla_wg=din(f"gla_wg{l}", [16, 224]),
            w_r=din(f"w_r{l}", [D, 20]),
            b_r=din(f"b_r{l}", [128, 20]),
            w_gate=din(f"w_gate{l}", [16, D, 512]),
            w_up=din(f"w_up{l}", [16, D, 512]),
            w_down=din(f"w_down{l}", [16, 512, D]),
        ))
    yN_d = dout("yN", [D, NTOK])

    with ExitStack() as st:
        def sb(name, shape, dt=F32):
            return st.enter_context(nc.sbuf_tensor("sb_" + name, shape, dt))

        P = Prog(nc)

        x = sb("x", [128, 8, NTOK])
        AB = sb("arenaB", [128, 45056], BF16)
        AFt = sb("arenaF", [128, 8320])
        cur = {"l": 0}

        class PerLayer:
            def __init__(self, name, shape, dt=F32):
                self.t = [sb(f"{name}{i}", shape, dt) for i in range(n_layers)]

            def __getitem__(self, idx):
                return self.t[cur["l"]][idx]

        vecs = PerLayer("vecs", [128, NV])
        modv = PerLayer("modv", [128, 64])
        drv = PerLayer("drv", [128, 48])
        saved = PerLayer("saved", [128, 264])
        role = sb("role", [128, 1])
        kf = sb("kf", [128, 512])
        kb = sb("kb", [128, NKB], BF16)
        cact = sb("cact", [128, 8])
        state = sb("state", [128, 264])
        b_r = PerLayer("b_r", [128, 20])

        ident = kb[:, K_ID:K_ID + 128]
        ones = kb[:, K_ONE:K_ONE + 128]
        maskb = kb[:, K_MASK:K_MASK + 128]

        def carveB(off, shape):
            n = 1
            for s in shape[1:]:
                n *= s
            v = AB[0:shape[0], off:off + n]
            if len(shape) == 3:
                v = v.rearrange("p (a b) -> p a b", b=shape[2])
            elif len(shape) == 4:
                v = v.rearrange("p (a b c) -> p a b c", b=shape[2], c=shape[3])
            return v

        def carveF(off, shape):
            n = 1
            for s in shape[1:]:
                n *= s
            v = AFt[0:shape[0], off:off + n]
            if len(shape) == 3:
                v = v.rearrange("p (a b) -> p a b", b=shape[2])
            elif len(shape) == 4:
                v = v.rearrange("p (a b c) -> p a b c", b=shape[2], c=shape[3])
            return v

        o = 0
        w_in = carveB(o, [128, 8, INW]); o += 8 * INW
        w_out = carveB(o, [128, 9, D]); o += 9 * D
        lru_w = carveB(o, [128, 6, 128]); o += 768
        gla_wg = carveB(o, [16, 224]); o += 224
        hT = carveB(o, [128, 8, T]); o += 8 * T
        xsq = carveB(o, [128, 2, T]); o += 2 * T
        xb_bf = carveB(o, [128, 3, T]); o += 3 * T
        ybf = carveB(o, [128, 2, T]); o += 2 * T
        ysq = carveB(o, [128, 2, T]); o += 2 * T
        glr_bf = carveB(o, [16, T]); o += T
        qin = carveB(o, [128, 2, T]); o += 2 * T
        kin = carveB(o, [128, 2, T]); o += 2 * T
        kout = carveB(o, [128, 2, T]); o += 2 * T
        kT = carveB(o, [128, T // 128, 224]); o += (T // 128) * 224
        v_bf = carveB(o, [128, T // 128, 384]); o += (T // 128) * 384
        scT = carveB(o, [128, T // 128, 512]); o += (T // 128) * 512
        sprev = carveB(o, [128, T // 64, 192]); o += (T // 64) * 192
        osq = carveB(o, [128, 4, T]); o += 4 * T
        mixed = carveB(o, [128, 9, T]); o += 9 * T
        dg = carveB(o, [128, 8, 128]); o += 1024
        ubuf = carveB(o, [128, 2, 30 + T]); o += 2 * (30 + T)
        assert o <= 45056, o
        o = 0
        h2 = carveB(o, [128, 8, NTOK]); o += 8 * NTOK
        wg_s = [carveB(o + i * 12288, [128, 8, 512]) for i in range(2)]
        wu_s = [carveB(o + i * 12288 + 4096, [128, 8, 512]) for i in range(2)]
        wd_s = [carveB(o + i * 12288 + 8192, [128, 4, D]) for i in range(2)]
        o += 2 * 12288
        hid = [carveB(o, [128, 4, TM]) for i in range(2)]
        xsq2 = carveB(o, [128, 2, TM])
        comb2 = carveB(o + 1024, [128, 16, 32])
        o += 2048
        combT = carveB(o, [32, NTOK]); o += NTOK
        assert o <= 45056, o
        w_r = PerLayer("w_r", [128, 8, 20], BF16)

        o = 0
        rstd = carveF(o, [128, T]); o += T
        xs = carveF(o, [128, 2, T]); o += 2 * T
        sg = carveF(o, [128, 2, T]); o += 2 * T
        yc = carveF(o, [128, 2, T]); o += 2 * T
        cmean = carveF(o, [128, T]); o += T
        crstd = carveF(o, [128, T]); o += T
        lrx = carveF(o, [128, 3, 3 + T]); o += 3 * (3 + T)
        LBs = []
        for c in range(2):
            LBs.append([carveF(o + i * T, [128, T]) for i in range(4)])
            o += 4 * T
        LB = [LBs[0], LBs[1], LBs[0]]
        T1 = carveF(o, [128, 2, T]); o += 2 * T
        T2 = carveF(o, [128, 2, T]); o += 2 * T
        HB = []
        for c in range(4):
            HB.append([carveF(o + i * T, [128, T]) for i in range(2)])
            o += 2 * T
        assert o <= 8320, o
        wada_f = carveF(0, [128, 8, 512])
        modrow = carveF(4096, [1, 512])
        wada_b = [carveB(i * 4096, [128, 8, 512]) for i in range(2)]
        o = 0
        rstd2 = carveF(o, [128, TM]); o += TM
        xs2 = carveF(o, [128, 2, TM]); o += 2 * TM
        sa = carveF(o, [128, 2, TM]); o += 2 * TM
        tu = carveF(o, [128, 2, TM]); o += 2 * TM
        cb = carveF(o, [128, 2, TM]); o += 2 * TM
        ost = carveF(o, [128, 2, TM]); o += 2 * TM
        small = carveF(o, [128, 2048]); o += 2048
        assert o <= 8320, o

        psb = [st.enter_context(nc.psum_tensor(f"ps{i}", [128, 512], F32)) for i in range(7)]
        pst = st.enter_context(nc.psum_tensor("pst", [128, 1024], BF16))
        ps_state = {"h": 0, "b": 0}

        def ps_bank():
            i = ps_state["b"]
            ps_state["b"] = (i + 1) % 7
            return psb[i][:, :]

        def ps_half():
            return ps_bank()[:, 0:256]

        P.dma("sp", kf[:], kf_d[:], "ld_c")
        P.dma("sp", role[:], role_d[:], "ld_c")
        P.dma("sp", cact[:], cT_d[:], "ld_c")
        P.dma("pool", kb[:], kb_d[:], "ld_kb")
        P.close("ld_c")
        P.close("ld_kb")
        P.memset("dve", state[:], 0.0)
        P.memset("dve", AFt[:, :], 0.0)
        P.memset("dve", AB[:, :], 0.0)
        P.act(cact[:], cact[:], AF.Silu)

        def load_mixer_weights(Ld):
            for kc in range(8):
                P.dma("pool", w_in[:, kc, :], Ld["w_in"][kc * 128:(kc + 1) * 128, :], "ld_wm")
            for j in range(5):
                P.dma("pool", w_out[:, j, :], Ld["w_out"][j * 128:(j + 1) * 128, :], "ld_wm")
            for h in range(4):
                P.dma("pool", w_out[0:96, 5 + h, :], Ld["w_out"][640 + h * 96:640 + (h + 1) * 96, :], "ld_wm")
            P.dma("pool", lru_w.rearrange("p a b -> p (a b)"), Ld["lru_w"][:, :], "ld_wm")
            P.dma("pool", gla_wg, Ld["gla_wg"][:, :], "ld_wm")
            P.close("ld_wm")

        def layer_prologue(l, Ld):
            P.dma("sp", vecs[:], Ld["vecs"][:, :], "ld_v")
            P.dma("sp", b_r[:], Ld["b_r"][:, :], "ld_v")
            for kc in range(8):
                P.dma("pool", w_r[:, kc, :], Ld["w_r"][kc * 128:(kc + 1) * 128, :], "ld_wr")
            P.close("ld_wr")
            P.close("ld_v")
            mod_ps = psb[6][:, :]
            for piece in range(12):
                c0 = piece * 512
                for kc in range(8):
                    P.dma("sp" if kc % 2 == 0 else "act", wada_f[:, kc, :],
                          Ld["w_ada"][kc * 128:(kc + 1) * 128, c0:c0 + 512], "ld_wa")
                P.close("ld_wa")
                wb = wada_b[piece % 2]
                P.copy("dve", wb, wada_f)
                prow = psb[piece % 2][:, :]
                for kc in range(8):
                    P.mm(prow[0:1, :], cact_bf[:, kc:kc + 1], wb[:, kc, :], start=(kc == 0), stop=(kc == 7))
                P.copy("act", modrow, prow[0:1, :])
                for jj in range(4):
                    j = piece * 4 + jj
                    P.mm(mod_ps[:, j:j + 1], modrow[0:1, jj * 128:(jj + 1) * 128], onef[0:1, 0:1])
            P.tt("dve", modv[:, 0:48], mod_ps[:, 0:48], vecs[:, V_BADA:V_BADA + 48], ALU.add)
            P.stt("dve", drv[:, 0:8], modv[:, 8:16], 1.0, vecs[:, V_GMIX:V_GMIX + 8], ALU.add, ALU.mult)
            P.stt("dve", drv[:, 8:16], modv[:, 32:40], 1.0, vecs[:, V_GFFN:V_GFFN + 8], ALU.add, ALU.mult)
            P.act(drv[:, 22:25], vecs[:, V_LAM:V_LAM + 3], AF.Exp, scale=-1.0)
            P.act(drv[:, 22:25], drv[:, 22:25], AF.Ln, bias=1.0)
            P.ts("dve", drv[:, 16:19], drv[:, 22:25], -8.0, ALU.mult)
            P.ts("dve", drv[:, 19:22], drv[:, 22:25], -16.0, ALU.mult)

        A1 = lambda c: drv[:, c:c + 1]
        A2 = lambda c: drv[:, 8 + c:9 + c]
        SH1 = lambda c: modv[:, c:c + 1]
        GT1 = lambda c: modv[:, 16 + c:17 + c]
        SH2 = lambda c: modv[:, 24 + c:25 + c]
        GT2 = lambda c: modv[:, 40 + c:41 + c]
        vcol = lambda j: vecs[:, j:j + 1]

        def rms_mod(t0, tw, sq_buf, rstd_buf, xs_buf, dst, Afn, Bfn):
            ms = ps_bank() if tw == 512 else ps_half()
            for c in range(8):
                P.act(sq_buf[:, c % 2, :], x[:, c, t0:t0 + tw], AF.Square)
                P.mm(ms[:, 0:tw], ones, sq_buf[:, c % 2, :], start=(c == 0), stop=(c == 7))
            P.act(rstd_buf, ms[:, 0:tw], AF.Ln, bias=vcol_eps, scale=1.0 / D)
            P.act(rstd_buf, rstd_buf, AF.Exp, scale=-0.5)
            for c in range(8):
                P.tt("dve", xs_buf[:, c % 2, :], x[:, c, t0:t0 + tw], rstd_buf, ALU.mult)
                if Bfn is None:
                    P.act(dst(c), xs_buf[:, c % 2, :], AF.Identity, scale=Afn(c))
                else:
                    P.act(dst(c), xs_buf[:, c % 2, :], AF.Identity, scale=Afn(c), bias=Bfn(c))

        epsT = sb("epsT", [128, 1])
        P.memset("dve", epsT[:], EPS)
        onef = sb("onef", [128, 1])
        P.memset("dve", onef[:], 1.0)
        cact_bf = sb("cact_bf", [128, 8], BF16)
        P.copy("dve", cact_bf[:], cact[:])
        vcol_eps = epsT[:, 0:1]

        def rsqrt_act(dst, src, bias, scale):
            P.act(dst, src, AF.Ln, bias=bias, scale=scale)
            P.act(dst, dst, AF.Exp, scale=-0.5)

        def mixer_tile(j, light=False):
            t0 = j * T
            rms_mod(t0, T, xsq, rstd, xs, lambda c: hT[:, c, :], A1, SH1)

            def zproj(c0, m, dst):
                for kc in range(8):
                    P.mm(dst, w_in[:, kc, c0:c0 + m], hT[:, kc, :], start=(kc == 0), stop=(kc == 7))

            A, B, C = [], [[], [], []], []
            do_conv = (not light) or j == NT - 1
            cst = {}

            def a1(c):
                pg = ps_half()
                zproj(C_CVG + c * 128, 128, pg)
                P.act(sg[:, c, :], pg, AF.Sigmoid)
                pv = ps_half()
                zproj(C_CVV + c * 128, 128, pv)
                P.tt("dve", ubuf[:, c, 30:30 + T], pv, sg[:, c, :], ALU.mult)

            def a2(c):
                if not light:
                    wc = V_CDW + c * 31
                    pc = ps_half()
                    for k in range(31):
                        dk = dg[:, k % 8, :]
                        P.ts("dve", dk, ident, vcol(wc + k), ALU.mult)
                        P.mm(pc, dk, ubuf[:, c, k:k + T], start=(k == 0), stop=(k == 30))
                    P.act(yc[:, c, :], pc, AF.Identity, bias=vcol(V_CDB + c))
                    P.act(ysq[:, c, :], pc, AF.Square, bias=vcol(V_CDB + c))
                    P.act(ybf[:, c, :], pc, AF.Identity, bias=vcol(V_CDB + c))
                P.copy("dve", ubuf[:, c, 0:30], ubuf[:, c, T:T + 30])

            def a3():
                pm = ps_half()
                pq = ps_half()
                for c in range(2):
                    P.mm(pm, ones, ybf[:, c, :], start=(c == 0), stop=(c == 1))
                for c in range(2):
                    P.mm(pq, ones, ysq[:, c, :], start=(c == 0), stop=(c == 1))
                P.act(cmean, pm, AF.Identity, scale=1.0 / 256)
                P.tt("dve", crstd, cmean, cmean, ALU.mult)
                P.stt("dve", crstd, pq, 1.0 / 256, crstd, ALU.mult, ALU.subtract)
                rsqrt_act(crstd, crstd, vcol_eps, 1.0)
                for c in range(2):
                    P.tt("dve", yc[:, c, :], yc[:, c, :], cmean, ALU.subtract)
                    P.tt("dve", yc[:, c, :], yc[:, c, :], crstd, ALU.mult)
                    P.act(mixed[:, c, :], yc[:, c, :], AF.Silu, scale=vcol(V_CLG + c), bias=vcol(V_CLB + c))

            if do_conv:
                A += [lambda: a1(0), lambda: a1(1), lambda: a2(0), lambda: a2(1)]
                if not light:
                    A.append(a3)

            def b1(c):
                Bx, Ba, Bi, Bm = LB[c]
                px = ps_half()
                zproj(C_LRX + c * 128, 128, px)
                P.copy("act", lrx[:, c, 3:3 + T], px)
                wl = V_LCW + c * 4
                P.ts("dve", Bx, lrx[:, c, 0:T], vcol(wl), ALU.mult, vcol(V_LCB + c), ALU.add)
                for k in range(1, 4):
                    P.stt("dve", Bx, lrx[:, c, k:k + T], vcol(wl + k), Bx, ALU.mult, ALU.add)
                P.copy("dve", lrx[:, c, 0:3], lrx[:, c, T:T + 3])
                P.copy("act", xb_bf[:, c, :], Bx)

            def b2(c):
                Bx, Ba, Bi, Bm = LB[c]
                pa = ps_half()
                P.mm(pa, lru_w[:, c, :], xb_bf[:, c, :])
                pi = ps_half()
                P.mm(pi, lru_w[:, 3 + c, :], xb_bf[:, c, :])
                P.act(Ba, pa, AF.Sigmoid, bias=vcol(V_LBA + c))
                P.act(Bi, pi, AF.Sigmoid, bias=vcol(V_LBI + c))
                P.act(Bm, Ba, AF.Exp, scale=drv[:, 19 + c:20 + c])
                P.act(Ba, Ba, AF.Exp, scale=drv[:, 16 + c:17 + c])
                P.ts("dve", Bm, Bm, -1.0, ALU.mult, 1.0, ALU.add)
                P.act(Bm, Bm, AF.Ln)
                P.act(Bm, Bm, AF.Exp, scale=0.5)
                P.tt("dve", Bi, Bi, Bx, ALU.mult)
                P.tt("dve", Bi, Bi, Bm, ALU.mult)

            def b3(c):
                Bx, Ba, Bi, Bm = LB[c]
                Bh, Bg = Bm, Bx
                P.scan("dve", Bh, Ba, Bi, state[:, c:c + 1], ALU.mult, ALU.add)
                P.copy("dve", state[:, c:c + 1], Bh[:, T - 1:T])
                if not light:
                    py = ps_half()
                    zproj(C_LRY + c * 128, 128, py)
                    P.act(Bg, py, AF.Gelu_apprx_tanh)
                    P.tt("dve", mixed[:, 2 + c, :], Bh, Bg, ALU.mult)

            for c in range(3):
                B[c] += [lambda c=c: b1(c), lambda c=c: b2(c), lambda c=c: b3(c)]

            e1v = T1[0:112, :, :].rearrange("p a (n c) -> p a n c", c=64)

            def c1():
                pgl = ps_half()
                zproj(C_GLR, 16, pgl[0:16, :])
                P.copy("act", glr_bf, pgl[0:16, :])
                for p in range(2):
                    pl = ps_half()
                    P.mm(pl[0:112, :], gla_wg[:, p * 112:(p + 1) * 112], glr_bf)
                    P.act(T1[0:112, p, :], pl[0:112, :], AF.Identity, bias=vecs[0:112, V_GBG + p:V_GBG + p + 1])
                t1 = T1[0:112, :, :]
                t2 = T2[0:112, :, :]
                P.stt("dve", t2, t1, -1.0, t1, ALU.mult, ALU.min)
                P.act(t2, t2, AF.Exp)
                P.act(t2, t2, AF.Ln, bias=1.0)
                P.ts("dve", t1, t1, 0.0, ALU.min)
                P.tt("dve", t1, t1, t2, ALU.subtract)
                t1f = T1[0:112, :, :].rearrange("p a t -> p (a t)")
                t2f = T2[0:112, :, :].rearrange("p a t -> p (a t)")
                P.scan("dve", t2f, kf[0:112, 0:2 * T], t1f, 0.0, ALU.mult, ALU.add)
                P.act(t1, t2, AF.Exp, scale=1.0 / 16)
                P.act(t2, t2, AF.Exp, scale=-1.0 / 16)

            def c2():
                t2 = T2[0:112, :, :]
                for p in range(2):
                    if not light:
                        pqq = ps_half()
                        zproj(C_Q + p * 112, 112, pqq[0:112, :])
                        P.stt("dve", qin[0:112, p, :], pqq[0:112, :], 48.0 ** -0.5, T1[0:112, p, :], ALU.mult, ALU.mult)
                    pk = ps_half()
                    zproj(C_K + p * 112, 112, pk[0:112, :])
                    P.tt("dve", T2[0:112, p, :], pk[0:112, :], T2[0:112, p, :], ALU.mult)
                if not light:
                    P.copy("act", kin[0:112, :, :], t2)
                e1last = e1v[:, :, :, 63:64].to_broadcast([112, 2, T // 64, 64])
                P.tt("dve", kout[0:112, :, :].rearrange("p a (n c) -> p a n c", c=64),
                     T2[0:112, :, :].rearrange("p a (n c) -> p a n c", c=64), e1last, ALU.mult)

            def c0():
                for g in range(T // 128):
                    pvv = ps_bank()
                    for kc in range(8):
                        P.mm(pvv[:, 0:384], hT[:, kc, g * 128:(g + 1) * 128], w_in[:, kc, C_V:C_V + 384],
                             start=(kc == 0), stop=(kc == 7))
                    P.copy("act", v_bf[:, g, :], pvv[:, 0:384])

            def c3():
                for g in range(T // 128):
                    for p in range(2):
                        P.transpose(pst[:, (g * 2 + p) * 112:(g * 2 + p + 1) * 112],
                                    kout[0:112, p, g * 128:(g + 1) * 128], ident[0:112, 0:112])
                    P.copy("act", kT[:, g, :], pst[:, g * 224:(g + 1) * 224])
                if not light:
                    for g in range(T // 128):
                        pscs = [ps_bank(), ps_bank()]
                        for h in (0, 2, 1, 3):
                            p, hp = h // 2, (h % 2) * 64
                            P.mm(pscs[h % 2][:, p * 128:(p + 1) * 128], kin[hp:hp + 48, p, g * 128:(g + 1) * 128],
                                 qin[hp:hp + 48, p, g * 128:(g + 1) * 128])
                        scv = scT[:, g, :].rearrange("p (a b c) -> p a b c", b=2, c=128)
                        for par in range(2):
                            P.tt("dve", scv[:, :, par, :], pscs[par][:, 0:256].rearrange("p (a c) -> p a c", c=128),
                                 maskb.unsqueeze(1).to_broadcast([128, 2, 128]), ALU.mult)

            def c4():
                Sv = state[0:112, 3:195].rearrange("p (a e) -> p a e", e=96)
                for n in range(T // 64):
                    g, r0 = n // 2, (n % 2) * 64
                    pkv = ps_half()
                    for h in (0, 2, 1, 3):
                        p, hp = h // 2, (h % 2) * 64
                        P.mm(pkv[hp:hp + 48, p * 96:(p + 1) * 96],
                             kT[r0:r0 + 64, g, p * 112 + hp:p * 112 + hp + 48],
                             v_bf[r0:r0 + 64, g, h * 96:(h + 1) * 96], sync_prev=(h == 1))
                    if not light:
                        P.copy("act", sprev[0:112, n, :].rearrange("p (a e) -> p a e", e=96), Sv)
                    for p in range(2):
                        P.stt("dve", Sv[:, p, :], Sv[:, p, :], e1v[:, p, n, 63:64], pkv[0:112, p * 96:(p + 1) * 96],
                              ALU.mult, ALU.add)

            def c5_mm():
                b0 = ps_state["b"]
                idxs = [(b0 + k) % 7 for k in range(4)]
                ps_state["b"] = (b0 + 4) % 7
                pos = [psb[i][:, 0:256] for i in idxs]
                cst["po"] = pos
                cst["free"] = [i for i in range(7) if i not in idxs]
                cst["fi"] = 0
                for n in range(T // 64):
                    g = n // 2
                    for h in range(4):
                        P.mm(pos[h][0:96, n * 64:(n + 1) * 64], v_bf[:, g, h * 96:(h + 1) * 96],
                             scT[:, g, h * 128 + (n % 2) * 64:h * 128 + (n % 2) * 64 + 64], start=True, stop=False)
                    for h in (0, 2, 1, 3):
                        p, hp = h // 2, (h % 2) * 64
                        P.mm(pos[h][0:96, n * 64:(n + 1) * 64], sprev[hp:hp + 48, n, p * 96:(p + 1) * 96],
                             qin[hp:hp + 48, p, n * 64:(n + 1) * 64], start=False, stop=True)

            def c5_tail():
                pos = cst["po"]

                def fbank():
                    i = cst["free"][cst["fi"] % 3]
                    cst["fi"] += 1
                    return psb[i][:, 0:256]
                for h in range(4):
                    P.act(osq[0:96, h, :], pos[h][0:96, :], AF.Square)
                for h in range(4):
                    pms = fbank()
                    P.mm(pms[0:96, :], ones[0:96, 0:96], osq[0:96, h, :])
                    P.act(HB[h][0][0:96, :], pms[0:96, :], AF.Ln, bias=epsT[0:96, 0:1], scale=1.0 / 96)
                for h in range(4):
                    P.act(HB[h][0][0:96, :], HB[h][0][0:96, :], AF.Exp, scale=-0.5)
                for h in range(4):
                    rs_h, sog_h = HB[h]
                    P.stt("dve", rs_h[0:96, :], pos[h][0:96, :], vecs[0:96, V_GNG + h:V_GNG + h + 1], rs_h[0:96, :],
                          ALU.mult, ALU.mult)
                    P.tt("dve", mixed[0:96, 5 + h, :], rs_h[0:96, :], sog_h[0:96, :], ALU.mult)

            def c_og():
                for h in range(4):
                    pog = ps_half()
                    zproj(C_OG + h * 96, 96, pog[0:96, :])
                    P.act(HB[h][1][0:96, :], pog[0:96, :], AF.Silu)

            C += [c0, c1, c2, c3, c4]
            if not light:
                C.insert(1, c_og)
            if not light:
                def c5_all():
                    c5_mm()
                    c5_tail()
                C += [c5_all]

            Bflat = B[0] + B[1] + B[2]
            if INTERLEAVE:
                lists = [A, Bflat, C]
                pos = [0, 0, 0]
                while any(pos[i] < len(lists[i]) for i in range(3)):
                    for i in range(3):
                        if pos[i] < len(lists[i]):
                            lists[i][pos[i]]()
                            pos[i] += 1
            else:
                for f in A + Bflat + C:
                    f()
            if light:
                return
            for dc in range(8):
                pw = ps_half()
                for kc in range(9):
                    kk = 128 if kc < 5 else 96
                    P.mm(pw, w_out[0:kk, kc, dc * 128:(dc + 1) * 128], mixed[0:kk, kc, :],
                         start=(kc == 0), stop=(kc == 8))
                P.stt("dve", x[:, dc, t0:t0 + T], pw, GT1(dc), x[:, dc, t0:t0 + T], ALU.mult, ALU.add)

        def moe_layer(Ld):
            def load_expert(e):
                s = e % 2
                for kc in range(8):
                    P.dma("pool", wg_s[s][:, kc, :], Ld["w_gate"][e, kc * 128:(kc + 1) * 128, :], f"ld_e{s}")
                    P.dma("pool", wu_s[s][:, kc, :], Ld["w_up"][e, kc * 128:(kc + 1) * 128, :], f"ld_e{s}")
                for kc in range(4):
                    P.dma("pool", wd_s[s][:, kc, :], Ld["w_down"][e, kc * 128:(kc + 1) * 128, :], f"ld_e{s}")
                P.close(f"ld_e{s}")

            for tm in range(NTM):
                rms_mod(tm * TM, TM, xsq2, rstd2, xs2, lambda c, tm=tm: h2[:, c, tm * TM:(tm + 1) * TM], A2, SH2)
            load_expert(0)
            load_expert(1)
            plg = ps_bank()
            for s in range(16):
                for kc in range(8):
                    P.mm(plg[:, s * 20:(s + 1) * 20], h2[:, kc, s * 128:(s + 1) * 128], w_r[:, kc, :],
                         start=(kc == 0), stop=(kc == 7))
            o = 0

            def sm(n):
                nonlocal o
                v = small[:, o:o + n]
                o += n
                return v
            Lg = sm(320).rearrange("p (s k) -> p s k", k=20)
            P.tt("dve", Lg, plg[:, 0:320].rearrange("p (s k) -> p s k", k=20),
                 b_r[:].unsqueeze(1).to_broadcast([128, 16, 20]), ALU.add)
            gl = Lg[:, :, 0:4]
            gmax = sm(16)
            P.reduce("dve", gmax, gl, ALU.max)
            ohg = sm(64).rearrange("p (s k) -> p s k", k=4)
            P.tt("dve", ohg, gl, gmax.unsqueeze(2).to_broadcast([128, 16, 4]), ALU.is_ge)
            ex = sm(64).rearrange("p (s k) -> p s k", k=4)
            P.tt("dve", ex, gl, gmax.unsqueeze(2).to_broadcast([128, 16, 4]), ALU.subtract)
            P.act(ex, ex, AF.Exp)
            psel = sm(16)
            P.reduce("dve", psel, ex, ALU.add)
            P.recip("dve", psel, psel)
            le = Lg[:, :, 4:20].rearrange("p s (g j) -> p s g j", j=4)
            tmp4 = sm(256).rearrange("p (s g j) -> p s g j", g=4, j=4)
            P.tt("dve", tmp4, le, ohg.unsqueeze(3).to_broadcast([128, 16, 4, 4]), ALU.mult)
            el = sm(64).rearrange("p (s j) -> p s j", j=4)
            P.reduce("dve", el, tmp4.rearrange("p s g j -> p s j g"), ALU.add)
            m1 = sm(16)
            P.reduce("dve", m1, el, ALU.max)
            oh1 = sm(64).rearrange("p (s j) -> p s j", j=4)
            P.tt("dve", oh1, el, m1.unsqueeze(2).to_broadcast([128, 16, 4]), ALU.is_ge)
            el2 = sm(64).rearrange("p (s j) -> p s j", j=4)
            P.stt("dve", el2, oh1, -1e30, el, ALU.mult, ALU.add)
            m2 = sm(16)
            P.reduce("dve", m2, el2, ALU.max)
            oh2 = sm(64).rearrange("p (s j) -> p s j", j=4)
            P.tt("dve", oh2, el2, m2.unsqueeze(2).to_broadcast([128, 16, 4]), ALU.is_ge)
            dd = sm(16)
            P.tt("dve", dd, m2, m1, ALU.subtract)
            P.act(dd, dd, AF.Exp)
            w1 = sm(16)
            P.ts("dve", w1, dd, 1.0, ALU.add)
            P.recip("dve", w1, w1)
            w2 = sm(16)
            P.tt("dve", w2, dd, w1, ALU.mult)
            P.tt("dve", w1, w1, psel, ALU.mult)
            P.tt("dve", w2, w2, psel, ALU.mult)
            wj = sm(64).rearrange("p (s j) -> p s j", j=4)
            P.tt("dve", wj, oh1, w1.unsqueeze(2).to_broadcast([128, 16, 4]), ALU.mult)
            P.tt("dve", oh2, oh2, w2.unsqueeze(2).to_broadcast([128, 16, 4]), ALU.mult)
            P.tt("dve", wj, wj, oh2, ALU.add)
            comb = sm(256).rearrange("p (s g j) -> p s g j", g=4, j=4)
            P.tt("dve", comb, ohg.unsqueeze(3).to_broadcast([128, 16, 4, 4]),
                 wj.unsqueeze(2).to_broadcast([128, 16, 4, 4]), ALU.mult)
            combf = comb.rearrange("p s g j -> p s (g j)")
            P.copy("dve", comb2[:, :, 0:16], combf)
            chif = sm(256).rearrange("p (s k) -> p s k", k=16)
            P.copy("dve", chif, comb2[:, :, 0:16])
            P.tt("dve", comb2[:, :, 16:32], combf, chif, ALU.subtract)
            for half in range(2):
                for s8 in range(8):
                    s = half * 8 + s8
                    P.transpose(pst[0:32, s8 * 128:(s8 + 1) * 128], comb2[:, s, :], ident)
                P.copy("act", combT[:, half * 1024:(half + 1) * 1024], pst[0:32, 0:1024])
            for e in range(16):
                s = e % 2
                wg, wu, wd = wg_s[s], wu_s[s], wd_s[s]
                for tm in range(NTM):
                    tsl = slice(tm * TM, (tm + 1) * TM)
                    pcb = ps_bank()
                    P.mm(pcb, kb[0:32, K_SEL + e * 128:K_SEL + (e + 1) * 128], combT[:, tsl])
                    cbv = cb[:, tm % 2, :]
                    P.copy("act", cbv, pcb)
                    hd = hid[0]
                    for fc in range(4):
                        pa = ps_bank()
                        for kc in range(8):
                            P.mm(pa, wg[:, kc, fc * 128:(fc + 1) * 128], h2[:, kc, tsl], start=(kc == 0), stop=(kc == 7))
                        pu = ps_bank()
                        for kc in range(8):
                            P.mm(pu, wu[:, kc, fc * 128:(fc + 1) * 128], h2[:, kc, tsl], start=(kc == 0), stop=(kc == 7))
                        P.act(sa[:, fc % 2, :], pa, AF.Silu)
                        P.tt("dve", tu[:, fc % 2, :], pu, cbv, ALU.mult)
                        P.tt("dve", hd[:, fc, :], sa[:, fc % 2, :], tu[:, fc % 2, :], ALU.mult)
                    for dg2 in range(4):
                        pys = [ps_bank(), ps_bank()]
                        for fc in range(4):
                            for i in range(2):
                                dc = dg2 * 2 + i
                                P.mm(pys[i], wd[:, fc, dc * 128:(dc + 1) * 128], hd[:, fc, :],
                                     start=(fc == 0), stop=(fc == 3))
                        for i in range(2):
                            dc = dg2 * 2 + i
                            P.stt("dve", x[:, dc, tsl], pys[i], GT2(dc), x[:, dc, tsl], ALU.mult, ALU.add)
                if e + 2 < 16:
                    load_expert(e + 2)

        for phase in range(2):
            for c in range(8):
                P.dma("sp", x[:, c, :], xT_ds[phase][c * 128:(c + 1) * 128, :], "ld_x")
            P.close("ld_x")
            for l in range(n_layers):
                Ld = L[l]
                cur["l"] = l
                if phase == 0:
                    layer_prologue(l, Ld)
                load_mixer_weights(Ld)
                if phase == 0:
                    P.memset("dve", state[:], 0.0)
                else:
                    P.ts("dve", state[:], saved[:, :], role[:, 0:1], ALU.mult)
                for c in range(2):
                    P.copy("dve", ubuf[:, c, 0:30], state[:, 195 + c * 30:195 + (c + 1) * 30])
                for c in range(3):
                    P.copy("dve", lrx[:, c, 0:3], state[:, 255 + c * 3:255 + (c + 1) * 3])
                light = (phase == 0 and l == n_layers - 1)
                for j in range(NT):
                    mixer_tile(j, light=light)
                if phase == 0:
                    for c in range(2):
                        P.copy("dve", state[:, 195 + c * 30:195 + (c + 1) * 30], ubuf[:, c, 0:30])
                    for c in range(3):
                        P.copy("dve", state[:, 255 + c * 3:255 + (c + 1) * 3], lrx[:, c, 0:3])
                    P.copy("dve", saved[:, :], state[:])
                if not (phase == 0 and l == n_layers - 1):
                    moe_layer(Ld)

        yNv = yN_d.rearrange("(c p) t -> p c t", p=128)
        GF = lambda c: vecs[:, V_GFIN + c:V_GFIN + c + 1]
        for tm in range(NTM):
            tsl = slice(tm * TM, (tm + 1) * TM)
            cnt = [0]

            def dstf(c):
                return ost[:, c % 2, :]
            ms = ps_bank()
            for c in range(8):
                P.act(xsq2[:, c % 2, :], x[:, c, tsl], AF.Square)
                P.mm(ms, ones, xsq2[:, c % 2, :], start=(c == 0), stop=(c == 7))
            P.act(rstd2, ms, AF.Ln, bias=vcol_eps, scale=1.0 / D)
            P.act(rstd2, rstd2, AF.Exp, scale=-0.5)
            for c in range(8):
                P.tt("dve", xs2[:, c % 2, :], x[:, c, tsl], rstd2, ALU.mult)
                P.act(ost[:, c % 2, :], xs2[:, c % 2, :], AF.Identity, scale=GF(c))
                P.dma("sp", yNv[:, c, tsl], ost[:, c % 2, :], f"st_o{c % 2}", out_sb=False, in_sb=True)
                P.close(f"st_o{c % 2}")
        P.emit(st, final_waits=["st_o0", "st_o1"])
    return nc


def _consts():
    kf = np.ones((128, 512), np.float32)
    kf[:, 0::64] = 0.0
    kb = np.zeros((128, NKB), np.float32)
    kb[:, K_ID:K_ID + 128] = np.eye(128, dtype=np.float32)
    kb[:, K_ONE:K_ONE + 128] = 1.0
    jj = np.arange(128)[:, None]
    cc = np.arange(128)[None, :]
    kb[:, K_MASK:K_MASK + 128] = ((jj // 64 == cc // 64) & (jj <= cc)).astype(np.float32)
    for e in range(16):
        kb[e, K_SEL + e * 128:K_SEL + (e + 1) * 128] = 1.0
        kb[16 + e, K_SEL + e * 128:K_SEL + (e + 1) * 128] = 1.0
    return kf, kb


def _col(v):
    v = np.asarray(v, np.float32)
    return np.ascontiguousarray(v.reshape(-1, 128).T)


def _layer_inputs(inp, l):
    f = lambda k: np.asarray(inp[k][l], np.float32)
    vecs = np.zeros((128, NV), np.float32)
    vecs[:, V_GMIX:V_GMIX + 8] = _col(f("g_mix"))
    vecs[:, V_GFFN:V_GFFN + 8] = _col(f("g_ffn"))
    vecs[:, V_CDB:V_CDB + 2] = _col(f("conv_dw_b"))
    vecs[:, V_CLG:V_CLG + 2] = _col(f("conv_ln_g"))
    vecs[:, V_CLB:V_CLB + 2] = _col(f("conv_ln_b"))
    vecs[:, V_LCB:V_LCB + 3] = _col(f("lru_conv_b"))
    vecs[:, V_LBA:V_LBA + 3] = _col(f("lru_b_a"))
    vecs[:, V_LBI:V_LBI + 3] = _col(f("lru_b_i"))
    vecs[:, V_LAM:V_LAM + 3] = _col(f("lru_lam"))
    gng = f("gla_norm_g").reshape(4, 96)
    bg = f("gla_b_gate").reshape(4, 48)
    for h in range(4):
        vecs[0:96, V_GNG + h] = gng[h]
        vecs[(h % 2) * 64:(h % 2) * 64 + 48, V_GBG + h // 2] = bg[h]
    vecs[:, V_BADA:V_BADA + 48] = _col(f("b_ada"))
    cw = f("conv_dw_w")
    for c in range(2):
        vecs[:, V_CDW + c * 31:V_CDW + (c + 1) * 31] = cw[:, c * 128:(c + 1) * 128].T
    lw = f("lru_conv_w")
    for c in range(3):
        vecs[:, V_LCW + c * 4:V_LCW + (c + 1) * 4] = lw[:, c * 128:(c + 1) * 128].T
    vecs[:, V_GFIN:V_GFIN + 8] = _col(np.asarray(inp["g_final"], np.float32))
    w_in = f("w_in")
    wp = np.zeros((D, INW), np.float32)
    wp[:, 0:1280] = w_in[:, 0:1280]
    for h in range(4):
        p, hp = h // 2, (h % 2) * 64
        wp[:, C_Q + p * 112 + hp:C_Q + p * 112 + hp + 48] = w_in[:, 1280 + h * 48:1280 + (h + 1) * 48]
        wp[:, C_K + p * 112 + hp:C_K + p * 112 + hp + 48] = w_in[:, 1472 + h * 48:1472 + (h + 1) * 48]
    wp[:, C_V:C_V + 384] = w_in[:, 1664:2048]
    wp[:, C_GLR:C_GLR + 16] = w_in[:, 2048:2064]
    wp[:, C_OG:C_OG + 384] = w_in[:, 2064:2448]
    wgt = f("gla_w_gate")
    gwg = np.zeros((16, 224), np.float32)
    for h in range(4):
        p, hp = h // 2, (h % 2) * 64
        gwg[:, p * 112 + hp:p * 112 + hp + 48] = wgt[:, h * 48:(h + 1) * 48]
    lru_w = np.zeros((128, 6, 128), np.float32)
    wa, wi = f("lru_w_a"), f("lru_w_i")
    for c in range(3):
        for b in range(2):
            lru_w[b * 64:(b + 1) * 64, c, b * 64:(b + 1) * 64] = wa[2 * c + b]
            lru_w[b * 64:(b + 1) * 64, 3 + c, b * 64:(b + 1) * 64] = wi[2 * c + b]
    w_r = np.concatenate([f("w_route_group")] + [f("w_route_expert")[g] for g in range(4)], axis=1)
    b_r = np.concatenate([f("b_route_group"), f("b_route_expert").reshape(-1)])
    return {
        "w_ada": np.ascontiguousarray(f("w_ada")),
        "vecs": vecs,
        "w_in": wp,
        "w_out": np.ascontiguousarray(f("w_out")),
        "lru_w": np.ascontiguousarray(lru_w.reshape(128, 768)),
        "gla_wg": gwg,
        "w_r": np.ascontiguousarray(w_r),
        "b_r": np.ascontiguousarray(np.broadcast_to(b_r[None, :], (128, 20))),
        "w_gate": np.ascontiguousarray(f("w_gate").reshape(16, D, 512)),
        "w_up": np.ascontiguousarray(f("w_up").reshape(16, D, 512)),
        "w_down": np.ascontiguousarray(f("w_down").reshape(16, 512, D)),
    }


_NC_CACHE = {}


def _get_nc():
    if "nc" not in _NC_CACHE:
        _NC_CACHE["nc"] = build_program()
    return _NC_CACHE["nc"]


def make_in_maps(inputs, cores=range(8)):
    x = np.asarray(inputs["x"], np.float32)
    c = np.asarray(inputs["c"], np.float32)
    kf, kb = _consts()
    Lin = [_layer_inputs(inputs, l) for l in range(2)]
    halves = {}
    in_maps = []
    for core in cores:
        b, h = core // 2, core % 2
        for hh in (0, h):
            if (b, hh) not in halves:
                halves[(b, hh)] = np.ascontiguousarray(x[b, hh * NTOK:(hh + 1) * NTOK, :].T)
        m = {"xT1": halves[(b, 0)], "xT2": halves[(b, h)], "cT": _col(c[b]),
             "role": np.full((128, 1), float(h), np.float32), "kf": kf, "kb": kb}
        for l in range(2):
            for k, v in Lin[l].items():
                m[f"{k}{l}"] = v
        in_maps.append(m)
    return in_maps


def kernel(**inputs):
    x = np.asarray(inputs["x"], np.float32)
    out = np.empty_like(x)
    nc = _get_nc()
    in_maps = make_in_maps(inputs)
    res = run_bass_kernel_spmd(nc, in_maps, core_ids=list(range(8)))
    for core in range(8):
        b, h = core // 2, core % 2
        out[b, h * NTOK:(h + 1) * NTOK, :] = res.results[core]["yN"].T
    return out
```

```python
import numpy as np
from contextlib import ExitStack
import concourse.bass as bass
import concourse.mybir as mybir
from concourse.bass_utils import run_bass_kernel_spmd

F32 = mybir.dt.float32
BF16 = mybir.dt.bfloat16
AF = mybir.ActivationFunctionType
ALU = mybir.AluOpType
AX = mybir.AxisListType

ENGS = ("pe", "act", "dve", "pool", "sp")
INTERLEAVE = True

D = 1024
NTOK = 2048
T = 256
NT = NTOK // T
TM = 512
NTM = NTOK // TM
INW = 2512
EPS = 1e-6
NV = 170
C_CVV, C_CVG, C_LRX, C_LRY = 0, 256, 512, 896
C_Q, C_K, C_V, C_GLR, C_OG = 1280, 1504, 1728, 2112, 2128
V_GMIX, V_GFFN, V_CDB, V_CLG, V_CLB, V_LCB, V_LBA, V_LBI, V_LAM = 0, 8, 16, 18, 20, 22, 25, 28, 31
V_GNG, V_GBG, V_BADA, V_CDW, V_LCW, V_GFIN = 34, 38, 40, 88, 150, 162
K_ID, K_ONE, K_MASK, K_SEL = 0, 128, 256, 384
NKB = 384 + 2048


def _region(ap):
    t = ap.tensor
    pstride = 1
    for s in list(t.shape)[1:]:
        pstride *= int(s)
    off = int(ap.offset)
    p0 = off // pstride
    f0 = off % pstride
    pe = 0
    fe = 0
    for step, cnt in ap.ap:
        step = int(step)
        cnt = int(cnt)
        if cnt <= 1:
            continue
        if step >= pstride and step % pstride == 0:
            pe += (cnt - 1) * (step // pstride)
        else:
            fe += (cnt - 1) * abs(step)
    return (t.name, p0, p0 + pe + 1, f0, f0 + fe + 1)


class _Op:
    __slots__ = ("eng", "fn", "deps", "idx", "need", "dma", "val")

    def __init__(self, eng, fn):
        self.eng = eng
        self.fn = fn
        self.deps = {}
        self.idx = -1
        self.need = False
        self.dma = None
        self.val = 0


class Prog:
    def __init__(self, nc):
        self.nc = nc
        self.ops = {e: [] for e in ENGS}
        self.track = {}
        self.dma_counts = {}
        self.gen_end = {}
        self.sems = {}

    def _tok(self, op):
        if op.dma is not None:
            return ("d", op.dma[0], op.dma[1])
        return ("e", op.eng, op.idx)

    def _add_dep(self, op, tok, kind):
        if tok is None:
            return
        if tok[0] == "e":
            if tok[1] == op.eng and op.dma is None:
                if tok[2] == op.idx or op.eng == "pe":
                    return
            key = ("e", tok[1])
        else:
            key = ("d", tok[1])
        if op.deps.get(key, -1) < tok[2]:
            op.deps[key] = tok[2]

    @staticmethod
    def _compress(toks):
        best = {}
        for t in toks:
            k = (t[0], t[1])
            if k not in best or best[k][2] < t[2]:
                best[k] = t
        return list(best.values())

    def _read(self, op, ap):
        name, p0, p1, f0, f1 = _region(ap)
        if name.startswith("ps"):
            return self._write(op, ap)
        ents = self.track.setdefault(name, [])
        tok = self._tok(op)
        for e in ents:
            if e[0] < p1 and p0 < e[1] and e[2] < f1 and f0 < e[3]:
                self._add_dep(op, e[4], "raw")
                e[5].append(tok)
                if len(e[5]) > 12:
                    e[5] = self._compress(e[5])

    def _write(self, op, ap):
        name, p0, p1, f0, f1 = _region(ap)
        if name.startswith("ps"):
            p0, p1, f0, f1 = 0, 128, 0, 1 << 20
        ents = self.track.setdefault(name, [])
        tok = self._tok(op)
        keep = []
        for e in ents:
            if e[0] < p1 and p0 < e[1] and e[2] < f1 and f0 < e[3]:
                self._add_dep(op, e[4], "waw")
                for r in e[5]:
                    self._add_dep(op, r, "war")
                if p0 <= e[0] and e[1] <= p1:
                    if e[2] < f0:
                        keep.append([e[0], e[1], e[2], f0, e[4], list(e[5])])
                    if f1 < e[3]:
                        keep.append([e[0], e[1], f1, e[3], e[4], list(e[5])])
                else:
                    keep.append(e)
            else:
                keep.append(e)
        keep.append([p0, p1, f0, f1, tok, []])
        self.track[name] = keep

    def op(self, eng, fn, writes=(), reads=()):
        o = _Op(eng, fn)
        o.idx = len(self.ops[eng])
        for ap in reads:
            self._read(o, ap)
        for ap in writes:
            self._write(o, ap)
        self.ops[eng].append(o)
        return o

    def dma(self, eng, out, in_, sem, out_sb=True, in_sb=False):
        o = _Op(eng, None)
        o.idx = len(self.ops[eng])
        self.dma_counts[sem] = self.dma_counts.get(sem, 0) + 16
        ends = self.gen_end.setdefault(sem, [])
        o.dma = (sem, len(ends))
        if len(ends) > 0:
            o.deps[("d", sem)] = len(ends) - 1
        o.fn = lambda e, sems: e.dma_start(out=out, in_=in_).then_inc(sems[sem], 16)
        if in_sb:
            self._read(o, in_)
        if out_sb:
            self._write(o, out)
        self.ops[eng].append(o)
        return o

    def xdma(self, eng, fn, sem, writes=(), reads=()):
        o = _Op(eng, None)
        o.idx = len(self.ops[eng])
        self.dma_counts[sem] = self.dma_counts.get(sem, 0) + 16
        ends = self.gen_end.setdefault(sem, [])
        o.dma = (sem, len(ends))
        if len(ends) > 0:
            o.deps[("d", sem)] = len(ends) - 1
        o.fn = lambda e, sems: fn(e).then_inc(sems[sem], 16)
        for ap in reads:
            self._read(o, ap)
        for ap in writes:
            self._write(o, ap)
        self.ops[eng].append(o)
        return o

    def close(self, sem):
        ends = self.gen_end.setdefault(sem, [])
        c = self.dma_counts.get(sem, 0)
        if not ends or ends[-1] != c:
            ends.append(c)

    def emit(self, stack, final_waits=()):
        nc = self.nc
        for sname in list(self.dma_counts):
            self.close(sname)
        for e in ENGS:
            for o in self.ops[e]:
                for k, v in o.deps.items():
                    if k[0] == "e":
                        self.ops[k[1]][v].need = True
        for e in ENGS:
            c = 0
            for o in self.ops[e]:
                if o.need and o.dma is None:
                    c += 1
                o.val = c
        sems = self.sems
        for e in ENGS:
            sems["e:" + e] = stack.enter_context(nc.semaphore("s_" + e))
        for s in self.dma_counts:
            sems[s] = stack.enter_context(nc.semaphore("d_" + s))
        block = stack.enter_context(nc.Block())
        prog = self

        def run(engname, eng):
            waited = {}
            for o in prog.ops[engname]:
                for k, v in o.deps.items():
                    if k[0] == "e":
                        val = prog.ops[k[1]][v].val
                        sk = "e:" + k[1]
                    else:
                        val = prog.gen_end[k[1]][v]
                        sk = k[1]
                    if waited.get(sk, 0) < val:
                        eng.wait_ge(sems[sk], val)
                        waited[sk] = val
                if o.dma is not None:
                    o.fn(eng, sems)
                else:
                    ins = o.fn(eng)
                    if o.need:
                        ins.then_inc(sems["e:" + engname], 1)
            if engname == "sp":
                for s in final_waits:
                    eng.wait_ge(sems[s], prog.dma_counts[s])

        @block.tensor
        def _(eng):
            run("pe", eng)

        @block.scalar
        def _(eng):
            run("act", eng)

        @block.vector
        def _(eng):
            run("dve", eng)

        @block.gpsimd
        def _(eng):
            run("pool", eng)

        @block.sync
        def _(eng):
            run("sp", eng)

    def mm(self, out, lhsT, rhs, start=True, stop=True, sync_prev=False):
        o = self.op("pe", lambda e: e.matmul(out, lhsT, rhs, start=start, stop=stop),
                    [out], [lhsT, rhs])
        if sync_prev and o.idx > 0:
            o.deps[("e", "pe")] = max(o.deps.get(("e", "pe"), -1), o.idx - 1)
        return o

    def transpose(self, out, in_, ident):
        return self.op("pe", lambda e: e.transpose(out, in_, ident), [out], [in_, ident])

    def act(self, out, in_, func, bias=None, scale=None):
        kw = {}
        rd = [in_]
        if bias is not None:
            kw["bias"] = bias
            if not isinstance(bias, (int, float)):
                rd.append(bias)
        if scale is not None:
            kw["scale"] = scale
            if not isinstance(scale, (int, float)):
                rd.append(scale)
        return self.op("act", lambda e: e.activation(out=out, in_=in_, func=func, **kw), [out], rd)

    def tt(self, eng, out, in0, in1, op):
        return self.op(eng, lambda e: e.tensor_tensor(out=out, in0=in0, in1=in1, op=op),
                       [out], [in0, in1])

    def ts(self, eng, out, in0, s1, op0, s2=None, op1=None):
        rd = [in0]
        if not isinstance(s1, (int, float)):
            rd.append(s1)
        if s2 is not None and not isinstance(s2, (int, float)):
            rd.append(s2)
        if op1 is None:
            return self.op(eng, lambda e: e.tensor_single_scalar(out=out, in_=in0, scalar=s1, op=op0),
                           [out], rd)
        return self.op(eng, lambda e: e.tensor_scalar(out=out, in0=in0, scalar1=s1, scalar2=s2,
                                                      op0=op0, op1=op1), [out], rd)

    def stt(self, eng, out, in0, scalar, in1, op0, op1):
        rd = [in0, in1]
        if not isinstance(scalar, (int, float)):
            rd.append(scalar)
        return self.op(eng, lambda e: e.scalar_tensor_tensor(out=out, in0=in0, scalar=scalar, in1=in1,
                                                             op0=op0, op1=op1), [out], rd)

    def copy(self, eng, out, in_):
        if eng == "act":
            return self.op(eng, lambda e: e.copy(out=out, in_=in_), [out], [in_])
        return self.op(eng, lambda e: e.tensor_copy(out=out, in_=in_), [out], [in_])

    def memset(self, eng, ap, val):
        return self.op(eng, lambda e: e.memset(ap, val), [ap], [])

    def scan(self, eng, out, d0, d1, initial, op0, op1):
        rd = [d0, d1]
        if not isinstance(initial, (int, float)):
            rd.append(initial)
        return self.op(eng, lambda e: e.tensor_tensor_scan(out=out, data0=d0, data1=d1, initial=initial,
                                                           op0=op0, op1=op1), [out], rd)

    def recip(self, eng, out, in_):
        return self.op(eng, lambda e: e.reciprocal(out=out, in_=in_), [out], [in_])

    def reduce(self, eng, out, in_, op):
        return self.op(eng, lambda e: e.tensor_reduce(out=out, in_=in_, axis=AX.X, op=op), [out], [in_])


def build_program():
    nc = bass.Bass("TRN2", target_bir_lowering=False)
    dram = {}

    def din(name, shape):
        dram[name] = nc.dram_tensor(name, shape, F32, kind="ExternalInput").ap()
        return dram[name]

    def dout(name, shape):
        dram[name] = nc.dram_tensor(name, shape, F32, kind="ExternalOutput").ap()
        return dram[name]

    n_layers = 2
    xT_ds = [din("xT1", [D, NTOK]), din("xT2", [D, NTOK])]
    cT_d = din("cT", [128, 8])
    role_d = din("role", [128, 1])
    kf_d = din("kf", [128, 512])
    kb_d = din("kb", [128, NKB])
    L = []
    for l in range(n_layers):
        L.append(dict(
            w_ada=din(f"w_ada{l}", [D, 6 * D]),
            vecs=din(f"vecs{l}", [128, NV]),
            w_in=din(f"w_in{l}", [D, INW]),
            w_out=din(f"w_out{l}", [D, D]),
            lru_w=din(f"lru_w{l}", [128, 6 * 128]),
            gla_wg=din(f"gla_wg{l}", [16, 224]),
            w_r=din(f"w_r{l}", [D, 20]),
            b_r=din(f"b_r{l}", [128, 20]),
            w_gate=din(f"w_gate{l}", [16, D, 512]),
            w_up=din(f"w_up{l}", [16, D, 512]),
            w_down=din(f"w_down{l}", [16, 512, D]),
        ))
    yN_d = dout("yN", [D, NTOK])

    with ExitStack() as st:
        def sb(name, shape, dt=F32):
            return st.enter_context(nc.sbuf_tensor("sb_" + name, shape, dt))

        P = Prog(nc)

        x = sb("x", [128, 8, NTOK])
        AB = sb("arenaB", [128, 45056], BF16)
        AFt = sb("arenaF", [128, 8320])
        cur = {"l": 0}

        class PerLayer:
            def __init__(self, name, shape, dt=F32):
                self.t = [sb(f"{name}{i}", shape, dt) for i in range(n_layers)]

            def __getitem__(self, idx):
                return self.t[cur["l"]][idx]

        vecs = PerLayer("vecs", [128, NV])
        modv = PerLayer("modv", [128, 64])
        drv = PerLayer("drv", [128, 48])
        saved = PerLayer("saved", [128, 264])
        role = sb("role", [128, 1])
        kf = sb("kf", [128, 512])
        kb = sb("kb", [128, NKB], BF16)
        cact = sb("cact", [128, 8])
        state = sb("state", [128, 264])
        b_r = PerLayer("b_r", [128, 20])

        ident = kb[:, K_ID:K_ID + 128]
        ones = kb[:, K_ONE:K_ONE + 128]
        maskb = kb[:, K_MASK:K_MASK + 128]

        def carveB(off, shape):
            n = 1
            for s in shape[1:]:
                n *= s
            v = AB[0:shape[0], off:off + n]
            if len(shape) == 3:
                v = v.rearrange("p (a b) -> p a b", b=shape[2])
            elif len(shape) == 4:
                v = v.rearrange("p (a b c) -> p a b c", b=shape[2], c=shape[3])
            return v

        def carveF(off, shape):
            n = 1
            for s in shape[1:]:
                n *= s
            v = AFt[0:shape[0], off:off + n]
            if len(shape) == 3:
                v = v.rearrange("p (a b) -> p a b", b=shape[2])
            elif len(shape) == 4:
                v = v.rearrange("p (a b c) -> p a b c", b=shape[2], c=shape[3])
            return v

        o = 0
        w_in = carveB(o, [128, 8, INW]); o += 8 * INW
        w_out = carveB(o, [128, 9, D]); o += 9 * D
        lru_w = carveB(o, [128, 6, 128]); o += 768
        gla_wg = carveB(o, [16, 224]); o += 224
        hT = carveB(o, [128, 8, T]); o += 8 * T
        xsq = carveB(o, [128, 2, T]); o += 2 * T
        xb_bf = carveB(o, [128, 3, T]); o += 3 * T
        ybf = carveB(o, [128, 2, T]); o += 2 * T
        ysq = carveB(o, [128, 2, T]); o += 2 * T
        glr_bf = carveB(o, [16, T]); o += T
        qin = carveB(o, [128, 2, T]); o += 2 * T
        kin = carveB(o, [128, 2, T]); o += 2 * T
        kout = carveB(o, [128, 2, T]); o += 2 * T
        kT = carveB(o, [128, T // 128, 224]); o += (T // 128) * 224
        v_bf = carveB(o, [128, T // 128, 384]); o += (T // 128) * 384
        scT = carveB(o, [128, T // 128, 512]); o += (T // 128) * 512
        sprev = carveB(o, [128, T // 64, 192]); o += (T // 64) * 192
        osq = carveB(o, [128, 4, T]); o += 4 * T
        mixed = carveB(o, [128, 9, T]); o += 9 * T
        dg = carveB(o, [128, 8, 128]); o += 1024
        ubuf = carveB(o, [128, 2, 30 + T]); o += 2 * (30 + T)
        assert o <= 45056, o
        o = 0
        h2 = carveB(o, [128, 8, NTOK]); o += 8 * NTOK
        wg_s = [carveB(o + i * 12288, [128, 8, 512]) for i in range(2)]
        wu_s = [carveB(o + i * 12288 + 4096, [128, 8, 512]) for i in range(2)]
        wd_s = [carveB(o + i * 12288 + 8192, [128, 4, D]) for i in range(2)]
        o += 2 * 12288
        hid = [carveB(o, [128, 4, TM]) for i in range(2)]
        xsq2 = carveB(o, [128, 2, TM])
        comb2 = carveB(o + 1024, [128, 16, 32])
        o += 2048
        combT = carveB(o, [32, NTOK]); o += NTOK
        assert o <= 45056, o
        w_r = PerLayer("w_r", [128, 8, 20], BF16)

        o = 0
        rstd = carveF(o, [128, T]); o += T
        xs = carveF(o, [128, 2, T]); o += 2 * T
        sg = carveF(o, [128, 2, T]); o += 2 * T
        yc = carveF(o, [128, 2, T]); o += 2 * T
        cmean = carveF(o, [128, T]); o += T
        crstd = carveF(o, [128, T]); o += T
        lrx = carveF(o, [128, 3, 3 + T]); o += 3 * (3 + T)
        LBs = []
        for c in range(2):
            LBs.append([carveF(o + i * T, [128, T]) for i in range(4)])
            o += 4 * T
        LB = [LBs[0], LBs[1], LBs[0]]
        T1 = carveF(o, [128, 2, T]); o += 2 * T
        T2 = carveF(o, [128, 2, T]); o += 2 * T
        HB = []
        for c in range(4):
            HB.append([carveF(o + i * T, [128, T]) for i in range(2)])
            o += 2 * T
        assert o <= 8320, o
        wada_f = carveF(0, [128, 8, 512])
        modrow = carveF(4096, [1, 512])
        wada_b = [carveB(i * 4096, [128, 8, 512]) for i in range(2)]
        o = 0
        rstd2 = carveF(o, [128, TM]); o += TM
        xs2 = carveF(o, [128, 2, TM]); o += 2 * TM
        sa = carveF(o, [128, 2, TM]); o += 2 * TM
        tu = carveF(o, [128, 2, TM]); o += 2 * TM
        cb = carveF(o, [128, 2, TM]); o += 2 * TM
        ost = carveF(o, [128, 2, TM]); o += 2 * TM
        small = carveF(o, [128, 2048]); o += 2048
        assert o <= 8320, o

        psb = [st.enter_context(nc.psum_tensor(f"ps{i}", [128, 512], F32)) for i in range(7)]
        pst = st.enter_context(nc.psum_tensor("pst", [128, 1024], BF16))
        ps_state = {"h": 0, "b": 0}

        def ps_bank():
            i = ps_state["b"]
            ps_state["b"] = (i + 1) % 7
            return psb[i][:, :]

        def ps_half():
            return ps_bank()[:, 0:256]

        P.dma("sp", kf[:], kf_d[:], "ld_c")
        P.dma("sp", role[:], role_d[:], "ld_c")
        P.dma("sp", cact[:], cT_d[:], "ld_c")
        P.dma("pool", kb[:], kb_d[:], "ld_kb")
        P.close("ld_c")
        P.close("ld_kb")
        P.memset("dve", state[:], 0.0)
        P.memset("dve", AFt[:, :], 0.0)
        P.memset("dve", AB[:, :], 0.0)
        P.act(cact[:], cact[:], AF.Silu)

        def load_mixer_weights(Ld):
            for kc in range(8):
                P.dma("pool", w_in[:, kc, :], Ld["w_in"][kc * 128:(kc + 1) * 128, :], "ld_wm")
            for j in range(5):
                P.dma("pool", w_out[:, j, :], Ld["w_out"][j * 128:(j + 1) * 128, :], "ld_wm")
            for h in range(4):
                P.dma("pool", w_out[0:96, 5 + h, :], Ld["w_out"][640 + h * 96:640 + (h + 1) * 96, :], "ld_wm")
            P.dma("pool", lru_w.rearrange("p a b -> p (a b)"), Ld["lru_w"][:, :], "ld_wm")
            P.dma("pool", gla_wg, Ld["gla_wg"][:, :], "ld_wm")
            P.close("ld_wm")

        def layer_prologue(l, Ld):
            P.dma("sp", vecs[:], Ld["vecs"][:, :], "ld_v")
            P.dma("sp", b_r[:], Ld["b_r"][:, :], "ld_v")
            for kc in range(8):
                P.dma("pool", w_r[:, kc, :], Ld["w_r"][kc * 128:(kc + 1) * 128, :], "ld_wr")
            P.close("ld_wr")
            P.close("ld_v")
            mod_ps = psb[6][:, :]
            for piece in range(12):
                c0 = piece * 512
                for kc in range(8):
                    P.dma("sp" if kc % 2 == 0 else "act", wada_f[:, kc, :],
                          Ld["w_ada"][kc * 128:(kc + 1) * 128, c0:c0 + 512], "ld_wa")
                P.close("ld_wa")
                wb = wada_b[piece % 2]
                P.copy("dve", wb, wada_f)
                prow = psb[piece % 2][:, :]
                for kc in range(8):
                    P.mm(prow[0:1, :], cact_bf[:, kc:kc + 1], wb[:, kc, :], start=(kc == 0), stop=(kc == 7))
                P.copy("act", modrow, prow[0:1, :])
                for jj in range(4):
                    j = piece * 4 + jj
                    P.mm(mod_ps[:, j:j + 1], modrow[0:1, jj * 128:(jj + 1) * 128], onef[0:1, 0:1])
            P.tt("dve", modv[:, 0:48], mod_ps[:, 0:48], vecs[:, V_BADA:V_BADA + 48], ALU.add)
            P.stt("dve", drv[:, 0:8], modv[:, 8:16], 1.0, vecs[:, V_GMIX:V_GMIX + 8], ALU.add, ALU.mult)
            P.stt("dve", drv[:, 8:16], modv[:, 32:40], 1.0, vecs[:, V_GFFN:V_GFFN + 8], ALU.add, ALU.mult)
            P.act(drv[:, 22:25], vecs[:, V_LAM:V_LAM + 3], AF.Exp, scale=-1.0)
            P.act(drv[:, 22:25], drv[:, 22:25], AF.Ln, bias=1.0)
            P.ts("dve", drv[:, 16:19], drv[:, 22:25], -8.0, ALU.mult)
            P.ts("dve", drv[:, 19:22], drv[:, 22:25], -16.0, ALU.mult)

        A1 = lambda c: drv[:, c:c + 1]
        A2 = lambda c: drv[:, 8 + c:9 + c]
        SH1 = lambda c: modv[:, c:c + 1]
        GT1 = lambda c: modv[:, 16 + c:17 + c]
        SH2 = lambda c: modv[:, 24 + c:25 + c]
        GT2 = lambda c: modv[:, 40 + c:41 + c]
        vcol = lambda j: vecs[:, j:j + 1]

        def rms_mod(t0, tw, sq_buf, rstd_buf, xs_buf, dst, Afn, Bfn):
            ms = ps_bank() if tw == 512 else ps_half()
            for c in range(8):
                P.act(sq_buf[:, c % 2, :], x[:, c, t0:t0 + tw], AF.Square)
                P.mm(ms[:, 0:tw], ones, sq_buf[:, c % 2, :], start=(c == 0), stop=(c == 7))
            P.act(rstd_buf, ms[:, 0:tw], AF.Ln, bias=vcol_eps, scale=1.0 / D)
            P.act(rstd_buf, rstd_buf, AF.Exp, scale=-0.5)
            for c in range(8):
                P.tt("dve", xs_buf[:, c % 2, :], x[:, c, t0:t0 + tw], rstd_buf, ALU.mult)
                if Bfn is None:
                    P.act(dst(c), xs_buf[:, c % 2, :], AF.Identity, scale=Afn(c))
                else:
                    P.act(dst(c), xs_buf[:, c % 2, :], AF.Identity, scale=Afn(c), bias=Bfn(c))

        epsT = sb("epsT", [128, 1])
        P.memset("dve", epsT[:], EPS)
        onef = sb("onef", [128, 1])
        P.memset("dve", onef[:], 1.0)
        cact_bf = sb("cact_bf", [128, 8], BF16)
        P.copy("dve", cact_bf[:], cact[:])
        vcol_eps = epsT[:, 0:1]

        def rsqrt_act(dst, src, bias, scale):
            P.act(dst, src, AF.Ln, bias=bias, scale=scale)
            P.act(dst, dst, AF.Exp, scale=-0.5)

        def mixer_tile(j, light=False):
            t0 = j * T
            rms_mod(t0, T, xsq, rstd, xs, lambda c: hT[:, c, :], A1, SH1)

            def zproj(c0, m, dst):
                for kc in range(8):
                    P.mm(dst, w_in[:, kc, c0:c0 + m], hT[:, kc, :], start=(kc == 0), stop=(kc == 7))

            A, B, C = [], [[], [], []], []
            do_conv = (not light) or j == NT - 1
            cst = {}

            def a1(c):
                pg = ps_half()
                zproj(C_CVG + c * 128, 128, pg)
                P.act(sg[:, c, :], pg, AF.Sigmoid)
                pv = ps_half()
                zproj(C_CVV + c * 128, 128, pv)
                P.tt("dve", ubuf[:, c, 30:30 + T], pv, sg[:, c, :], ALU.mult)

            def a2(c):
                if not light:
                    wc = V_CDW + c * 31
                    pc = ps_half()
                    for k in range(31):
                        dk = dg[:, k % 8, :]
                        P.ts("dve", dk, ident, vcol(wc + k), ALU.mult)
                        P.mm(pc, dk, ubuf[:, c, k:k + T], start=(k == 0), stop=(k == 30))
                    P.act(yc[:, c, :], pc, AF.Identity, bias=vcol(V_CDB + c))
                    P.act(ysq[:, c, :], pc, AF.Square, bias=vcol(V_CDB + c))
                    P.act(ybf[:, c, :], pc, AF.Identity, bias=vcol(V_CDB + c))
                P.copy("dve", ubuf[:, c, 0:30], ubuf[:, c, T:T + 30])

            def a3():
                pm = ps_half()
                pq = ps_half()
                for c in range(2):
                    P.mm(pm, ones, ybf[:, c, :], start=(c == 0), stop=(c == 1))
                for c in range(2):
                    P.mm(pq, ones, ysq[:, c, :], start=(c == 0), stop=(c == 1))
                P.act(cmean, pm, AF.Identity, scale=1.0 / 256)
                P.tt("dve", crstd, cmean, cmean, ALU.mult)
                P.stt("dve", crstd, pq, 1.0 / 256, crstd, ALU.mult, ALU.subtract)
                rsqrt_act(crstd, crstd, vcol_eps, 1.0)
                for c in range(2):
                    P.tt("dve", yc[:, c, :], yc[:, c, :], cmean, ALU.subtract)
                    P.tt("dve", yc[:, c, :], yc[:, c, :], crstd, ALU.mult)
                    P.act(mixed[:, c, :], yc[:, c, :], AF.Silu, scale=vcol(V_CLG + c), bias=vcol(V_CLB + c))

            if do_conv:
                A += [lambda: a1(0), lambda: a1(1), lambda: a2(0), lambda: a2(1)]
                if not light:
                    A.append(a3)

            def b1(c):
                Bx, Ba, Bi, Bm = LB[c]
                px = ps_half()
                zproj(C_LRX + c * 128, 128, px)
                P.copy("act", lrx[:, c, 3:3 + T], px)
                wl = V_LCW + c * 4
                P.ts("dve", Bx, lrx[:, c, 0:T], vcol(wl), ALU.mult, vcol(V_LCB + c), ALU.add)
                for k in range(1, 4):
                    P.stt("dve", Bx, lrx[:, c, k:k + T], vcol(wl + k), Bx, ALU.mult, ALU.add)
                P.copy("dve", lrx[:, c, 0:3], lrx[:, c, T:T + 3])
                P.copy("act", xb_bf[:, c, :], Bx)

            def b2(c):
                Bx, Ba, Bi, Bm = LB[c]
                pa = ps_half()
                P.mm(pa, lru_w[:, c, :], xb_bf[:, c, :])
                pi = ps_half()
                P.mm(pi, lru_w[:, 3 + c, :], xb_bf[:, c, :])
                P.act(Ba, pa, AF.Sigmoid, bias=vcol(V_LBA + c))
                P.act(Bi, pi, AF.Sigmoid, bias=vcol(V_LBI + c))
                P.act(Bm, Ba, AF.Exp, scale=drv[:, 19 + c:20 + c])
                P.act(Ba, Ba, AF.Exp, scale=drv[:, 16 + c:17 + c])
                P.ts("dve", Bm, Bm, -1.0, ALU.mult, 1.0, ALU.add)
                P.act(Bm, Bm, AF.Ln)
                P.act(Bm, Bm, AF.Exp, scale=0.5)
                P.tt("dve", Bi, Bi, Bx, ALU.mult)
                P.tt("dve", Bi, Bi, Bm, ALU.mult)

            def b3(c):
                Bx, Ba, Bi, Bm = LB[c]
                Bh, Bg = Bm, Bx
                P.scan("dve", Bh, Ba, Bi, state[:, c:c + 1], ALU.mult, ALU.add)
                P.copy("dve", state[:, c:c + 1], Bh[:, T - 1:T])
                if not light:
                    py = ps_half()
                    zproj(C_LRY + c * 128, 128, py)
                    P.act(Bg, py, AF.Gelu_apprx_tanh)
                    P.tt("dve", mixed[:, 2 + c, :], Bh, Bg, ALU.mult)

            for c in range(3):
                B[c] += [lambda c=c: b1(c), lambda c=c: b2(c), lambda c=c: b3(c)]

            e1v = T1[0:112, :, :].rearrange("p a (n c) -> p a n c", c=64)

            def c1():
                pgl = ps_half()
                zproj(C_GLR, 16, pgl[0:16, :])
                P.copy("act", glr_bf, pgl[0:16, :])
                for p in range(2):
                    pl = ps_half()
                    P.mm(pl[0:112, :], gla_wg[:, p * 112:(p + 1) * 112], glr_bf)
                    P.act(T1[0:112, p, :], pl[0:112, :], AF.Identity, bias=vecs[0:112, V_GBG + p:V_GBG + p + 1])
                t1 = T1[0:112, :, :]
                t2 = T2[0:112, :, :]
                P.stt("dve", t2, t1, -1.0, t1, ALU.mult, ALU.min)
                P.act(t2, t2, AF.Exp)
                P.act(t2, t2, AF.Ln, bias=1.0)
                P.ts("dve", t1, t1, 0.0, ALU.min)
                P.tt("dve", t1, t1, t2, ALU.subtract)
                t1f = T1[0:112, :, :].rearrange("p a t -> p (a t)")
                t2f = T2[0:112, :, :].rearrange("p a t -> p (a t)")
                P.scan("dve", t2f, kf[0:112, 0:2 * T], t1f, 0.0, ALU.mult, ALU.add)
                P.act(t1, t2, AF.Exp, scale=1.0 / 16)
                P.act(t2, t2, AF.Exp, scale=-1.0 / 16)

            def c2():
                t2 = T2[0:112, :, :]
                for p in range(2):
                    if not light:
                        pqq = ps_half()
                        zproj(C_Q + p * 112, 112, pqq[0:112, :])
                        P.stt("dve", qin[0:112, p, :], pqq[0:112, :], 48.0 ** -0.5, T1[0:112, p, :], ALU.mult, ALU.mult)
                    pk = ps_half()
                    zproj(C_K + p * 112, 112, pk[0:112, :])
                    P.tt("dve", T2[0:112, p, :], pk[0:112, :], T2[0:112, p, :], ALU.mult)
                if not light:
                    P.copy("act", kin[0:112, :, :], t2)
                e1last = e1v[:, :, :, 63:64].to_broadcast([112, 2, T // 64, 64])
                P.tt("dve", kout[0:112, :, :].rearrange("p a (n c) -> p a n c", c=64),
                     T2[0:112, :, :].rearrange("p a (n c) -> p a n c", c=64), e1last, ALU.mult)

            def c0():
                for g in range(T // 128):
                    pvv = ps_bank()
                    for kc in range(8):
                        P.mm(pvv[:, 0:384], hT[:, kc, g * 128:(g + 1) * 128], w_in[:, kc, C_V:C_V + 384],
                             start=(kc == 0), stop=(kc == 7))
                    P.copy("act", v_bf[:, g, :], pvv[:, 0:384])

            def c3():
                for g in range(T // 128):
                    for p in range(2):
                        P.transpose(pst[:, (g * 2 + p) * 112:(g * 2 + p + 1) * 112],
                                    kout[0:112, p, g * 128:(g + 1) * 128], ident[0:112, 0:112])
                    P.copy("act", kT[:, g, :], pst[:, g * 224:(g + 1) * 224])
                if not light:
                    for g in range(T // 128):
                        pscs = [ps_bank(), ps_bank()]
                        for h in (0, 2, 1, 3):
                            p, hp = h // 2, (h % 2) * 64
                            P.mm(pscs[h % 2][:, p * 128:(p + 1) * 128], kin[hp:hp + 48, p, g * 128:(g + 1) * 128],
                                 qin[hp:hp + 48, p, g * 128:(g + 1) * 128])
                        scv = scT[:, g, :].rearrange("p (a b c) -> p a b c", b=2, c=128)
                        for par in range(2):
                            P.tt("dve", scv[:, :, par, :], pscs[par][:, 0:256].rearrange("p (a c) -> p a c", c=128),
                                 maskb.unsqueeze(1).to_broadcast([128, 2, 128]), ALU.mult)

            def c4():
                Sv = state[0:112, 3:195].rearrange("p (a e) -> p a e", e=96)
                for n in range(T // 64):
                    g, r0 = n // 2, (n % 2) * 64
                    pkv = ps_half()
                    for h in (0, 2, 1, 3):
                        p, hp = h // 2, (h % 2) * 64
                        P.mm(pkv[hp:hp + 48, p * 96:(p + 1) * 96],
                             kT[r0:r0 + 64, g, p * 112 + hp:p * 112 + hp + 48],
                             v_bf[r0:r0 + 64, g, h * 96:(h + 1) * 96], sync_prev=(h == 1))
                    if not light:
                        P.copy("act", sprev[0:112, n, :].rearrange("p (a e) -> p a e", e=96), Sv)
                    for p in range(2):
                        P.stt("dve", Sv[:, p, :], Sv[:, p, :], e1v[:, p, n, 63:64], pkv[0:112, p * 96:(p + 1) * 96],
                              ALU.mult, ALU.add)

            def c5_mm():
                b0 = ps_state["b"]
                idxs = [(b0 + k) % 7 for k in range(4)]
                ps_state["b"] = (b0 + 4) % 7
                pos = [psb[i][:, 0:256] for i in idxs]
                cst["po"] = pos
                cst["free"] = [i for i in range(7) if i not in idxs]
                cst["fi"] = 0
                for n in range(T // 64):
                    g = n // 2
                    for h in range(4):
                        P.mm(pos[h][0:96, n * 64:(n + 1) * 64], v_bf[:, g, h * 96:(h + 1) * 96],
                             scT[:, g, h * 128 + (n % 2) * 64:h * 128 + (n % 2) * 64 + 64], start=True, stop=False)
                    for h in (0, 2, 1, 3):
                        p, hp = h // 2, (h % 2) * 64
                        P.mm(pos[h][0:96, n * 64:(n + 1) * 64], sprev[hp:hp + 48, n, p * 96:(p + 1) * 96],
                             qin[hp:hp + 48, p, n * 64:(n + 1) * 64], start=False, stop=True)

            def c5_tail():
                pos = cst["po"]

                def fbank():
                    i = cst["free"][cst["fi"] % 3]
                    cst["fi"] += 1
                    return psb[i][:, 0:256]
                for h in range(4):
                    P.act(osq[0:96, h, :], pos[h][0:96, :], AF.Square)
                for h in range(4):
                    pms = fbank()
                    P.mm(pms[0:96, :], ones[0:96, 0:96], osq[0:96, h, :])
                    P.act(HB[h][0][0:96, :], pms[0:96, :], AF.Ln, bias=epsT[0:96, 0:1], scale=1.0 / 96)
                for h in range(4):
                    P.act(HB[h][0][0:96, :], HB[h][0][0:96, :], AF.Exp, scale=-0.5)
                for h in range(4):
                    rs_h, sog_h = HB[h]
                    P.stt("dve", rs_h[0:96, :], pos[h][0:96, :], vecs[0:96, V_GNG + h:V_GNG + h + 1], rs_h[0:96, :],
                          ALU.mult, ALU.mult)
                    P.tt("dve", mixed[0:96, 5 + h, :], rs_h[0:96, :], sog_h[0:96, :], ALU.mult)

            def c_og():
                for h in range(4):
                    pog = ps_half()
                    zproj(C_OG + h * 96, 96, pog[0:96, :])
                    P.act(HB[h][1][0:96, :], pog[0:96, :], AF.Silu)

            C += [c0, c1, c2, c3, c4]
            if not light:
                C.insert(1, c_og)
            if not light:
                def c5_all():
                    c5_mm()
                    c5_tail()
                C += [c5_all]

            Bflat = B[0] + B[1] + B[2]
            if INTERLEAVE:
                lists = [A, Bflat, C]
                pos = [0, 0, 0]
                while any(pos[i] < len(lists[i]) for i in range(3)):
                    for i in range(3):
                        if pos[i] < len(lists[i]):
                            lists[i][pos[i]]()
                            pos[i] += 1
            else:
                for f in A + Bflat + C:
                    f()
            if light:
                return
            for dc in range(8):
                pw = ps_half()
                for kc in range(9):
                    kk = 128 if kc < 5 else 96
                    P.mm(pw, w_out[0:kk, kc, dc * 128:(dc + 1) * 128], mixed[0:kk, kc, :],
                         start=(kc == 0), stop=(kc == 8))
                P.stt("dve", x[:, dc, t0:t0 + T], pw, GT1(dc), x[:, dc, t0:t0 + T], ALU.mult, ALU.add)

        def moe_layer(Ld):
            def load_expert(e):
                s = e % 2
                for kc in range(8):
                    P.dma("pool", wg_s[s][:, kc, :], Ld["w_gate"][e, kc * 128:(kc + 1) * 128, :], f"ld_e{s}")
                    P.dma("pool", wu_s[s][:, kc, :], Ld["w_up"][e, kc * 128:(kc + 1) * 128, :], f"ld_e{s}")
                for kc in range(4):
                    P.dma("pool", wd_s[s][:, kc, :], Ld["w_down"][e, kc * 128:(kc + 1) * 128, :], f"ld_e{s}")
                P.close(f"ld_e{s}")

            for tm in range(NTM):
                rms_mod(tm * TM, TM, xsq2, rstd2, xs2, lambda c, tm=tm: h2[:, c, tm * TM:(tm + 1) * TM], A2, SH2)
            load_expert(0)
            load_expert(1)
            plg = ps_bank()
            for s in range(16):
                for kc in range(8):
                    P.mm(plg[:, s * 20:(s + 1) * 20], h2[:, kc, s * 128:(s + 1) * 128], w_r[:, kc, :],
                         start=(kc == 0), stop=(kc == 7))
            o = 0

            def sm(n):
                nonlocal o
                v = small[:, o:o + n]
                o += n
                return v
            Lg = sm(320).rearrange("p (s k) -> p s k", k=20)
            P.tt("dve", Lg, plg[:, 0:320].rearrange("p (s k) -> p s k", k=20),
                 b_r[:].unsqueeze(1).to_broadcast([128, 16, 20]), ALU.add)
            gl = Lg[:, :, 0:4]
            gmax = sm(16)
            P.reduce("dve", gmax, gl, ALU.max)
            ohg = sm(64).rearrange("p (s k) -> p s k", k=4)
            P.tt("dve", ohg, gl, gmax.unsqueeze(2).to_broadcast([128, 16, 4]), ALU.is_ge)
            ex = sm(64).rearrange("p (s k) -> p s k", k=4)
            P.tt("dve", ex, gl, gmax.unsqueeze(2).to_broadcast([128, 16, 4]), ALU.subtract)
            P.act(ex, ex, AF.Exp)
            psel = sm(16)
            P.reduce("dve", psel, ex, ALU.add)
            P.recip("dve", psel, psel)
            le = Lg[:, :, 4:20].rearrange("p s (g j) -> p s g j", j=4)
            tmp4 = sm(256).rearrange("p (s g j) -> p s g j", g=4, j=4)
            P.tt("dve", tmp4, le, ohg.unsqueeze(3).to_broadcast([128, 16, 4, 4]), ALU.mult)
            el = sm(64).rearrange("p (s j) -> p s j", j=4)
            P.reduce("dve", el, tmp4.rearrange("p s g j -> p s j g"), ALU.add)
            m1 = sm(16)
            P.reduce("dve", m1, el, ALU.max)
            oh1 = sm(64).rearrange("p (s j) -> p s j", j=4)
            P.tt("dve", oh1, el, m1.unsqueeze(2).to_broadcast([128, 16, 4]), ALU.is_ge)
            el2 = sm(64).rearrange("p (s j) -> p s j", j=4)
            P.stt("dve", el2, oh1, -1e30, el, ALU.mult, ALU.add)
            m2 = sm(16)
            P.reduce("dve", m2, el2, ALU.max)
            oh2 = sm(64).rearrange("p (s j) -> p s j", j=4)
            P.tt("dve", oh2, el2, m2.unsqueeze(2).to_broadcast([128, 16, 4]), ALU.is_ge)
            dd = sm(16)
            P.tt("dve", dd, m2, m1, ALU.subtract)
            P.act(dd, dd, AF.Exp)
            w1 = sm(16)
            P.ts("dve", w1, dd, 1.0, ALU.add)
            P.recip("dve", w1, w1)
            w2 = sm(16)
            P.tt("dve", w2, dd, w1, ALU.mult)
            P.tt("dve", w1, w1, psel, ALU.mult)
            P.tt("dve", w2, w2, psel, ALU.mult)
            wj = sm(64).rearrange("p (s j) -> p s j", j=4)
            P.tt("dve", wj, oh1, w1.unsqueeze(2).to_broadcast([128, 16, 4]), ALU.mult)
            P.tt("dve", oh2, oh2, w2.unsqueeze(2).to_broadcast([128, 16, 4]), ALU.mult)
            P.tt("dve", wj, wj, oh2, ALU.add)
            comb = sm(256).rearrange("p (s g j) -> p s g j", g=4, j=4)
            P.tt("dve", comb, ohg.unsqueeze(3).to_broadcast([128, 16, 4, 4]),
                 wj.unsqueeze(2).to_broadcast([128, 16, 4, 4]), ALU.mult)
            combf = comb.rearrange("p s g j -> p s (g j)")
            P.copy("dve", comb2[:, :, 0:16], combf)
            chif = sm(256).rearrange("p (s k) -> p s k", k=16)
            P.copy("dve", chif, comb2[:, :, 0:16])
            P.tt("dve", comb2[:, :, 16:32], combf, chif, ALU.subtract)
            for half in range(2):
                for s8 in range(8):
                    s = half * 8 + s8
                    P.transpose(pst[0:32, s8 * 128:(s8 + 1) * 128], comb2[:, s, :], ident)
                P.copy("act", combT[:, half * 1024:(half + 1) * 1024], pst[0:32, 0:1024])
            for e in range(16):
                s = e % 2
                wg, wu, wd = wg_s[s], wu_s[s], wd_s[s]
                for tm in range(NTM):
                    tsl = slice(tm * TM, (tm + 1) * TM)
                    pcb = ps_bank()
                    P.mm(pcb, kb[0:32, K_SEL + e * 128:K_SEL + (e + 1) * 128], combT[:, tsl])
                    cbv = cb[:, tm % 2, :]
                    P.copy("act", cbv, pcb)
                    hd = hid[0]
                    for fc in range(4):
                        pa = ps_bank()
                        for kc in range(8):
                            P.mm(pa, wg[:, kc, fc * 128:(fc + 1) * 128], h2[:, kc, tsl], start=(kc == 0), stop=(kc == 7))
                        pu = ps_bank()
                        for kc in range(8):
                            P.mm(pu, wu[:, kc, fc * 128:(fc + 1) * 128], h2[:, kc, tsl], start=(kc == 0), stop=(kc == 7))
                        P.act(sa[:, fc % 2, :], pa, AF.Silu)
                        P.tt("dve", tu[:, fc % 2, :], pu, cbv, ALU.mult)
                        P.tt("dve", hd[:, fc, :], sa[:, fc % 2, :], tu[:, fc % 2, :], ALU.mult)
                    for dcs in ((0, 1, 2), (3, 4, 5), (6, 7)):
                        pys = [ps_bank() for _ in dcs]
                        for fc in range(4):
                            for i, dc in enumerate(dcs):
                                P.mm(pys[i], wd[:, fc, dc * 128:(dc + 1) * 128], hd[:, fc, :],
                                     start=(fc == 0), stop=(fc == 3))
                        for i, dc in enumerate(dcs):
                            P.stt("dve", x[:, dc, tsl], pys[i], GT2(dc), x[:, dc, tsl], ALU.mult, ALU.add)
                if e + 2 < 16:
                    load_expert(e + 2)

        for phase in range(2):
            for c in range(8):
                P.dma("sp", x[:, c, :], xT_ds[phase][c * 128:(c + 1) * 128, :], "ld_x")
            P.close("ld_x")
            for l in range(n_layers):
                Ld = L[l]
                cur["l"] = l
                if phase == 0:
                    layer_prologue(l, Ld)
                load_mixer_weights(Ld)
                if phase == 0:
                    P.memset("dve", state[:], 0.0)
                else:
                    P.ts("dve", state[:], saved[:, :], role[:, 0:1], ALU.mult)
                for c in range(2):
                    P.copy("dve", ubuf[:, c, 0:30], state[:, 195 + c * 30:195 + (c + 1) * 30])
                for c in range(3):
                    P.copy("dve", lrx[:, c, 0:3], state[:, 255 + c * 3:255 + (c + 1) * 3])
                light = (phase == 0 and l == n_layers - 1)
                for j in range(NT):
                    mixer_tile(j, light=light)
                if phase == 0:
                    for c in range(2):
                        P.copy("dve", state[:, 195 + c * 30:195 + (c + 1) * 30], ubuf[:, c, 0:30])
                    for c in range(3):
                        P.copy("dve", state[:, 255 + c * 3:255 + (c + 1) * 3], lrx[:, c, 0:3])
                    P.copy("dve", saved[:, :], state[:])
                if not (phase == 0 and l == n_layers - 1):
                    moe_layer(Ld)

        yNv = yN_d.rearrange("(c p) t -> p c t", p=128)
        GF = lambda c: vecs[:, V_GFIN + c:V_GFIN + c + 1]
        for tm in range(NTM):
            tsl = slice(tm * TM, (tm + 1) * TM)
            cnt = [0]

            def dstf(c):
                return ost[:, c % 2, :]
            ms = ps_bank()
            for c in range(8):
                P.act(xsq2[:, c % 2, :], x[:, c, tsl], AF.Square)
                P.mm(ms, ones, xsq2[:, c % 2, :], start=(c == 0), stop=(c == 7))
            P.act(rstd2, ms, AF.Ln, bias=vcol_eps, scale=1.0 / D)
            P.act(rstd2, rstd2, AF.Exp, scale=-0.5)
            for c in range(8):
                P.tt("dve", xs2[:, c % 2, :], x[:, c, tsl], rstd2, ALU.mult)
                P.act(ost[:, c % 2, :], xs2[:, c % 2, :], AF.Identity, scale=GF(c))
                P.dma("sp", yNv[:, c, tsl], ost[:, c % 2, :], f"st_o{c % 2}", out_sb=False, in_sb=True)
                P.close(f"st_o{c % 2}")
        P.emit(st, final_waits=["st_o0", "st_o1"])
    return nc


def _consts():
    kf = np.ones((128, 512), np.float32)
    kf[:, 0::64] = 0.0
    kb = np.zeros((128, NKB), np.float32)
    kb[:, K_ID:K_ID + 128] = np.eye(128, dtype=np.float32)
    kb[:, K_ONE:K_ONE + 128] = 1.0
    jj = np.arange(128)[:, None]
    cc = np.arange(128)[None, :]
    kb[:, K_MASK:K_MASK + 128] = ((jj // 64 == cc // 64) & (jj <= cc)).astype(np.float32)
    for e in range(16):
        kb[e, K_SEL + e * 128:K_SEL + (e + 1) * 128] = 1.0
        kb[16 + e, K_SEL + e * 128:K_SEL + (e + 1) * 128] = 1.0
    return kf, kb


def _col(v):
    v = np.asarray(v, np.float32)
    return np.ascontiguousarray(v.reshape(-1, 128).T)


def _layer_inputs(inp, l):
    f = lambda k: np.asarray(inp[k][l], np.float32)
    vecs = np.zeros((128, NV), np.float32)
    vecs[:, V_GMIX:V_GMIX + 8] = _col(f("g_mix"))
    vecs[:, V_GFFN:V_GFFN + 8] = _col(f("g_ffn"))
    vecs[:, V_CDB:V_CDB + 2] = _col(f("conv_dw_b"))
    vecs[:, V_CLG:V_CLG + 2] = _col(f("conv_ln_g"))
    vecs[:, V_CLB:V_CLB + 2] = _col(f("conv_ln_b"))
    vecs[:, V_LCB:V_LCB + 3] = _col(f("lru_conv_b"))
    vecs[:, V_LBA:V_LBA + 3] = _col(f("lru_b_a"))
    vecs[:, V_LBI:V_LBI + 3] = _col(f("lru_b_i"))
    vecs[:, V_LAM:V_LAM + 3] = _col(f("lru_lam"))
    gng = f("gla_norm_g").reshape(4, 96)
    bg = f("gla_b_gate").reshape(4, 48)
    for h in range(4):
        vecs[0:96, V_GNG + h] = gng[h]
        vecs[(h % 2) * 64:(h % 2) * 64 + 48, V_GBG + h // 2] = bg[h]
    vecs[:, V_BADA:V_BADA + 48] = _col(f("b_ada"))
    cw = f("conv_dw_w")
    for c in range(2):
        vecs[:, V_CDW + c * 31:V_CDW + (c + 1) * 31] = cw[:, c * 128:(c + 1) * 128].T
    lw = f("lru_conv_w")
    for c in range(3):
        vecs[:, V_LCW + c * 4:V_LCW + (c + 1) * 4] = lw[:, c * 128:(c + 1) * 128].T
    vecs[:, V_GFIN:V_GFIN + 8] = _col(np.asarray(inp["g_final"], np.float32))
    w_in = f("w_in")
    wp = np.zeros((D, INW), np.float32)
    wp[:, 0:1280] = w_in[:, 0:1280]
    for h in range(4):
        p, hp = h // 2, (h % 2) * 64
        wp[:, C_Q + p * 112 + hp:C_Q + p * 112 + hp + 48] = w_in[:, 1280 + h * 48:1280 + (h + 1) * 48]
        wp[:, C_K + p * 112 + hp:C_K + p * 112 + hp + 48] = w_in[:, 1472 + h * 48:1472 + (h + 1) * 48]
    wp[:, C_V:C_V + 384] = w_in[:, 1664:2048]
    wp[:, C_GLR:C_GLR + 16] = w_in[:, 2048:2064]
    wp[:, C_OG:C_OG + 384] = w_in[:, 2064:2448]
    wgt = f("gla_w_gate")
    gwg = np.zeros((16, 224), np.float32)
    for h in range(4):
        p, hp = h // 2, (h % 2) * 64
        gwg[:, p * 112 + hp:p * 112 + hp + 48] = wgt[:, h * 48:(h + 1) * 48]
    lru_w = np.zeros((128, 6, 128), np.float32)
    wa, wi = f("lru_w_a"), f("lru_w_i")
    for c in range(3):
        for b in range(2):
            lru_w[b * 64:(b + 1) * 64, c, b * 64:(b + 1) * 64] = wa[2 * c + b]
            lru_w[b * 64:(b + 1) * 64, 3 + c, b * 64:(b + 1) * 64] = wi[2 * c + b]
    w_r = np.concatenate([f("w_route_group")] + [f("w_route_expert")[g] for g in range(4)], axis=1)
    b_r = np.concatenate([f("b_route_group"), f("b_route_expert").reshape(-1)])
    return {
        "w_ada": np.ascontiguousarray(f("w_ada")),
        "vecs": vecs,
        "w_in": wp,
        "w_out": np.ascontiguousarray(f("w_out")),
        "lru_w": np.ascontiguousarray(lru_w.reshape(128, 768)),
        "gla_wg": gwg,
        "w_r": np.ascontiguousarray(w_r),
        "b_r": np.ascontiguousarray(np.broadcast_to(b_r[None, :], (128, 20))),
        "w_gate": np.ascontiguousarray(f("w_gate").reshape(16, D, 512)),
        "w_up": np.ascontiguousarray(f("w_up").reshape(16, D, 512)),
        "w_down": np.ascontiguousarray(f("w_down").reshape(16, 512, D)),
    }


_NC_CACHE = {}


def _get_nc():
    if "nc" not in _NC_CACHE:
        _NC_CACHE["nc"] = build_program()
    return _NC_CACHE["nc"]


def make_in_maps(inputs, cores=range(8)):
    x = np.asarray(inputs["x"], np.float32)
    c = np.asarray(inputs["c"], np.float32)
    kf, kb = _consts()
    Lin = [_layer_inputs(inputs, l) for l in range(2)]
    halves = {}
    in_maps = []
    for core in cores:
        b, h = core // 2, core % 2
        for hh in (0, h):
            if (b, hh) not in halves:
                halves[(b, hh)] = np.ascontiguousarray(x[b, hh * NTOK:(hh + 1) * NTOK, :].T)
        m = {"xT1": halves[(b, 0)], "xT2": halves[(b, h)], "cT": _col(c[b]),
             "role": np.full((128, 1), float(h), np.float32), "kf": kf, "kb": kb}
        for l in range(2):
            for k, v in Lin[l].items():
                m[f"{k}{l}"] = v
        in_maps.append(m)
    return in_maps


def kernel(**inputs):
    x = np.asarray(inputs["x"], np.float32)
    out = np.empty_like(x)
    nc = _get_nc()
    in_maps = make_in_maps(inputs)
    res = run_bass_kernel_spmd(nc, in_maps, core_ids=list(range(8)))
    for core in range(8):
        b, h = core // 2, core % 2
        out[b, h * NTOK:(h + 1) * NTOK, :] = res.results[core]["yN"].T
    return out
```
